# Optimizing a Trainium2 kernel written in Bass

```python
import math
import jax, jax.numpy as jnp
from jax import lax
import numpy as np

D_MODEL = 1024
BATCH = 4
SEQ = 8192
DEPTH = 2

GRID_W = 64
CTX_LEN = 256

H_A = 8
KV_A = 2
G_A = H_A // KV_A
DH_A = 64
WINDOW = 128
BLOCK = 128
BAND = BLOCK + 2 * WINDOW

H_B = 8
NOPE_B = 64
ROPE_B = 32
V_B = 64
Q_RANK = 384
KV_RANK = 256
MLA_SCALE = (NOPE_B + ROPE_B) ** -0.5

W_QA = H_A * DH_A
W_KA = KV_A * DH_A
W_VA = KV_A * DH_A
IN_WIDTHS = (W_QA, W_KA, W_VA, Q_RANK, KV_RANK, ROPE_B)
IN_DIM = sum(IN_WIDTHS)
SPLIT_POINTS = tuple(int(v) for v in np.cumsum(IN_WIDTHS)[:-1])
MIX_DIM = H_A * DH_A + H_B * V_B

N_EXPERTS = 16
CAP_FACTOR = 2
D_FF = 512

ROPE_BASE = 10000.0
EPS = 1e-6

kernel_name = "hybrid_swa_mla_ec_moe_dit"


def rmsnorm(x, w):
    xf = x.astype(jnp.float32)
    y = xf * lax.rsqrt(jnp.mean(xf * xf, axis=-1, keepdims=True) + EPS)
    return (y * w.astype(jnp.float32)).astype(x.dtype)


def modulate(h, shift, scale):
    return h * (1 + scale) + shift


def axial_rope(x, rows, cols):
    d = x.shape[-1]
    da = d // 2
    nf = da // 2
    inv = ROPE_BASE ** (-jnp.arange(nf, dtype=jnp.float32) / nf)

    def rot(xa, pos):
        ang = pos.astype(jnp.float32)[:, None] * inv[None, :]
        cos = jnp.cos(ang)[None, :, None, :]
        sin = jnp.sin(ang)[None, :, None, :]
        x1, x2 = xa[..., :nf], xa[..., nf:]
        return jnp.concatenate([x1 * cos - x2 * sin, x1 * sin + x2 * cos], axis=-1)

    xf = x.astype(jnp.float32)
    out = jnp.concatenate([rot(xf[..., :da], rows), rot(xf[..., da:], cols)], axis=-1)
    return out.astype(x.dtype)


def gqa_sink_attend(qb, keys, vals, bias, sink_g):
    s = jnp.einsum('bqkgd,bskd->bkgqs', qb, keys, preferred_element_type=jnp.float32) * (DH_A ** -0.5)
    if bias is not None:
        s = s + bias
    b, q = qb.shape[0], qb.shape[1]
    s_sink = jnp.broadcast_to(sink_g.astype(jnp.float32)[None, :, :, None, None], (b, KV_A, G_A, q, 1))
    p = jax.nn.softmax(jnp.concatenate([s, s_sink], axis=-1), axis=-1)[..., :-1]
    o = jnp.einsum('bkgqs,bskd->bqkgd', p.astype(vals.dtype), vals)
    return o.reshape(b, q, H_A * DH_A)


def window_attention_latent(q, k, v, kc, vc, sink_g):
    b, s_len = q.shape[:2]
    lc = kc.shape[1]
    nb = s_len // BLOCK
    qg = q.reshape(b, s_len, KV_A, G_A, DH_A)
    pad = ((0, 0), (WINDOW, WINDOW), (0, 0), (0, 0))
    kp = jnp.pad(k, pad)
    vp = jnp.pad(v, pad)
    ctx_bias = jnp.zeros((BLOCK, lc), jnp.float32)

    def block(i):
        start = i * BLOCK
        qb = lax.dynamic_slice_in_dim(qg, start, BLOCK, axis=1)
        kb = lax.dynamic_slice_in_dim(kp, start, BAND, axis=1)
        vb = lax.dynamic_slice_in_dim(vp, start, BAND, axis=1)
        tq = start + jnp.arange(BLOCK)
        ts = start - WINDOW + jnp.arange(BAND)
        valid = (jnp.abs(tq[:, None] - ts[None, :]) <= WINDOW) & (ts[None, :] >= 0) & (ts[None, :] < s_len)
        band_bias = jnp.where(valid, 0.0, -jnp.inf).astype(jnp.float32)
        keys = jnp.concatenate([kc, kb], axis=1)
        vals = jnp.concatenate([vc, vb], axis=1)
        bias = jnp.concatenate([ctx_bias, band_bias], axis=1)
        return gqa_sink_attend(qb, keys, vals, bias, sink_g)

    out = lax.map(block, jnp.arange(nb))
    return jnp.transpose(out, (1, 0, 2, 3)).reshape(b, s_len, H_A * DH_A)


def mla_attend(qn, qr, kn, kr, v):
    s = (jnp.einsum('bqhd,bkhd->bhqk', qn, kn, preferred_element_type=jnp.float32)
         + jnp.einsum('bqhr,bkr->bhqk', qr, kr, preferred_element_type=jnp.float32)) * MLA_SCALE
    p = jax.nn.softmax(s, axis=-1)
    o = jnp.einsum('bhqk,bkhd->bqhd', p.astype(v.dtype), v)
    return o.reshape(qn.shape[0], qn.shape[1], H_B * V_B)


def mla_latent(qn, qr, kn, kr, v):
    b, s_len = qn.shape[:2]
    nb = s_len // BLOCK

    def block(i):
        start = i * BLOCK
        qnb = lax.dynamic_slice_in_dim(qn, start, BLOCK, axis=1)
        qrb = lax.dynamic_slice_in_dim(qr, start, BLOCK, axis=1)
        return mla_attend(qnb, qrb, kn, kr, v)

    out = lax.map(block, jnp.arange(nb))
    return jnp.transpose(out, (1, 0, 2, 3)).reshape(b, s_len, H_B * V_B)


def project_groups(t, w_in, q_norm, kv_norm, w_q_up, w_kv_up):
    bt, n = t.shape[:2]
    p = jnp.einsum('bsd,de->bse', t, w_in)
    qa, ka, va, cq, ckv, kr = jnp.split(p, SPLIT_POINTS, axis=-1)
    qa = qa.reshape(bt, n, H_A, DH_A)
    ka = ka.reshape(bt, n, KV_A, DH_A)
    va = va.reshape(bt, n, KV_A, DH_A)
    qb = jnp.einsum('bsr,re->bse', rmsnorm(cq, q_norm), w_q_up).reshape(bt, n, H_B, NOPE_B + ROPE_B)
    kvb = jnp.einsum('bsr,re->bse', rmsnorm(ckv, kv_norm), w_kv_up).reshape(bt, n, H_B, NOPE_B + V_B)
    return (qa, ka, va, qb[..., :NOPE_B], qb[..., NOPE_B:], kr,
            kvb[..., :NOPE_B], kvb[..., NOPE_B:])


def token_mixers(h, hc, rows, cols, w_in, sink, q_norm, kv_norm, w_q_up, w_kv_up, w_out, with_ctx_out):
    sink_g = sink.reshape(KV_A, G_A)
    qa, ka, va, qbn, qbr, kr, kbn, vb = project_groups(h, w_in, q_norm, kv_norm, w_q_up, w_kv_up)
    qa_c, ka_c, va_c, qbn_c, qbr_c, kr_c, kbn_c, vb_c = project_groups(hc, w_in, q_norm, kv_norm, w_q_up, w_kv_up)
    qa = axial_rope(qa, rows, cols)
    ka = axial_rope(ka, rows, cols)
    qbr = axial_rope(qbr, rows, cols)
    kr = axial_rope(kr[:, :, None, :], rows, cols)[:, :, 0, :]
    o_a = window_attention_latent(qa, ka, va, ka_c, va_c, sink_g)
    o_b = mla_latent(qbn, qbr,
                     jnp.concatenate([kbn_c, kbn], axis=1),
                     jnp.concatenate([kr_c, kr], axis=1),
                     jnp.concatenate([vb_c, vb], axis=1))
    out = jnp.einsum('bse,ed->bsd', jnp.concatenate([o_a, o_b], axis=-1), w_out)
    if not with_ctx_out:
        return out, None
    lc = hc.shape[1]
    o_a_c = gqa_sink_attend(qa_c.reshape(hc.shape[0], lc, KV_A, G_A, DH_A), ka_c, va_c, None, sink_g)
    o_b_c = mla_attend(qbn_c, qbr_c, kbn_c, kr_c, vb_c)
    out_c = jnp.einsum('bse,ed->bsd', jnp.concatenate([o_a_c, o_b_c], axis=-1), w_out)
    return out, out_c


def ec_moe(h, w_router, w_gate, w_up, w_down):
    n = h.shape[1]
    cap = CAP_FACTOR * n // N_EXPERTS
    aff = jax.nn.softmax(jnp.einsum('bnd,de->bne', h, w_router, preferred_element_type=jnp.float32), axis=-1)
    vals, idx = lax.top_k(jnp.swapaxes(aff, 1, 2), cap)

    def per_sample(hs, idx_s, val_s):
        xs = hs[idx_s]
        a = jnp.einsum('ecd,edf->ecf', xs, w_gate)
        u = jnp.einsum('ecd,edf->ecf', xs, w_up)
        y = jnp.einsum('ecf,efd->ecd', jax.nn.silu(a) * u, w_down) * val_s[..., None].astype(hs.dtype)
        return jnp.zeros_like(hs).at[idx_s.reshape(-1)].add(y.reshape(-1, hs.shape[-1]).astype(hs.dtype))

    return jax.vmap(per_sample)(h, idx, vals)


def setup_inputs(seed: int = 0) -> dict:
    key = jax.random.key(seed)
    ks = jax.random.split(key, 20)
    f32 = jnp.float32
    L = DEPTH

    def nrm(k, shape, scale):
        return jax.random.normal(k, shape, f32) * scale

    return {
        "x": nrm(ks[0], (BATCH, SEQ, D_MODEL), 1.0),
        "c": nrm(ks[1], (BATCH, D_MODEL), 1.0),
        "ctx": nrm(ks[2], (BATCH, CTX_LEN, D_MODEL), 1.0),
        "c_ctx": nrm(ks[3], (D_MODEL,), 1.0),
        "w_mod": nrm(ks[4], (L, D_MODEL, 6 * D_MODEL), 0.5 * D_MODEL ** -0.5),
        "b_mod": nrm(ks[5], (L, 6 * D_MODEL), 0.01),
        "norm_attn": 1.0 + nrm(ks[6], (L, D_MODEL), 0.02),
        "norm_ffn": 1.0 + nrm(ks[7], (L, D_MODEL), 0.02),
        "w_in": nrm(ks[8], (L, D_MODEL, IN_DIM), D_MODEL ** -0.5),
        "sink": nrm(ks[9], (L, H_A), 0.5),
        "q_norm": 1.0 + nrm(ks[10], (L, Q_RANK), 0.02),
        "kv_norm": 1.0 + nrm(ks[11], (L, KV_RANK), 0.02),
        "w_q_up": nrm(ks[12], (L, Q_RANK, H_B * (NOPE_B + ROPE_B)), Q_RANK ** -0.5),
        "w_kv_up": nrm(ks[13], (L, KV_RANK, H_B * (NOPE_B + V_B)), KV_RANK ** -0.5),
        "w_out": nrm(ks[14], (L, MIX_DIM, D_MODEL), MIX_DIM ** -0.5),
        "w_router": nrm(ks[15], (L, D_MODEL, N_EXPERTS), D_MODEL ** -0.5),
        "w_gate": nrm(ks[16], (L, N_EXPERTS, D_MODEL, D_FF), D_MODEL ** -0.5),
        "w_up": nrm(ks[17], (L, N_EXPERTS, D_MODEL, D_FF), D_MODEL ** -0.5),
        "w_down": nrm(ks[18], (L, N_EXPERTS, D_FF, D_MODEL), D_FF ** -0.5),
        "norm_final": 1.0 + nrm(ks[19], (D_MODEL,), 0.02),
    }


def reference(x, c, ctx, c_ctx, w_mod, b_mod, norm_attn, norm_ffn, w_in, sink, q_norm, kv_norm,
              w_q_up, w_kv_up, w_out, w_router, w_gate, w_up, w_down, norm_final):
    n_lat = x.shape[1]
    ROWS = n_lat // GRID_W
    rows = jnp.repeat(jnp.arange(ROWS, dtype=jnp.int32), GRID_W)
    cols = jnp.tile(jnp.arange(GRID_W, dtype=jnp.int32), ROWS)
    xc = ctx
    for l in range(DEPTH):
        last = l == DEPTH - 1
        mod = jnp.einsum('bd,de->be', jax.nn.silu(c), w_mod[l]) + b_mod[l]
        mod_c = jnp.einsum('d,de->e', jax.nn.silu(c_ctx), w_mod[l]) + b_mod[l]
        sh_a, sc_a, g_a, sh_f, sc_f, g_f = [m[:, None, :] for m in jnp.split(mod, 6, axis=-1)]
        csh_a, csc_a, cg_a, csh_f, csc_f, cg_f = jnp.split(mod_c, 6, axis=-1)

        h = modulate(rmsnorm(x, norm_attn[l]), sh_a, sc_a)
        hc = modulate(rmsnorm(xc, norm_attn[l]), csh_a, csc_a)
        mix, mix_c = token_mixers(h, hc, rows, cols, w_in[l], sink[l], q_norm[l], kv_norm[l],
                                  w_q_up[l], w_kv_up[l], w_out[l], not last)
        x = x + g_a * mix
        h2 = modulate(rmsnorm(x, norm_ffn[l]), sh_f, sc_f)
        x = x + g_f * ec_moe(h2, w_router[l], w_gate[l], w_up[l], w_down[l])

        if not last:
            xc = xc + cg_a * mix_c
            h2c = modulate(rmsnorm(xc, norm_ffn[l]), csh_f, csc_f)
            xc = xc + cg_f * ec_moe(h2c, w_router[l], w_gate[l], w_up[l], w_down[l])
    return rmsnorm(x, norm_final)
```

```python
import numpy as np
import ml_dtypes
from contextlib import ExitStack
import concourse.bass as bass
import concourse.mybir as mybir
from concourse.bass_utils import run_bass_kernel_spmd

F32 = mybir.dt.float32
BF16 = mybir.dt.bfloat16
I32 = mybir.dt.int32
U32 = mybir.dt.uint32
AF = mybir.ActivationFunctionType
ALU = mybir.AluOpType
AX = mybir.AxisListType

D = 1024
SEQ = 8192
CT = 256
NT = SEQ + CT
NBLK = NT // 128
DEPTH = 2
NE = 16
DFF = 512
EPS = 1e-6
MLA_SCALE = 96 ** -0.5
C_QA, C_QAS, C_KA, C_KAS, C_CQ, C_CKV, C_KR, C_KRS, C_VA = 0, 512, 1024, 1152, 1280, 1664, 1920, 1952, 1984
WIN_COLS = 2112


class Buf:
    __slots__ = ("name", "w", "rd", "dsem", "persist")

    def __init__(self, name, persist=False):
        self.name = name
        self.w = None
        self.rd = []
        self.dsem = None
        self.persist = persist


class Prog:
    ENGS = ("pe", "act", "dve", "pool", "sp")

    def __init__(self, nc, stack):
        self.nc = nc
        self.stack = stack
        self.h = {"pe": nc.tensor, "act": nc.scalar, "dve": nc.vector, "pool": nc.gpsimd, "sp": nc.sync}
        self.esem = {e: stack.enter_context(nc.semaphore("s_" + e)) for e in self.ENGS}
        self.bar = stack.enter_context(nc.semaphore("s_bar"))
        self.nbar = 0
        self.ecnt = {e: 0 for e in self.ENGS}
        self.known = {e: {} for e in self.ENGS}
        self.sems = {("e", e): self.esem[e] for e in self.ENGS}
        self.q = {e: [] for e in self.ENGS}
        self.semcnt = {}
        self.free = {"sp": [], "pool": []}
        self.live = []
        self.ninstr = 0

    def _chan(self, chan, kind):
        if chan.dsem is None:
            chan.dsem = {}
        if kind not in chan.dsem:
            if self.free[kind]:
                chan.dsem[kind] = self.free[kind].pop()
            else:
                idx = len(self.semcnt)
                key = ("d", idx)
                self.sems[key] = self.stack.enter_context(self.nc.semaphore("d%d" % idx))
                self.semcnt[key] = 0
                chan.dsem[kind] = key
            if not getattr(chan, "persist", False):
                self.live.append((chan, kind))
        return chan.dsem[kind]

    def end_phase(self):
        for ch, kind in self.live:
            self.free[kind].append(ch.dsem.pop(kind))
        self.live = []

    def _deps(self, eng, reads, writes, skip_self=False):
        deps = {}

        def add(d):
            if d is None:
                return
            k, v = d
            if skip_self and k == ("e", eng):
                return
            if deps.get(k, 0) < v:
                deps[k] = v
        for b in reads:
            add(b.w)
        for b in writes:
            add(b.w)
            for r in b.rd:
                add(r)
        out = []
        kn = self.known[eng]
        for k, v in deps.items():
            if kn.get(k, 0) >= v:
                continue
            kn[k] = v
            out.append((self.sems[k], v))
        return out

    def _emit(self, eng, waits, fn, sem, inc):
        h = self.h[eng]
        self.ninstr += 1 + len(waits)

        def run():
            for s, v in waits:
                h.wait_ge(s, v)
            fn(h).then_inc(sem, inc)
        self.q[eng].append(run)

    def _record(self, me, reads, writes):
        for b in reads:
            if len(b.rd) > 24:
                b.rd = b.rd[-24:] if False else b.rd
            b.rd.append(me)
        for b in writes:
            b.w = me
            b.rd = []

    def op(self, eng, fn, reads=(), writes=()):
        waits = self._deps(eng, reads, writes, skip_self=(eng == "pe"))
        self.ecnt[eng] += 1
        me = (("e", eng), self.ecnt[eng])
        self._emit(eng, waits, fn, self.esem[eng], 1)
        self._record(me, reads, writes)

    def mm(self, fn, reads=(), writes=()):
        self.op("pe", fn, reads, writes)

    def dma(self, eng, out_ap, in_ap, chan, reads=(), writes=(), **kw):
        key = self._chan(chan, eng)
        waits = self._deps(eng, reads, writes)
        self.semcnt[key] += 16
        me = (key, self.semcnt[key])
        self._emit(eng, waits, I("dma_start", out=out_ap, in_=in_ap, **kw), self.sems[key], 16)
        self._record(me, reads, writes)

    def barrier(self):
        waits = []
        kn = self.known["sp"]
        for e in self.ENGS:
            if e != "sp" and self.ecnt[e] > kn.get(("e", e), 0):
                kn[("e", e)] = self.ecnt[e]
                waits.append((self.esem[e], self.ecnt[e]))
        for k, v in self.semcnt.items():
            if v > kn.get(k, 0):
                kn[k] = v
                waits.append((self.sems[k], v))
        self.nbar += 1
        n = self.nbar
        bar = self.bar
        hs = self.h["sp"]

        def run_sp():
            for s, v in waits:
                hs.wait_ge(s, v)
            hs.sem_inc(bar, 1)
        self.q["sp"].append(run_sp)
        for e in self.ENGS:
            if e == "sp":
                continue
            he = self.h[e]
            self.q[e].append(lambda he=he: he.wait_ge(bar, n))
            for e2 in self.ENGS:
                self.known[e][("e", e2)] = self.ecnt[e2]
            for k, v in self.semcnt.items():
                self.known[e][k] = v

    def replay(self, block):
        q = self.q

        @block.tensor
        def _(e):
            for f in q["pe"]:
                f()

        @block.scalar
        def _(e):
            for f in q["act"]:
                f()

        @block.vector
        def _(e):
            for f in q["dve"]:
                f()

        @block.gpsimd
        def _(e):
            for f in q["pool"]:
                f()

        @block.sync
        def _(e):
            for f in q["sp"]:
                f()


def I(method, *args, **kw):
    def fn(h):
        return getattr(h, method)(*args, **kw)
    return fn


class Ctx:
    pass


_UID = [0]


def _sb(st, nc, name, shape, dt):
    _UID[0] += 1
    return st.enter_context(nc.sbuf_tensor("%s_%d" % (name, _UID[0]), shape, dt))


def _ps(st, nc, name, shape, dt):
    _UID[0] += 1
    return st.enter_context(nc.psum_tensor("%s_%d" % (name, _UID[0]), shape, dt))


def phase_init(C):
    P, nc = C.P, C.nc
    with ExitStack() as st:
        zf = _sb(st, nc, "z_f", [128, 1024], F32)
        zb = _sb(st, nc, "z_b", [128, 1024], BF16)
        b_z = Buf("z")
        P.op("pool", I("memset", zf[:], 0.0), writes=[b_z])
        P.op("pool", I("memset", zb[:], 0.0), writes=[b_z])
        P.dma("sp", C.xs[NT:NT + 128, :], zf[:], b_z, reads=[b_z], writes=[C.b_xs])
        P.dma("sp", C.aff[NT:NT + 128, :], zf[:, 0:NE], b_z, reads=[b_z], writes=[C.b_aff])
        P.dma("sp", C.h2tok[NT:NT + 128, :], zb[:], b_z, reads=[b_z], writes=[C.b_h2tok])
        P.barrier()
        P.end_phase()


def phase_cast(C, l, part):
    P = C.P
    if part == "small":
        for nm in ("w_in", "w_qu", "w_kvu", "w_out"):
            b = C.wbuf[(nm, l)]
            P.dma("pool", C.wb[nm][l], C.din[nm][l], b, writes=[b])
    else:
        for e in range(NE):
            for nm in ("w_gate", "w_up", "w_down"):
                b = C.wbuf[(nm, l, e)]
                P.dma("pool", C.wb[nm][l, e], C.din[nm][l, e], b, writes=[b])


def phase_mod(C, l):
    P, nc = C.P, C.nc
    with ExitStack() as st:
        cs = _sb(st, nc, "m_cs", [128, 8, 2], F32)
        wm = [_sb(st, nc, "m_wm%d" % i, [128, 8, 512], F32) for i in range(2)]
        brow = _sb(st, nc, "m_brow", [2, 6144], F32)
        mrow = _sb(st, nc, "m_mrow", [2, 6144], F32)
        ps_r = [_ps(st, nc, "m_psr%d" % i, [2, 512], F32) for i in range(2)]
        ps_t = _ps(st, nc, "m_pst", [128, 96], F32)
        ps_b = [_ps(st, nc, "m_psb%d" % i, [128, 512], F32) for i in range(2)]
        b_cs, b_brow, b_mrow, b_pst = Buf("cs"), Buf("brow"), Buf("mrow"), Buf("pst")
        b_wm = [Buf("wm0"), Buf("wm1")]
        b_psr = [Buf("psr0"), Buf("psr1")]
        b_psb = [Buf("psb0"), Buf("psb1")]
        P.dma("sp", cs[:, :, 0], C.din["c"].rearrange("(k p) -> p k", p=128), b_cs, writes=[b_cs],
              allow_slow_non_contiguous=True)
        P.dma("sp", cs[:, :, 1], C.din["c_ctx"].rearrange("(k p) -> p k", p=128), b_cs, writes=[b_cs],
              allow_slow_non_contiguous=True)
        P.op("act", I("activation", out=cs[:], in_=cs[:], func=AF.Silu), reads=[b_cs], writes=[b_cs])
        for r in range(2):
            P.dma("sp", brow[r:r + 1, :], C.din["b_mod"][l:l + 1, :], b_brow, writes=[b_brow])
        wsrc = C.din["w_mod"][l].rearrange("(k p) c -> p k c", p=128)
        for cb in range(12):
            i = cb % 2
            P.dma("sp", wm[i][:], wsrc[:, :, cb * 512:(cb + 1) * 512], b_wm[i], writes=[b_wm[i]])
            for k in range(8):
                P.mm(I("matmul", ps_r[i][:], lhsT=cs[:, k, :], rhs=wm[i][:, k, :],
                                                 start=(k == 0), stop=(k == 7)),
                     reads=[b_cs, b_wm[i]], writes=[b_psr[i]])
            P.op("dve", I("tensor_tensor", out=mrow[:, cb * 512:(cb + 1) * 512], in0=ps_r[i][:],
                                                             in1=brow[:, cb * 512:(cb + 1) * 512], op=ALU.add),
                 reads=[b_psr[i], b_brow], writes=[b_mrow])
        P.dma("sp", C.mrow_d[l], mrow[:], b_mrow, reads=[b_mrow], writes=[C.b_mrow_d])
        for j in range(48):
            P.mm(I("transpose", ps_t[:, 2 * j:2 * j + 2], mrow[:, j * 128:(j + 1) * 128], C.ident[0:2, 0:2]),
                 reads=[b_mrow], writes=[b_pst])
        P.op("dve", I("tensor_copy", out=C.modT[:].rearrange("p j r -> p (j r)"), in_=ps_t[:]),
             reads=[b_pst], writes=[C.b_modT])
        P.dma("sp", C.nrm[:, 0, :], C.din["norm_attn"][l].rearrange("(k p) -> p k", p=128), C.b_nrm, writes=[C.b_nrm],
              allow_slow_non_contiguous=True)
        P.dma("sp", C.nrm[:, 1, :], C.din["norm_ffn"][l].rearrange("(k p) -> p k", p=128), C.b_nrm, writes=[C.b_nrm],
              allow_slow_non_contiguous=True)
        for r in range(2):
            for which, j0 in ((0, 8), (1, 32)):
                P.op("dve", I("scalar_tensor_tensor",
                    out=C.gcol[:, which, r, :], in0=C.modT[:, j0:j0 + 8, r], scalar=1.0, in1=C.nrm[:, which, :],
                    op0=ALU.add, op1=ALU.mult),
                    reads=[C.b_modT, C.b_nrm], writes=[C.b_gcol])
        for which, c0 in ((0, 2048), (1, 5120)):
            for r in range(2):
                for hf in range(2):
                    i = (r * 2 + hf) % 2
                    P.mm(I("matmul",
                        ps_b[i][:], lhsT=C.sel2[0:2, r, :], rhs=mrow[:, c0 + hf * 512:c0 + (hf + 1) * 512],
                        start=True, stop=True), reads=[b_mrow], writes=[b_psb[i]])
                    P.op("act", I("activation",
                        out=C.gate_bc[which][:, r, hf * 512:(hf + 1) * 512], in_=ps_b[i][:], func=AF.Copy),
                        reads=[b_psb[i]], writes=[C.b_gate[which]])
        P.barrier()
        P.end_phase()


def norm_mod_T(C, T, x_sb, b_x, nblk, which, r, out_b=None, b_out_b=None):
    P = C.P
    dst_t = T["hT_f"] if out_b is None else out_b
    b_dst = T["b_hT_f"] if out_b is None else b_out_b
    ss, rstd, xn = T["ss"], T["rstd"], T["xn"]
    junk = T["junk"]
    for b in range(nblk):
        P.op("act", I("activation", out=junk[:], in_=x_sb[:, b, :], func=AF.Square,
                                                accum_out=ss[:, b:b + 1]),
             reads=[b_x], writes=[T["b_junk"], T["b_ss"]])
    P.op("act", I("activation", out=rstd[:, 0:nblk], in_=ss[:, 0:nblk], func=AF.Ln, scale=1.0 / D, bias=C.eps_col[:]),
         reads=[T["b_ss"]], writes=[T["b_rstd"]])
    P.op("act", I("activation", out=rstd[:, 0:nblk], in_=rstd[:, 0:nblk], func=AF.Exp, scale=-0.5),
         reads=[T["b_rstd"]], writes=[T["b_rstd"]])
    for b in range(nblk):
        P.op("dve", I("tensor_scalar", out=xn[:, b, :], in0=x_sb[:, b, :], scalar1=rstd[:, b:b + 1],
                                                   scalar2=None, op0=ALU.mult),
             reads=[b_x, T["b_rstd"]], writes=[T["b_xn"]])
    ntok = nblk * 128
    for hf in range(2):
        for cc in range(4):
            c = hf * 4 + cc
            pt = T["ps_tr"][cc]
            for b in range(nblk):
                P.mm(I("transpose", pt[:, b * 128:(b + 1) * 128], xn[:, b, c * 128:(c + 1) * 128],
                                                            C.ident[:]),
                     reads=[T["b_xn"]], writes=[T["b_ps_tr"][cc]])
            eng = "dve" if cc % 2 == 0 else "act"
            if eng == "dve":
                P.op("dve", I("tensor_scalar",
                    out=dst_t[:, c, 0:ntok], in0=pt[:, 0:ntok], scalar1=C.gcol[:, which, r, c:c + 1],
                    scalar2=C.modT[:, (0 if which == 0 else 24) + c, r:r + 1], op0=ALU.mult, op1=ALU.add),
                    reads=[T["b_ps_tr"][cc], C.b_gcol, C.b_modT], writes=[b_dst])
            else:
                P.op("act", I("activation",
                    out=dst_t[:, c, 0:ntok], in_=pt[:, 0:ntok], func=AF.Identity,
                    scale=C.gcol[:, which, r, c:c + 1], bias=C.modT[:, (0 if which == 0 else 24) + c, r:r + 1]),
                    reads=[T["b_ps_tr"][cc], C.b_gcol, C.b_modT], writes=[b_dst])
    if out_b is None:
        P.op("pool", I("tensor_copy", out=T["hT_b"][:, :, 0:ntok], in_=T["hT_f"][:, :, 0:ntok]),
             reads=[T["b_hT_f"]], writes=[T["b_hT_b"]])


def alloc_norm_tiles(C, st, pfx, with_out=True):
    nc = C.nc
    T = {}
    T["ss"] = _sb(st, nc, pfx + "ss", [128, 4], F32)
    T["rstd"] = _sb(st, nc, pfx + "rstd", [128, 4], F32)
    T["xn"] = _sb(st, nc, pfx + "xn", [128, 4, 1024], F32)
    T["junk"] = _sb(st, nc, pfx + "junk", [128, 1024], BF16)
    if with_out:
        T["hT_f"] = _sb(st, nc, pfx + "hTf", [128, 8, 512], F32)
        T["hT_b"] = _sb(st, nc, pfx + "hTb", [128, 8, 512], BF16)
    T["ps_tr"] = [_ps(st, nc, pfx + "pstr%d" % i, [128, 512], F32) for i in range(4)]
    for k in ("ss", "rstd", "xn", "junk", "hT_f", "hT_b"):
        T["b_" + k] = Buf(pfx + k)
    T["b_ps_tr"] = [Buf(pfx + "pstr%d" % i) for i in range(4)]
    return T


def x_rows(C, l, t0, n):
    if l == 0:
        if t0 < CT:
            return C.din["ctx"][t0:t0 + n, :]
        return C.din["x"][t0 - CT:t0 - CT + n, :]
    return C.xs[t0:t0 + n, :]


def tiles_of(l):
    tl = [(0, 2, 1)]
    for i in range(SEQ // 512):
        tl.append((CT + i * 512, 4, 0))
    return tl


def phase_proj(C, l):
    P, nc = C.P, C.nc
    with ExitStack() as st:
        T = alloc_norm_tiles(C, st, "p_", with_out=False)
        hTb = [_sb(st, nc, "p_hTb%d" % i, [128, 8, 512], BF16) for i in range(2)]
        b_hTb = [Buf("p_hTb0"), Buf("p_hTb1")]
        w_in = _sb(st, nc, "p_win", [128, 8, WIN_COLS], BF16)
        w_qu = _sb(st, nc, "p_wqu", [128, 3, 1024], BF16)
        w_kvu = _sb(st, nc, "p_wkvu", [128, 2, 1024], BF16)
        qn_col = _sb(st, nc, "p_qn", [128, 3], F32)
        kvn_col = _sb(st, nc, "p_kvn", [128, 2], F32)
        xt = [_sb(st, nc, "p_x%d" % i, [128, 4, 1024], F32) for i in range(2)]
        rope = [_sb(st, nc, "p_rope%d" % i, [128, 4, 512], F32) for i in range(2)]
        ev = [_sb(st, nc, "p_ev%d" % i, [128, 512], F32) for i in range(4)]
        ob = [_sb(st, nc, "p_ob%d" % i, [128, 512], BF16) for i in range(6)]
        cq_f = _sb(st, nc, "p_cqf", [128, 5, 512], F32)
        cq_sq = _sb(st, nc, "p_cqsq", [128, 5, 512], BF16)
        cq_n = _sb(st, nc, "p_cqn", [128, 5, 512], BF16)
        rq = _sb(st, nc, "p_rq", [128, 2, 512], F32)
        ps = [_ps(st, nc, "p_ps%d" % i, [128, 512], F32) for i in range(4)]
        b_w = Buf("p_w")
        b_xt = [Buf("p_x0"), Buf("p_x1")]
        b_rope = [Buf("p_rope0"), Buf("p_rope1")]
        b_ev = [Buf("p_ev%d" % i) for i in range(4)]
        b_ob = [Buf("p_ob%d" % i) for i in range(6)]
        b_cqf, b_cqsq, b_cqn, b_rq = Buf("cqf"), Buf("cqsq"), Buf("cqn"), Buf("rq")
        b_ps = [Buf("p_ps%d" % i) for i in range(4)]
        cnt = {"ps": 0, "ev": 0, "ob": 0}

        def nxt(k, n):
            i = cnt[k] % n
            cnt[k] += 1
            return i

        P.dma("sp", w_in[:], C.wb["w_in"][l].rearrange("(k p) c -> p k c", p=128), b_w,
              reads=[C.wbuf[("w_in", l)]], writes=[b_w])
        P.dma("sp", w_qu[:], C.wb["w_qu"][l].rearrange("(k p) c -> p k c", p=128), b_w,
              reads=[C.wbuf[("w_qu", l)]], writes=[b_w])
        P.dma("sp", w_kvu[:], C.wb["w_kvu"][l].rearrange("(k p) c -> p k c", p=128), b_w,
              reads=[C.wbuf[("w_kvu", l)]], writes=[b_w])
        P.dma("sp", qn_col[:], C.din["q_norm"][l].rearrange("(k p) -> p k", p=128), b_w, writes=[b_w],
              allow_slow_non_contiguous=True)
        P.dma("sp", kvn_col[:], C.din["kv_norm"][l].rearrange("(k p) -> p k", p=128), b_w, writes=[b_w],
              allow_slow_non_contiguous=True)

        tl = tiles_of(l)

        def loadx(ti):
            t0, nb, r = tl[ti]
            i = ti % 2
            P.dma("sp", xt[i][:, 0:nb, :], x_rows(C, l, t0, nb * 128).rearrange("(b p) d -> p b d", p=128), b_xt[i],
                  reads=([C.b_xs] if l > 0 else []), writes=[b_xt[i]])

        def loadr(ti):
            t0, nb, r = tl[ti]
            i = ti % 2
            P.dma("sp", rope[i][:, :, 0:nb * 128], C.din["rope"][:, :, t0:t0 + nb * 128], b_rope[i], writes=[b_rope[i]])

        def front(ti):
            t0, nb, r = tl[ti]
            i = ti % 2
            norm_mod_T(C, T, xt[i], b_xt[i], nb, 0, r, out_b=hTb[i], b_out_b=b_hTb[i])

        loadx(0)
        loadx(1)
        loadr(0)
        front(0)
        for ti, (t0, nb, r) in enumerate(tl):
            if ti + 1 < len(tl):
                loadr(ti + 1)
                front(ti + 1)
            if ti + 2 < len(tl):
                loadx(ti + 2)
            i = ti % 2
            n = nb * 128
            hT = hTb[i]
            b_h = b_hTb[i]
            rp = rope[i]
            b_rp = b_rope[i]

            def proj(c0, m, pi):
                for k in range(8):
                    P.mm(I("matmul", ps[pi][0:m, 0:n], lhsT=w_in[:, k, c0:c0 + m], rhs=hT[:, k, 0:n],
                                                 start=(k == 0), stop=(k == 7)),
                         reads=[b_w, b_h], writes=[b_ps[pi]])

            def roped(c0, c0s, m, tab, store):
                pa, pb = nxt("ps", 4), nxt("ps", 4)
                proj(c0, m, pa)
                proj(c0s, m, pb)
                ea, eb, o = nxt("ev", 4), nxt("ev", 4), nxt("ob", 6)
                P.op("dve", I("tensor_tensor", out=ev[ea][0:m, 0:n], in0=ps[pa][0:m, 0:n],
                                                      in1=rp[0:m, tab, 0:n], op=ALU.mult),
                     reads=[b_ps[pa], b_rp], writes=[b_ev[ea]])
                P.op("dve", I("tensor_tensor", out=ev[eb][0:m, 0:n], in0=ps[pb][0:m, 0:n],
                                                      in1=rp[0:m, tab + 1, 0:n], op=ALU.mult),
                     reads=[b_ps[pb], b_rp], writes=[b_ev[eb]])
                P.op("pool", I("tensor_tensor", out=ob[o][0:m, 0:n], in0=ev[ea][0:m, 0:n],
                                                       in1=ev[eb][0:m, 0:n], op=ALU.add),
                     reads=[b_ev[ea], b_ev[eb]], writes=[b_ob[o]])
                store(ob[o], b_ob[o])

            for c in range(4):
                def st_q(o, bo, c=c):
                    for hh in range(2):
                        hq = 2 * c + hh
                        g, j = hq // 4, hq % 4
                        dst = C.QaT[g, :, t0 // 128:t0 // 128 + nb, j, :]
                        P.dma("sp", dst, o[hh * 64:(hh + 1) * 64, 0:n].rearrange("p (b q) -> p b q", q=128),
                              bo, reads=[bo], writes=[C.b_QaT])
                roped(C_QA + c * 128, C_QAS + c * 128, 128, 0, st_q)
            def st_k(o, bo):
                P.dma("sp", C.KaT[:, t0:t0 + n], o[:, 0:n], bo, reads=[bo], writes=[C.b_KaT])
            roped(C_KA, C_KAS, 128, 0, st_k)
            def st_kr(o, bo):
                for hh in range(8):
                    P.dma("sp", C.KbT[hh, 64:96, t0:t0 + n], o[0:32, 0:n], bo, reads=[bo], writes=[C.b_KbT])
            roped(C_KR, C_KRS, 32, 2, st_kr)
            pv = nxt("ps", 4)
            for b in range(nb):
                for k in range(8):
                    P.mm(I("matmul", ps[pv][:, b * 128:(b + 1) * 128], lhsT=hT[:, k, b * 128:(b + 1) * 128],
                                                      rhs=w_in[:, k, C_VA:C_VA + 128], start=(k == 0), stop=(k == 7)),
                         reads=[b_w, b_h], writes=[b_ps[pv]])
            o = nxt("ob", 6)
            P.op("act", I("activation", out=ob[o][:, 0:n], in_=ps[pv][:, 0:n], func=AF.Copy),
                 reads=[b_ps[pv]], writes=[b_ob[o]])
            P.dma("sp", C.Va[t0:t0 + n, :].rearrange("(b p) d -> p b d", p=128),
                  ob[o][:, 0:n].rearrange("p (b d) -> p b d", d=128), b_ob[o], reads=[b_ob[o]], writes=[C.b_Va])
            for c in range(5):
                pi = nxt("ps", 4)
                proj(C_CQ + c * 128, 128, pi)
                P.op("act", I("activation", out=cq_f[:, c, 0:n], in_=ps[pi][:, 0:n], func=AF.Copy),
                     reads=[b_ps[pi]], writes=[b_cqf])
                P.op("act", I("activation", out=cq_sq[:, c, 0:n], in_=ps[pi][:, 0:n], func=AF.Square),
                     reads=[b_ps[pi]], writes=[b_cqsq])
            for (grp, c0, nch, ones) in ((0, 0, 3, C.ones_q), (1, 3, 2, C.ones_kv)):
                pi = nxt("ps", 4)
                for c in range(nch):
                    P.mm(I("matmul", ps[pi][:, 0:n], lhsT=ones[:], rhs=cq_sq[:, c0 + c, 0:n],
                                                 start=(c == 0), stop=(c == nch - 1)),
                         reads=[b_cqsq], writes=[b_ps[pi]])
                P.op("act", I("activation", out=rq[:, grp, 0:n], in_=ps[pi][:, 0:n], func=AF.Ln,
                                                                   bias=C.eps_col[:]),
                     reads=[b_ps[pi]], writes=[b_rq])
                P.op("act", I("activation", out=rq[:, grp, 0:n], in_=rq[:, grp, 0:n], func=AF.Exp, scale=-0.5),
                     reads=[b_rq], writes=[b_rq])
                for c in range(nch):
                    col = qn_col[:, c:c + 1] if grp == 0 else kvn_col[:, c:c + 1]
                    P.op("dve", I("scalar_tensor_tensor",
                        out=cq_n[:, c0 + c, 0:n], in0=cq_f[:, c0 + c, 0:n], scalar=col, in1=rq[:, grp, 0:n],
                        op0=ALU.mult, op1=ALU.mult), reads=[b_cqf, b_rq, b_w], writes=[b_cqn])
            for c in range(4):
                pi = nxt("ps", 4)
                for k in range(3):
                    P.mm(I("matmul", ps[pi][:, 0:n], lhsT=w_qu[:, k, c * 128:(c + 1) * 128],
                                                      rhs=cq_n[:, k, 0:n], start=(k == 0), stop=(k == 2)),
                         reads=[b_w, b_cqn], writes=[b_ps[pi]])
                o = nxt("ob", 6)
                P.op("act", I("activation", out=ob[o][:, 0:n], in_=ps[pi][:, 0:n], func=AF.Copy),
                     reads=[b_ps[pi]], writes=[b_ob[o]])
                for hh in range(2):
                    P.dma("sp", C.QbT[2 * c + hh, 0:64, t0:t0 + n], ob[o][hh * 64:(hh + 1) * 64, 0:n], b_ob[o],
                          reads=[b_ob[o]], writes=[C.b_QbT])
            for c in range(2):
                pa, pb = nxt("ps", 4), nxt("ps", 4)
                for (pi, cc0) in ((pa, 512), (pb, 768)):
                    for k in range(3):
                        P.mm(I("matmul", ps[pi][:, 0:n], lhsT=w_qu[:, k, cc0 + c * 128:cc0 + (c + 1) * 128],
                                                                     rhs=cq_n[:, k, 0:n], start=(k == 0), stop=(k == 2)),
                             reads=[b_w, b_cqn], writes=[b_ps[pi]])
                ea, eb, o = nxt("ev", 4), nxt("ev", 4), nxt("ob", 6)
                P.op("dve", I("tensor_tensor", out=ev[ea][:, 0:n], in0=ps[pa][:, 0:n], in1=rp[:, 2, 0:n], op=ALU.mult),
                     reads=[b_ps[pa], b_rp], writes=[b_ev[ea]])
                P.op("dve", I("tensor_tensor", out=ev[eb][:, 0:n], in0=ps[pb][:, 0:n], in1=rp[:, 3, 0:n], op=ALU.mult),
                     reads=[b_ps[pb], b_rp], writes=[b_ev[eb]])
                P.op("pool", I("tensor_tensor", out=ob[o][:, 0:n], in0=ev[ea][:, 0:n], in1=ev[eb][:, 0:n], op=ALU.add),
                     reads=[b_ev[ea], b_ev[eb]], writes=[b_ob[o]])
                for hh in range(4):
                    P.dma("sp", C.QbT[4 * c + hh, 64:96, t0:t0 + n], ob[o][hh * 32:(hh + 1) * 32, 0:n], b_ob[o],
                          reads=[b_ob[o]], writes=[C.b_QbT])
            for c in range(4):
                pi = nxt("ps", 4)
                for k in range(2):
                    P.mm(I("matmul", ps[pi][:, 0:n], lhsT=w_kvu[:, k, c * 128:(c + 1) * 128],
                                                      rhs=cq_n[:, 3 + k, 0:n], start=(k == 0), stop=(k == 1)),
                         reads=[b_w, b_cqn], writes=[b_ps[pi]])
                o = nxt("ob", 6)
                P.op("act", I("activation", out=ob[o][:, 0:n], in_=ps[pi][:, 0:n], func=AF.Copy),
                     reads=[b_ps[pi]], writes=[b_ob[o]])
                for hh in range(2):
                    P.dma("sp", C.KbT[2 * c + hh, 0:64, t0:t0 + n], ob[o][hh * 64:(hh + 1) * 64, 0:n], b_ob[o],
                          reads=[b_ob[o]], writes=[C.b_KbT])
            for b in range(nb):
                pi = nxt("ps", 4)
                for k in range(2):
                    P.mm(I("matmul", ps[pi][:, :], lhsT=cq_n[:, 3 + k, b * 128:(b + 1) * 128],
                                                      rhs=w_kvu[:, k, 512:1024], start=(k == 0), stop=(k == 1)),
                         reads=[b_w, b_cqn], writes=[b_ps[pi]])
                o = nxt("ob", 6)
                P.op("dve", I("tensor_copy", out=ob[o][:, :], in_=ps[pi][:, :]),
                     reads=[b_ps[pi]], writes=[b_ob[o]])
                P.dma("sp", C.Vb[t0 + b * 128:t0 + (b + 1) * 128, :], ob[o][:, :], b_ob[o], reads=[b_ob[o]], writes=[C.b_Vb])
        P.barrier()
        P.end_phase()


def phase_attn(C, l):
    P, nc = C.P, C.nc
    LA = 2
    with ExitStack() as st:
        kT = [_sb(st, nc, "a_kT%d" % i, [128, NT], BF16) for i in range(2)]
        vv = [_sb(st, nc, "a_v%d" % i, [128, NBLK, 128], BF16) for i in range(2)]
        qq = [_sb(st, nc, "a_q%d" % i, [128, 512], BF16) for i in range(3)]
        pt = [_sb(st, nc, "a_pt%d" % i, [128, 512], BF16) for i in range(4)]
        rc = [_sb(st, nc, "a_rc%d" % i, [128, 512], F32) for i in range(2)]
        bcs = _sb(st, nc, "a_bcs", [128, 512], F32)
        obf = [_sb(st, nc, "a_obf%d" % i, [128, 512], BF16) for i in range(2)]
        mask = _sb(st, nc, "a_mask", [128, 2, 4, 128], BF16)
        maskf = _sb(st, nc, "a_maskf", [128, 256], F32)
        es8 = _sb(st, nc, "a_es8", [128, 8], F32)
        esink = _sb(st, nc, "a_esink", [128, 8, 128], F32)
        den = _sb(st, nc, "a_den", [128, 512], F32)
        b_den = Buf("den")
        ones_r = _sb(st, nc, "a_ones", [128, 64], F32)
        ps_s = [_ps(st, nc, "a_pss%d" % i, [128, 512], F32) for i in range(4)]
        ps_o = [_ps(st, nc, "a_pso%d" % i, [128, 512], F32) for i in range(2)]
        ps_b = _ps(st, nc, "a_psb", [128, 512], F32)
        b_kT = [Buf("kT0"), Buf("kT1")]
        b_v = [Buf("v0"), Buf("v1")]
        b_q = [Buf("q%d" % i) for i in range(3)]
        b_pt = [Buf("pt%d" % i) for i in range(4)]
        b_bcs, b_cst, b_psb = Buf("bcs"), Buf("acst"), Buf("psb")
        b_rc = [Buf("rc0"), Buf("rc1")]
        b_obf = [Buf("obf0"), Buf("obf1")]
        b_pss = [Buf("pss%d" % i) for i in range(4)]
        b_pso = [Buf("pso0"), Buf("pso1")]
        cnt = {"ob": 0}

        P.dma("sp", maskf[:], C.din["cst"][:, 128:384], b_cst, writes=[b_cst])
        for m in range(2):
            P.op("dve", I("tensor_copy", out=mask[:, m, :, :],
                          in_=maskf[:, m * 128:(m + 1) * 128].rearrange("p (o q) -> p o q", o=1).to_broadcast([128, 4, 128])),
                 reads=[b_cst], writes=[b_cst])
        P.dma("sp", es8[64:65, :], C.din["sink"][l:l + 1, :], b_cst, writes=[b_cst])
        P.op("act", I("activation", out=es8[64:65, :], in_=es8[64:65, :], func=AF.Exp), reads=[b_cst], writes=[b_cst])
        P.op("dve", I("tensor_copy", out=esink[64:65, :, :],
                      in_=es8[64:65, :].rearrange("p (e o) -> p e o", o=1).to_broadcast([1, 8, 128])),
             reads=[b_cst], writes=[b_cst])
        P.op("dve", I("memset", ones_r[:], 1.0), writes=[b_cst])
        for i in range(2):
            P.op("pool", I("memset", vv[i][:, :, 64:128], 0.0), writes=[b_v[i]])
            P.op("pool", I("memset", vv[i][:, :, 64:65], 1.0), writes=[b_v[i]])
        for i in range(2):
            P.op("pool", I("memset", kT[i][64:128, :], 0.0), writes=[b_kT[i]])
        for i in range(3):
            P.op("pool", I("memset", qq[i][64:128, :], 0.0), writes=[b_q[i]])
        phase_cast(C, l, "experts")
        if l + 1 < DEPTH:
            phase_cast(C, l + 1, "small")

        pending = []

        def finish(po, n, dst_fn, sink_g=None):
            flush()
            ri = cnt["ob"] % 2
            if sink_g is None:
                P.op("dve", I("reciprocal", out=rc[ri][64:65, 0:n], in_=ps_o[po][64:65, 0:n]), reads=[b_pso[po]], writes=[b_rc[ri]])
            else:
                P.op("dve", I("tensor_tensor", out=den[64:65, :], in0=ps_o[po][64:65, :],
                              in1=esink[64:65, sink_g * 4:(sink_g + 1) * 4, :].rearrange("p e q -> p (e q)"), op=ALU.add),
                     reads=[b_pso[po], b_cst], writes=[b_den])
                P.op("dve", I("reciprocal", out=rc[ri][64:65, 0:n], in_=den[64:65, 0:n]), reads=[b_den], writes=[b_rc[ri]])
            pending.append((po, n, dst_fn, ri))

        def flush():
            while pending:
                finish_b(*pending.pop(0))

        def finish_b(po, n, dst_fn, ri):
            P.mm(I("matmul", ps_b[0:64, 0:n], lhsT=ones_r[64:65, 0:64], rhs=rc[ri][64:65, 0:n], start=True, stop=True),
                 reads=[b_rc[ri], b_cst], writes=[b_psb])
            P.op("dve", I("tensor_copy", out=bcs[0:64, 0:n], in_=ps_b[0:64, 0:n]), reads=[b_psb], writes=[b_bcs])
            o = cnt["ob"] % 2
            cnt["ob"] += 1
            P.op("dve", I("tensor_tensor", out=obf[o][0:64, 0:n], in0=ps_o[po][0:64, 0:n], in1=bcs[0:64, 0:n], op=ALU.mult),
                 reads=[b_pso[po], b_bcs], writes=[b_obf[o]])
            dst_fn(obf[o], b_obf[o])

        srcs = [("w", g) for g in range(2)] + [("m", hh) for hh in range(8)]
        groups = []
        for si, (kind, idx) in enumerate(srcs):
            if kind == "w":
                for qb in range(0 if l == 0 else 2, NBLK):
                    if qb < 2:
                        kbs = [(0, None), (1, None)]
                    else:
                        kbs = [(0, None), (1, None)]
                        if qb - 1 >= 2:
                            kbs.append((qb - 1, 0))
                        kbs.append((qb, None))
                        if qb + 1 < NBLK:
                            kbs.append((qb + 1, 1))
                    groups.append(dict(si=si, kind="w", idx=idx, qb=qb, n=512, kbs=kbs))
            else:
                for (t0, nb, r) in tiles_of(l):
                    if l > 0 and r == 1:
                        continue
                    nkb = 2 if r == 1 else NBLK
                    groups.append(dict(si=si, kind="m", idx=idx, t0=t0, n=nb * 128, kbs=[(kb, None) for kb in range(nkb)]))

        def load_src(si):
            kind, idx = srcs[si]
            i = si % 2
            if kind == "w":
                P.dma("sp", kT[i][0:64, :], C.KaT[idx * 64:(idx + 1) * 64, :], b_kT[i], reads=[C.b_KaT], writes=[b_kT[i]])
                P.dma("sp", vv[i][:, :, 0:64], C.Va.rearrange("(b p) c -> p b c", p=128)[:, :, idx * 64:(idx + 1) * 64], b_v[i],
                      reads=[C.b_Va], writes=[b_v[i]])
            else:
                P.dma("sp", kT[i][0:96, :], C.KbT[idx], b_kT[i], reads=[C.b_KbT], writes=[b_kT[i]])
                P.dma("sp", vv[i][:, :, 0:64], C.Vb.rearrange("(b p) c -> p b c", p=128)[:, :, idx * 64:(idx + 1) * 64], b_v[i],
                      reads=[C.b_Vb], writes=[b_v[i]])

        def load_q(gi):
            G = groups[gi]
            qi = gi % 3
            if G["kind"] == "w":
                P.dma("sp", qq[qi][0:64, :], C.QaT[G["idx"], :, G["qb"], :, :].rearrange("d j q -> d (j q)"), b_q[qi],
                      reads=[C.b_QaT], writes=[b_q[qi]])
            else:
                P.dma("sp", qq[qi][0:96, 0:G["n"]], C.QbT[G["idx"], :, G["t0"]:G["t0"] + G["n"]], b_q[qi],
                      reads=[C.b_QbT], writes=[b_q[qi]])

        steps = []
        for gi, G in enumerate(groups):
            for ki, (kb, mk) in enumerate(G["kbs"]):
                steps.append((gi, ki, kb, mk))

        def stage_A(sidx):
            gi, ki, kb, mk = steps[sidx]
            G = groups[gi]
            if ki == 0:
                if gi == 0:
                    load_src(0)
                    load_q(0)
                if gi + 1 < len(groups):
                    load_q(gi + 1)
            i = G["si"] % 2
            qi = gi % 3
            kd = 128 if G["kind"] == "w" else 96
            n = G["n"]
            s = sidx % 4
            P.mm(I("matmul", ps_s[s][:, 0:n], lhsT=kT[i][0:kd, kb * 128:(kb + 1) * 128], rhs=qq[qi][0:kd, 0:n],
                   start=True, stop=True), reads=[b_kT[i], b_q[qi]], writes=[b_pss[s]])
            p = sidx % 4
            scale = 0.125 if G["kind"] == "w" else MLA_SCALE
            P.op("act", I("activation", out=pt[p][:, 0:n], in_=ps_s[s][:, 0:n], func=AF.Exp, scale=scale),
                 reads=[b_pss[s]], writes=[b_pt[p]])
            if mk is not None:
                P.op("dve", I("tensor_tensor", out=pt[p][:, :], in0=pt[p][:, :],
                              in1=mask[:, mk, :, :].rearrange("p j q -> p (j q)"), op=ALU.mult),
                     reads=[b_pt[p], b_cst], writes=[b_pt[p]])

        def stage_C(sidx):
            gi, ki, kb, mk = steps[sidx]
            G = groups[gi]
            i = G["si"] % 2
            n = G["n"]
            po = gi % 2
            p = sidx % 4
            if ki == 0 and (gi == 0 or groups[gi - 1]["si"] != G["si"]) and G["si"] + 1 < len(srcs):
                load_src(G["si"] + 1)
            first = (ki == 0)
            lastk = (ki == len(G["kbs"]) - 1)
            P.mm(I("matmul", ps_o[po][:, 0:n], lhsT=vv[i][:, kb, :], rhs=pt[p][:, 0:n], start=first, stop=lastk),
                 reads=[b_v[i], b_pt[p]], writes=[b_pso[po]])
            if lastk:
                if G["kind"] == "w":
                    g, qb = G["idx"], G["qb"]

                    def dst_w(o, bo):
                        dst = C.OT[g * 256:(g + 1) * 256, qb * 128:(qb + 1) * 128].rearrange("(j d) q -> d j q", d=64)
                        P.dma("sp", dst, o[0:64, :].rearrange("p (j q) -> p j q", q=128), bo, reads=[bo], writes=[C.b_OT])
                    finish(po, 512, dst_w, sink_g=g)
                else:
                    hh, t0 = G["idx"], G["t0"]

                    def dst_m(o, bo):
                        P.dma("sp", C.OT[512 + hh * 64:512 + (hh + 1) * 64, t0:t0 + n], o[0:64, 0:n], bo, reads=[bo], writes=[C.b_OT])
                    finish(po, n, dst_m)

        ns = len(steps)
        for i in range(ns + LA):
            if i < ns:
                stage_A(i)
            if i - LA >= 0:
                stage_C(i - LA)
        flush()
        P.barrier()
        P.end_phase()


def phase_oproj(C, l):
    P, nc = C.P, C.nc
    last = (l == DEPTH - 1)
    with ExitStack() as st:
        T = alloc_norm_tiles(C, st, "o_")
        w_out = _sb(st, nc, "o_wout", [128, 8, 1024], BF16)
        w_r = _sb(st, nc, "o_wr", [128, 8, NE], F32)
        ot = [_sb(st, nc, "o_ot%d" % i, [128, 8, 512], BF16) for i in range(2)]
        xt = [_sb(st, nc, "o_x%d" % i, [128, 4, 1024], F32) for i in range(2)]
        tmp = [_sb(st, nc, "o_tmp%d" % i, [128, 512], F32) for i in range(2)]
        lg = _sb(st, nc, "o_lg", [128, 4, NE], F32)
        mx = _sb(st, nc, "o_mx", [128, 4], F32)
        sm = _sb(st, nc, "o_sm", [128, 4], F32)
        af = [_sb(st, nc, "o_af%d" % i, [128, 4, NE], F32) for i in range(2)]
        ps = [_ps(st, nc, "o_ps%d" % i, [128, 512], F32) for i in range(2)]
        ps_r = _ps(st, nc, "o_psr", [128, 4, NE], F32)
        b_w = Buf("o_w")
        b_ot = [Buf("o_ot0"), Buf("o_ot1")]
        b_xt = [Buf("o_x0"), Buf("o_x1")]
        b_tmp = [Buf("o_tmp0"), Buf("o_tmp1")]
        b_lg, b_mx, b_sm, b_psr = Buf("lg"), Buf("mx"), Buf("sm"), Buf("psr")
        b_af = [Buf("af0"), Buf("af1")]
        b_ps = [Buf("o_ps0"), Buf("o_ps1")]
        cnt = {"ps": 0, "tmp": 0}

        def nxt(k, n):
            i = cnt[k] % n
            cnt[k] += 1
            return i
        P.dma("sp", w_out[:], C.wb["w_out"][l].rearrange("(k p) c -> p k c", p=128), b_w,
              reads=[C.wbuf[("w_out", l)]], writes=[b_w])
        P.dma("sp", w_r[:], C.din["w_router"][l].rearrange("(k p) e -> p k e", p=128), b_w, writes=[b_w])
        rowF = _sb(st, nc, "o_rowF", [128, 2, 2, 1024], F32)
        nfb = _sb(st, nc, "o_nfb", [128, 1024], F32)
        h2t = [_sb(st, nc, "o_h2t%d" % i, [128, 4, 1024], BF16) for i in range(2)]
        b_rowF = Buf("rowF")
        b_h2t = [Buf("h2t0"), Buf("h2t1")]
        P.dma("sp", nfb[:], C.din["norm_ffn"][l:l + 1, :].partition_broadcast(128), b_rowF, writes=[b_rowF])
        for r in range(2):
            P.dma("sp", rowF[:, r, 0, :], C.mrow_d[l, r:r + 1, 4096:5120].partition_broadcast(128), b_rowF,
                  reads=[C.b_mrow_d], writes=[b_rowF])
            P.dma("sp", rowF[:, r, 1, :], C.mrow_d[l, r:r + 1, 3072:4096].partition_broadcast(128), b_rowF,
                  reads=[C.b_mrow_d], writes=[b_rowF])
            P.op("dve", I("scalar_tensor_tensor", out=rowF[:, r, 0, :], in0=rowF[:, r, 0, :], scalar=1.0, in1=nfb[:],
                          op0=ALU.add, op1=ALU.mult), reads=[b_rowF], writes=[b_rowF])
        tl = [t for t in tiles_of(l) if not (last and t[2] == 1)]

        def load(ti):
            t0, nb, r = tl[ti]
            i = ti % 2
            n = nb * 128
            P.dma("sp", ot[i][:, :, 0:n], C.OT.rearrange("(k p) t -> p k t", p=128)[:, :, t0:t0 + n], b_ot[i],
                  reads=[C.b_OT], writes=[b_ot[i]])
            P.dma("sp", xt[i][:, 0:nb, :], x_rows(C, l, t0, n).rearrange("(b p) d -> p b d", p=128), b_xt[i],
                  reads=([C.b_xs] if l > 0 else []), writes=[b_xt[i]])
        load(0)
        for ti, (t0, nb, r) in enumerate(tl):
            if ti + 1 < len(tl):
                load(ti + 1)
            i = ti % 2
            n = nb * 128
            for b in range(nb):
                for hf in range(2):
                    pi = nxt("ps", 2)
                    for k in range(8):
                        P.mm(I("matmul", ps[pi][:, :], lhsT=ot[i][:, k, b * 128:(b + 1) * 128],
                               rhs=w_out[:, k, hf * 512:(hf + 1) * 512], start=(k == 0), stop=(k == 7)),
                             reads=[b_ot[i], b_w], writes=[b_ps[pi]])
                    tp = nxt("tmp", 2)
                    P.op("dve", I("tensor_tensor", out=tmp[tp][:, :], in0=ps[pi][:, :],
                                  in1=C.gate_bc[0][:, r, hf * 512:(hf + 1) * 512], op=ALU.mult),
                         reads=[b_ps[pi], C.b_gate[0]], writes=[b_tmp[tp]])
                    P.op("pool", I("tensor_tensor", out=xt[i][:, b, hf * 512:(hf + 1) * 512], in0=tmp[tp][:, :],
                                   in1=xt[i][:, b, hf * 512:(hf + 1) * 512], op=ALU.add),
                         reads=[b_tmp[tp], b_xt[i]], writes=[b_xt[i]])
            P.dma("sp", C.xs[t0:t0 + n, :].rearrange("(b p) d -> p b d", p=128), xt[i][:, 0:nb, :], b_xt[i],
                  reads=[b_xt[i]], writes=[C.b_xs])
            norm_mod_T(C, T, xt[i], b_xt[i], nb, 1, r)
            for b in range(nb):
                P.op("dve", I("tensor_tensor", out=T["xn"][:, b, :], in0=T["xn"][:, b, :], in1=rowF[:, r, 0, :], op=ALU.mult),
                     reads=[T["b_xn"], b_rowF], writes=[T["b_xn"]])
                P.op("pool" if b % 2 == 0 else "dve", I("tensor_tensor", out=h2t[i][:, b, :], in0=T["xn"][:, b, :], in1=rowF[:, r, 1, :], op=ALU.add),
                     reads=[T["b_xn"], b_rowF], writes=[b_h2t[i]])
            P.dma("sp", C.h2tok[t0:t0 + n, :].rearrange("(b p) d -> p b d", p=128), h2t[i][:, 0:nb, :], b_h2t[i],
                  reads=[b_h2t[i]], writes=[C.b_h2tok])
            for b in range(nb):
                for k in range(8):
                    P.mm(I("matmul", ps_r[:, b, :], lhsT=T["hT_f"][:, k, b * 128:(b + 1) * 128], rhs=w_r[:, k, :],
                           start=(k == 0), stop=(k == 7)), reads=[T["b_hT_f"], b_w], writes=[b_psr])
            a = ti % 2
            P.op("dve", I("tensor_reduce", out=mx[:, 0:nb], in_=ps_r[:, 0:nb, :], axis=AX.X, op=ALU.max),
                 reads=[b_psr], writes=[b_mx])
            P.op("dve", I("tensor_tensor", out=lg[:, 0:nb, :], in0=ps_r[:, 0:nb, :],
                          in1=mx[:, 0:nb].rearrange("p (b o) -> p b o", o=1).to_broadcast([128, nb, NE]), op=ALU.subtract),
                 reads=[b_psr, b_mx], writes=[b_lg])
            P.op("act", I("activation", out=lg[:, 0:nb, :], in_=lg[:, 0:nb, :], func=AF.Exp), reads=[b_lg], writes=[b_lg])
            P.op("dve", I("tensor_reduce", out=sm[:, 0:nb], in_=lg[:, 0:nb, :], axis=AX.X, op=ALU.add),
                 reads=[b_lg], writes=[b_sm])
            P.op("dve", I("reciprocal", out=sm[:, 0:nb], in_=sm[:, 0:nb]), reads=[b_sm], writes=[b_sm])
            P.op("dve", I("tensor_tensor", out=af[a][:, 0:nb, :], in0=lg[:, 0:nb, :],
                          in1=sm[:, 0:nb].rearrange("p (b o) -> p b o", o=1).to_broadcast([128, nb, NE]), op=ALU.mult),
                 reads=[b_lg, b_sm], writes=[b_af[a]])
            P.dma("sp", C.aff[t0:t0 + n, :].rearrange("(b p) e -> p b e", p=128), af[a][:, 0:nb, :], b_af[a],
                  reads=[b_af[a]], writes=[C.b_aff])
        P.barrier()
        P.end_phase()


def phase_thr(C, l):
    P, nc = C.P, C.nc
    last = (l == DEPTH - 1)
    with ExitStack() as st:
        araw = _sb(st, nc, "t_araw", [128, 64, NE], F32)
        craw = _sb(st, nc, "t_craw", [128, 2, NE], F32)
        A = _sb(st, nc, "t_A", [128, 32, 64], F32)
        cmpt = _sb(st, nc, "t_cmp", [128, 32, 64], BF16)
        cntp = _sb(st, nc, "t_cnt", [128, 32], F32)
        ones = _sb(st, nc, "t_ones", [128, 128], F32)
        lo = _sb(st, nc, "t_lo", [128, 32], F32)
        hi = _sb(st, nc, "t_hi", [128, 32], F32)
        mid = _sb(st, nc, "t_mid", [128, 32], F32)
        cap = _sb(st, nc, "t_cap", [128, 32], F32)
        ge = _sb(st, nc, "t_ge", [128, 32], U32)
        lt = _sb(st, nc, "t_lt", [128, 32], U32)
        ps = _ps(st, nc, "t_ps", [128, 32], F32)
        b_araw, b_A, b_cmp, b_cnt, b_c = Buf("araw"), Buf("A"), Buf("cmp"), Buf("cnt"), Buf("tc")
        b_lo, b_hi, b_mid, b_ge, b_lt, b_ps = Buf("lo"), Buf("hi"), Buf("mid"), Buf("ge"), Buf("lt"), Buf("tps")
        P.dma("sp", araw[:], C.aff[CT:NT, :].rearrange("(p j) e -> p j e", p=128), b_araw, reads=[C.b_aff], writes=[b_araw])
        P.op("pool", I("memset", A[:], 0.0), writes=[b_A])
        P.op("dve", I("tensor_copy", out=A[:, 0:16, :], in_=araw[:].rearrange("p j e -> p e j")), reads=[b_araw], writes=[b_A])
        if not last:
            P.dma("sp", craw[:], C.aff[0:CT, :].rearrange("(p j) e -> p j e", p=128), b_araw, reads=[C.b_aff], writes=[b_araw])
            P.op("dve", I("tensor_copy", out=A[:, 16:32, 0:2], in_=craw[:].rearrange("p j e -> p e j")), reads=[b_araw], writes=[b_A])
        P.op("dve", I("memset", ones[:], 1.0), writes=[b_c])
        P.op("dve", I("memset", cap[:, 0:16], float(2 * SEQ // NE)), writes=[b_c])
        P.op("dve", I("memset", cap[:, 16:32], float(2 * CT // NE)), writes=[b_c])
        P.op("dve", I("memset", lo[:], 0.0), writes=[b_lo])
        P.op("dve", I("memset", hi[:], 1.0), writes=[b_hi])
        for it in range(31):
            P.op("dve", I("tensor_tensor", out=mid[:], in0=lo[:], in1=hi[:], op=ALU.add), reads=[b_lo, b_hi], writes=[b_mid])
            P.op("dve", I("tensor_scalar", out=mid[:], in0=mid[:], scalar1=0.5, scalar2=None, op0=ALU.mult),
                 reads=[b_mid], writes=[b_mid])
            P.op("dve", I("tensor_tensor", out=cmpt[:], in0=A[:],
                          in1=mid[:].rearrange("p (e o) -> p e o", o=1).to_broadcast([128, 32, 64]), op=ALU.is_gt),
                 reads=[b_A, b_mid], writes=[b_cmp])
            P.op("dve", I("tensor_reduce", out=cntp[:], in_=cmpt[:], axis=AX.X, op=ALU.add), reads=[b_cmp], writes=[b_cnt])
            P.mm(I("matmul", ps[:], lhsT=ones[:], rhs=cntp[:], start=True, stop=True), reads=[b_cnt, b_c], writes=[b_ps])
            P.op("dve", I("tensor_tensor", out=ge[:], in0=ps[:], in1=cap[:], op=ALU.is_ge), reads=[b_ps, b_c], writes=[b_ge])
            P.op("dve", I("tensor_tensor", out=lt[:], in0=ps[:], in1=cap[:], op=ALU.is_lt), reads=[b_ps, b_c], writes=[b_lt])
            P.op("dve", I("copy_predicated", lo[:], ge[:], mid[:]), reads=[b_ge, b_mid], writes=[b_lo])
            P.op("dve", I("copy_predicated", hi[:], lt[:], mid[:]), reads=[b_lt, b_mid], writes=[b_hi])
        P.op("dve", I("tensor_copy", out=C.thr[:], in_=lo[:]), reads=[b_lo], writes=[C.b_thr])
        P.barrier()
        P.end_phase()


def phase_moe(C, l):
    P, nc = C.P, C.nc
    last = (l == DEPTH - 1)
    with ExitStack() as st:
        yacc = _sb(st, nc, "e_yacc", [128, 8, 1024], F32)
        wg = [_sb(st, nc, "e_wg%d" % i, [128, 8, DFF], BF16) for i in range(2)]
        wu = [_sb(st, nc, "e_wu%d" % i, [128, 8, DFF], BF16) for i in range(2)]
        wd = [_sb(st, nc, "e_wd%d" % i, [128, 4, 1024], BF16) for i in range(2)]
        hT = [_sb(st, nc, "e_hT%d" % i, [128, 8, 512], BF16) for i in range(2)]
        afr = [_sb(st, nc, "e_af%d" % i, [128, 4, NE], F32) for i in range(2)]
        gt = [_sb(st, nc, "e_gt%d" % i, [128, 4, NE], F32) for i in range(2)]
        sA = [_sb(st, nc, "e_sA%d" % i, [128, 512], F32) for i in range(2)]
        act = [_sb(st, nc, "e_act%d" % i, [128, 4, 512], BF16) for i in range(2)]
        xt = _sb(st, nc, "e_x", [128, 4, 1024], F32)
        nfb = _sb(st, nc, "e_nfb", [128, 1024], F32)
        ss = _sb(st, nc, "e_ss", [128, 4], F32)
        junk = _sb(st, nc, "e_junk", [128, 1024], BF16)
        psA = [_ps(st, nc, "e_psA%d" % i, [128, 512], F32) for i in range(2)]
        psU = [_ps(st, nc, "e_psU%d" % i, [128, 512], F32) for i in range(2)]
        psY = [_ps(st, nc, "e_psY%d" % i, [128, 512], F32) for i in range(3)]
        b_yacc = [Buf("yacc%d" % i) for i in range(8)]
        b_w = [Buf("ew0"), Buf("ew1")]
        b_hT = [Buf("ehT0"), Buf("ehT1")]
        b_afr = [Buf("eaf0"), Buf("eaf1")]
        b_gt = [Buf("egt0"), Buf("egt1")]
        b_sA = [Buf("esA0"), Buf("esA1")]
        b_act = [Buf("eact0"), Buf("eact1")]
        b_x, b_nfb, b_ss, b_junk = Buf("ex"), Buf("enfb"), Buf("ess"), Buf("ejunk")
        b_psA = [Buf("psA0"), Buf("psA1")]
        b_psU = [Buf("psU0"), Buf("psU1")]
        b_psY = [Buf("psY%d" % i) for i in range(3)]
        cnt = {"A": 0, "U": 0, "Y": 0, "sA": 0, "act": 0, "w": 0}

        def nxt(k, n):
            i = cnt[k] % n
            cnt[k] += 1
            return i
        if last:
            P.dma("sp", nfb[:], C.din["norm_final"].rearrange("(o d) -> o d", o=1).partition_broadcast(128), b_nfb, writes=[b_nfb])
        tl = [t for t in tiles_of(l) if not (last and t[2] == 1)]
        sts = []
        if not last:
            sts.append(tl[0:2])
            rest = tl[2:]
        else:
            rest = tl
        for i in range(0, len(rest), 2):
            sts.append(rest[i:i + 2])
        for stl in sts:
            blk0 = []
            nb_tot = 0
            for si, (t0, nb, r) in enumerate(stl):
                n = nb * 128
                P.dma("sp", hT[si][:, :, 0:n], C.h2T.rearrange("(k p) t -> p k t", p=128)[:, :, t0:t0 + n], b_hT[si],
                      reads=[C.b_h2T], writes=[b_hT[si]])
                P.dma("sp", afr[si][:, 0:nb, :], C.aff[t0:t0 + n, :].rearrange("(b p) e -> p b e", p=128), b_afr[si],
                      reads=[C.b_aff], writes=[b_afr[si]])
                thr = C.thr[:, 16 * r:16 * r + 16].rearrange("p (o e) -> p o e", o=1).to_broadcast([128, nb, NE])
                P.op("dve", I("tensor_tensor", out=gt[si][:, 0:nb, :], in0=afr[si][:, 0:nb, :], in1=thr, op=ALU.is_gt),
                     reads=[b_afr[si], C.b_thr], writes=[b_gt[si]])
                P.op("dve", I("tensor_tensor", out=gt[si][:, 0:nb, :], in0=gt[si][:, 0:nb, :], in1=afr[si][:, 0:nb, :], op=ALU.mult),
                     reads=[b_gt[si], b_afr[si]], writes=[b_gt[si]])
                blk0.append(nb_tot)
                nb_tot += nb
            for e in range(NE):
                wi = nxt("w", 2)
                P.dma("sp", wg[wi][:], C.wb["w_gate"][l, e].rearrange("(k p) f -> p k f", p=128), b_w[wi],
                      reads=[C.wbuf[("w_gate", l, e)]], writes=[b_w[wi]])
                P.dma("sp", wu[wi][:], C.wb["w_up"][l, e].rearrange("(k p) f -> p k f", p=128), b_w[wi],
                      reads=[C.wbuf[("w_up", l, e)]], writes=[b_w[wi]])
                P.dma("sp", wd[wi][:], C.wb["w_down"][l, e].rearrange("(k p) d -> p k d", p=128), b_w[wi],
                      reads=[C.wbuf[("w_down", l, e)]], writes=[b_w[wi]])
                for si, (t0, nb, r) in enumerate(stl):
                    n = nb * 128
                    ai = nxt("act", 2)
                    for fc in range(4):
                        pa, pu = nxt("A", 2), nxt("U", 2)
                        for k in range(8):
                            P.mm(I("matmul", psA[pa][:, 0:n], lhsT=wg[wi][:, k, fc * 128:(fc + 1) * 128], rhs=hT[si][:, k, 0:n],
                                   start=(k == 0), stop=(k == 7)), reads=[b_w[wi], b_hT[si]], writes=[b_psA[pa]])
                        for k in range(8):
                            P.mm(I("matmul", psU[pu][:, 0:n], lhsT=wu[wi][:, k, fc * 128:(fc + 1) * 128], rhs=hT[si][:, k, 0:n],
                                   start=(k == 0), stop=(k == 7)), reads=[b_w[wi], b_hT[si]], writes=[b_psU[pu]])
                        sa = nxt("sA", 2)
                        P.op("act", I("activation", out=sA[sa][:, 0:n], in_=psA[pa][:, 0:n], func=AF.Silu),
                             reads=[b_psA[pa]], writes=[b_sA[sa]])
                        P.op("dve", I("tensor_tensor", out=act[ai][:, fc, 0:n], in0=sA[sa][:, 0:n], in1=psU[pu][:, 0:n], op=ALU.mult),
                             reads=[b_sA[sa], b_psU[pu]], writes=[b_act[ai]])
                    for b in range(nb):
                        blk = blk0[si] + b
                        for hf in range(2):
                            py = nxt("Y", 3)
                            for fc in range(4):
                                P.mm(I("matmul", psY[py][:, :], lhsT=act[ai][:, fc, b * 128:(b + 1) * 128],
                                       rhs=wd[wi][:, fc, hf * 512:(hf + 1) * 512], start=(fc == 0), stop=(fc == 3)),
                                     reads=[b_act[ai], b_w[wi]], writes=[b_psY[py]])
                            ya = yacc[:, blk, hf * 512:(hf + 1) * 512]
                            if e == 0:
                                P.op("dve", I("tensor_scalar", out=ya, in0=psY[py][:, :], scalar1=gt[si][:, b, e:e + 1],
                                              scalar2=None, op0=ALU.mult),
                                     reads=[b_psY[py], b_gt[si]], writes=[b_yacc[blk]])
                            else:
                                P.op("dve", I("scalar_tensor_tensor", out=ya, in0=psY[py][:, :], scalar=gt[si][:, b, e:e + 1],
                                              in1=ya, op0=ALU.mult, op1=ALU.add),
                                     reads=[b_psY[py], b_gt[si], b_yacc[blk]], writes=[b_yacc[blk]])
            for si, (t0, nb, r) in enumerate(stl):
                n = nb * 128
                P.dma("sp", xt[:, 0:nb, :], C.xs[t0:t0 + n, :].rearrange("(b p) d -> p b d", p=128), b_x,
                      reads=[C.b_xs], writes=[b_x])
                for b in range(nb):
                    blk = blk0[si] + b
                    P.op("dve", I("tensor_tensor", out=yacc[:, blk, :], in0=yacc[:, blk, :], in1=C.gate_bc[1][:, r, :], op=ALU.mult),
                         reads=[b_yacc[blk], C.b_gate[1]], writes=[b_yacc[blk]])
                    P.op("pool", I("tensor_tensor", out=xt[:, b, :], in0=xt[:, b, :], in1=yacc[:, blk, :], op=ALU.add),
                         reads=[b_x, b_yacc[blk]], writes=[b_x])
                if not last:
                    P.dma("sp", C.xs[t0:t0 + n, :].rearrange("(b p) d -> p b d", p=128), xt[:, 0:nb, :], b_x,
                          reads=[b_x], writes=[C.b_xs])
                else:
                    for b in range(nb):
                        P.op("act", I("activation", out=junk[:], in_=xt[:, b, :], func=AF.Square, accum_out=ss[:, b:b + 1]),
                             reads=[b_x], writes=[b_junk, b_ss])
                    P.op("act", I("activation", out=ss[:, 0:nb], in_=ss[:, 0:nb], func=AF.Ln, scale=1.0 / D, bias=C.eps_col[:]),
                         reads=[b_ss], writes=[b_ss])
                    P.op("act", I("activation", out=ss[:, 0:nb], in_=ss[:, 0:nb], func=AF.Exp, scale=-0.5), reads=[b_ss], writes=[b_ss])
                    for b in range(nb):
                        P.op("dve", I("scalar_tensor_tensor", out=xt[:, b, :], in0=xt[:, b, :], scalar=ss[:, b:b + 1], in1=nfb[:],
                                      op0=ALU.mult, op1=ALU.mult), reads=[b_x, b_ss, b_nfb], writes=[b_x])
                    P.dma("sp", C.out[t0 - CT:t0 - CT + n, :].rearrange("(b p) d -> p b d", p=128), xt[:, 0:nb, :], b_x,
                          reads=[b_x], writes=[C.b_out])
        P.barrier()
        P.end_phase()


def phase_idx(C, l):
    P, nc = C.P, C.nc
    last = (l == DEPTH - 1)
    nsets = 1 if last else 2
    with ExitStack() as st:
        c2 = _sb(st, nc, "i_c2", [128, 2048], F32)
        affp = _sb(st, nc, "i_affp", [128, NBLK, NE], F32)
        maskb = _sb(st, nc, "i_mask", [128, NBLK, NE], BF16)
        trib = _sb(st, nc, "i_trib", [128, 128], BF16)
        onesb = _sb(st, nc, "i_onesb", [128, 1], BF16)
        cum_sb = _sb(st, nc, "i_cum", [128, NBLK, NE], F32)
        tot = _sb(st, nc, "i_tot", [128, NE], F32)
        cumB = _sb(st, nc, "i_cumB", [128, NE], F32)
        offB = _sb(st, nc, "i_offB", [128, NE], F32)
        cumS = _sb(st, nc, "i_cumS", [128, 2, NE], F32)
        offS = _sb(st, nc, "i_offS", [128, 2, NE], F32)
        cumx = _sb(st, nc, "i_cumx", [128, NE, 132], F32)
        t1 = _sb(st, nc, "i_t1", [128, 1024], F32)
        oh = [_sb(st, nc, "i_oh%d" % i, [128, 1024], F32) for i in range(2)]
        junk = _sb(st, nc, "i_junk", [128, 128], F32)
        rr = _sb(st, nc, "i_rr", [128, 8], F32)
        idf = _sb(st, nc, "i_idf", [128, 8], F32)
        gfull = _sb(st, nc, "i_gfull", [128, 8, 132], F32)
        b_c2, b_affp, b_mask, b_cum, b_tot, b_cb, b_cs, b_cumx = (Buf("c2"), Buf("affp"), Buf("mask"), Buf("cum"), Buf("tot"),
                                                                 Buf("cb"), Buf("cs"), Buf("cumx"))
        b_t1, b_junk, b_rr, b_idf, b_gsb = Buf("t1"), Buf("junk"), Buf("rr"), Buf("idf"), Buf("gsb")
        b_oh = [Buf("oh0"), Buf("oh1")]
        P.dma("sp", c2[:], C.din["cst2"], b_c2, writes=[b_c2])
        P.dma("sp", affp[:], C.aff[0:NT, :].rearrange("(b p) e -> p b e", p=128), b_affp, reads=[C.b_aff], writes=[b_affp])
        P.op("dve", I("tensor_copy", out=trib[:], in_=c2[:, 0:128]), reads=[b_c2], writes=[b_c2])
        P.op("dve", I("memset", onesb[:], 1.0), writes=[b_c2])
        P.op("dve", I("tensor_tensor", out=maskb[:, 2:NBLK, :], in0=affp[:, 2:NBLK, :],
                      in1=C.thr[:, 0:16].rearrange("p (o e) -> p o e", o=1).to_broadcast([128, NBLK - 2, NE]), op=ALU.is_gt),
             reads=[b_affp, C.b_thr], writes=[b_mask])
        if last:
            P.op("dve", I("memset", maskb[:, 0:2, :], 0.0), writes=[b_mask])
        else:
            P.op("dve", I("tensor_tensor", out=maskb[:, 0:2, :], in0=affp[:, 0:2, :],
                          in1=C.thr[:, 16:32].rearrange("p (o e) -> p o e", o=1).to_broadcast([128, 2, NE]), op=ALU.is_gt),
                 reads=[b_affp, C.b_thr], writes=[b_mask])
        import os
        STOP = int(os.environ.get("IDX_STOP", "9"))
        if STOP <= 1:
            P.barrier()
            P.end_phase()
            return
        with ExitStack() as st2:
            ps_cum = [_ps(st2, nc, "i_pscum%d" % i, [128, 352], F32) for i in range(3)]
            ps_tot = _ps(st2, nc, "i_pstot", [128, NE], F32)
            ps_cb = _ps(st2, nc, "i_pscb", [128, NE], F32)
            b_pscum = [Buf("pscum%d" % i) for i in range(3)]
            b_pstot, b_pscb = Buf("pstot"), Buf("pscb")
            mflat = maskb[:].rearrange("p b e -> p (b e)")
            cflat = cum_sb[:].rearrange("p b e -> p (b e)")
            for j in range(3):
                P.mm(I("matmul", ps_cum[j][:, :], lhsT=trib[:], rhs=mflat[:, j * 352:(j + 1) * 352], start=True, stop=True),
                     reads=[b_mask, b_c2], writes=[b_pscum[j]])
                P.op("act", I("activation", out=cflat[:, j * 352:(j + 1) * 352], in_=ps_cum[j][:, :], func=AF.Copy),
                     reads=[b_pscum[j]], writes=[b_cum])
            for e in range(NE if STOP > 2 else 0):
                P.mm(I("matmul", ps_tot[0:NBLK, e:e + 1], lhsT=maskb[:, :, e], rhs=onesb[:], start=True, stop=True),
                     reads=[b_mask, b_c2], writes=[b_pstot])
            P.op("dve", I("tensor_copy", out=tot[0:NBLK, :], in_=ps_tot[0:NBLK, :]), reads=[b_pstot], writes=[b_tot])
            P.mm(I("matmul", ps_cb[0:NBLK, :], lhsT=c2[0:NBLK, 128:128 + NBLK], rhs=tot[0:NBLK, :], start=True, stop=True),
                 reads=[b_tot, b_c2], writes=[b_pscb])
            P.op("dve", I("tensor_copy", out=cumB[0:NBLK, :], in_=ps_cb[0:NBLK, :]), reads=[b_pscb], writes=[b_cb])
            P.op("dve", I("tensor_tensor", out=offB[0:NBLK, :], in0=cumB[0:NBLK, :], in1=tot[0:NBLK, :], op=ALU.subtract),
                 reads=[b_cb, b_tot], writes=[b_cb])
            for sset in range(2):
                col = c2[0:NBLK, 256 + sset:257 + sset]
                P.op("dve", I("tensor_scalar", out=cumS[0:NBLK, sset, :], in0=cumB[0:NBLK, :], scalar1=col, scalar2=None, op0=ALU.mult),
                     reads=[b_cb, b_c2], writes=[b_cs])
                P.op("dve", I("tensor_scalar", out=offS[0:NBLK, sset, :], in0=offB[0:NBLK, :], scalar1=col, scalar2=None, op0=ALU.mult),
                     reads=[b_cb, b_c2], writes=[b_cs])
            P.barrier()
        if STOP <= 3:
            P.end_phase()
            return
        with ExitStack() as st3:
            ps_tr = [_ps(st3, nc, "i_pstr%d" % i, [128, 512], F32) for i in range(2)]
            ps_g = [_ps(st3, nc, "i_psg%d" % i, [128, 512], F32) for i in range(2)]
            b_pstr = [Buf("pstr0"), Buf("pstr1")]
            b_psg = [Buf("psg0"), Buf("psg1")]
            for e in range(NE):
                bank = (e // 4) % 2
                reg = ps_tr[bank][0:NBLK, (e % 4) * 128:(e % 4 + 1) * 128]
                P.mm(I("transpose", reg, cum_sb[:, :, e], C.ident[:]), reads=[b_cum], writes=[b_pstr[bank]])
                P.op("dve", I("tensor_scalar", out=cumx[0:NBLK, e, 0:128], in0=reg, scalar1=offB[0:NBLK, e:e + 1], scalar2=None,
                              op0=ALU.add), reads=[b_pstr[bank], b_cb], writes=[b_cumx])
                P.op("pool", I("tensor_copy", out=cumx[0:NBLK, e, 128:130], in_=c2[0:NBLK, 258:260]), reads=[b_c2], writes=[b_cumx])
            it = 0
            for sset in range(nsets if STOP > 4 else 0):
                nsb = 8 if sset == 0 else 1
                ns = nsb * 128
                for e in range(NE):
                    o = it % 2
                    it += 1
                    iota_s = c2[0:NBLK, 1024:1024 + ns]
                    P.op("dve", I("tensor_scalar", out=t1[0:NBLK, 0:ns], in0=iota_s, scalar1=offS[0:NBLK, sset, e:e + 1], scalar2=None,
                                  op0=ALU.is_ge), reads=[b_c2, b_cs], writes=[b_t1])
                    P.op("dve", I("tensor_scalar", out=oh[o][0:NBLK, 0:ns], in0=iota_s, scalar1=cumS[0:NBLK, sset, e:e + 1], scalar2=None,
                                  op0=ALU.is_lt), reads=[b_c2, b_cs], writes=[b_oh[o]])
                    P.op("dve", I("tensor_tensor", out=oh[o][0:NBLK, 0:ns], in0=oh[o][0:NBLK, 0:ns], in1=t1[0:NBLK, 0:ns], op=ALU.mult),
                         reads=[b_oh[o], b_t1], writes=[b_oh[o]])
                    if STOP <= 5:
                        continue
                    for sb in range(nsb):
                        gi = sb % 2
                        P.mm(I("matmul", ps_g[gi][:, 0:130], lhsT=oh[o][0:NBLK, sb * 128:(sb + 1) * 128], rhs=cumx[0:NBLK, e, 0:130],
                               start=True, stop=True), reads=[b_oh[o], b_cumx], writes=[b_psg[gi]])
                        if STOP <= 6:
                            continue
                        P.op("act", I("activation", out=gfull[:, sb, 0:130], in_=ps_g[gi][:, 0:130], func=AF.Copy),
                             reads=[b_psg[gi]], writes=[b_gsb])
                        P.op("dve", I("tensor_tensor", out=junk[:], in0=gfull[:, sb, 0:128],
                                      in1=c2[:, 260 + sb:261 + sb].to_broadcast([128, 128]), op=ALU.is_le),
                             reads=[b_gsb, b_c2], writes=[b_junk])
                        P.op("dve", I("tensor_reduce", out=rr[:, sb:sb + 1], in_=junk[:], axis=AX.X, op=ALU.add),
                             reads=[b_junk], writes=[b_rr])
                    if STOP <= 7:
                        continue
                    P.op("dve", I("tensor_tensor", out=idf[:, 0:nsb], in0=rr[:, 0:nsb], in1=gfull[:, 0:nsb, 128], op=ALU.add),
                         reads=[b_rr, b_gsb], writes=[b_idf])
                    P.op("dve", I("tensor_scalar", out=idf[:, 0:nsb], in0=idf[:, 0:nsb], scalar1=c2[:, 268:269], scalar2=None,
                                  op0=ALU.subtract), reads=[b_idf, b_c2], writes=[b_idf])
                    P.op("dve", I("tensor_tensor", out=idf[:, 0:nsb], in0=idf[:, 0:nsb], in1=gfull[:, 0:nsb, 129], op=ALU.mult),
                         reads=[b_idf, b_gsb], writes=[b_idf])
                    P.op("dve", I("tensor_scalar", out=idf[:, 0:nsb], in0=idf[:, 0:nsb], scalar1=c2[:, 268:269], scalar2=None,
                                  op0=ALU.add), reads=[b_idf, b_c2], writes=[b_idf])
                    P.op("dve", I("tensor_copy", out=C.idxu[:, sset, e, 0:nsb], in_=idf[:, 0:nsb]), reads=[b_idf], writes=[C.b_idxu])
            P.barrier()
        P.end_phase()


def _indirect(C, kind, sb_ap, dram_ap, idx_ap, chan, reads, writes):
    P = C.P
    key = P._chan(chan, "pool")
    waits = P._deps("pool", reads, writes)
    P.semcnt[key] += 16
    me = (key, P.semcnt[key])
    if kind == "g":
        fn = I("indirect_dma_start", out=sb_ap, out_offset=None, in_=dram_ap,
               in_offset=bass.IndirectOffsetOnAxis(ap=idx_ap, axis=0))
    else:
        fn = I("indirect_dma_start", out=dram_ap, out_offset=bass.IndirectOffsetOnAxis(ap=idx_ap, axis=0),
               in_=sb_ap, in_offset=None, compute_op=ALU.add)
    P._emit("pool", waits, fn, P.sems[key], 16)
    P._record(me, reads, writes)


def phase_moe2(C, l):
    P, nc = C.P, C.nc
    last = (l == DEPTH - 1)
    nsets = 1 if last else 2
    with ExitStack() as st:
        wg = [_sb(st, nc, "e_wg%d" % i, [128, 8, DFF], BF16) for i in range(2)]
        wu = [_sb(st, nc, "e_wu%d" % i, [128, 8, DFF], BF16) for i in range(2)]
        wd = [_sb(st, nc, "e_wd%d" % i, [128, 4, 1024], BF16) for i in range(2)]
        xg = [_sb(st, nc, "e_xg%d" % i, [128, 1024], BF16) for i in range(8)]
        ag = [_sb(st, nc, "e_ag%d" % i, [128, NE], F32) for i in range(8)]
        xT = [_sb(st, nc, "e_xT%d" % i, [128, 8, 512], BF16) for i in range(2)]
        sA = [_sb(st, nc, "e_sA%d" % i, [128, 512], F32) for i in range(2)]
        act = [_sb(st, nc, "e_act%d" % i, [128, 4, 512], BF16) for i in range(2)]
        ysb = [_sb(st, nc, "e_y%d" % i, [128, 1024], F32) for i in range(3)]
        identb = _sb(st, nc, "e_identb", [128, 128], BF16)
        ps_t = [_ps(st, nc, "e_pst%d" % i, [128, 8, 128], BF16) for i in range(2)]
        psA = [_ps(st, nc, "e_psA%d" % i, [128, 512], F32) for i in range(2)]
        psU = [_ps(st, nc, "e_psU%d" % i, [128, 512], F32) for i in range(2)]
        psY = [_ps(st, nc, "e_psY%d" % i, [128, 512], F32) for i in range(2)]
        b_w = [Buf("ew0"), Buf("ew1")]
        b_xg = [Buf("xg%d" % i) for i in range(8)]
        b_ag = [Buf("ag%d" % i) for i in range(8)]
        b_xT = [Buf("xT0"), Buf("xT1")]
        b_sA = [Buf("sA0"), Buf("sA1")]
        b_act = [Buf("act0"), Buf("act1")]
        b_y = [Buf("y%d" % i) for i in range(3)]
        b_pst = [Buf("pst0"), Buf("pst1")]
        b_psA, b_psU = [Buf("psA0"), Buf("psA1")], [Buf("psU0"), Buf("psU1")]
        b_psY = [Buf("psY%d" % i) for i in range(2)]
        b_ib = Buf("identb")
        cnt = {"xg": 0, "ag": 0, "t": 0, "xT": 0, "sA": 0, "act": 0, "Y": 0, "y": 0, "w": 0, "A": 0}

        def nxt(k, n):
            i = cnt[k] % n
            cnt[k] += 1
            return i
        P.op("dve", I("tensor_copy", out=identb[:], in_=C.ident[:]), reads=[C.b_const], writes=[b_ib])
        jobs = [(sset, e) for sset in range(nsets) for e in range(NE)]

        def load_w(ji):
            sset, e = jobs[ji]
            wi = ji % 2
            P.dma("sp", wg[wi][:], C.wb["w_gate"][l, e].rearrange("(k p) f -> p k f", p=128), b_w[wi],
                  reads=[C.wbuf[("w_gate", l, e)]], writes=[b_w[wi]])
            P.dma("sp", wu[wi][:], C.wb["w_up"][l, e].rearrange("(k p) f -> p k f", p=128), b_w[wi],
                  reads=[C.wbuf[("w_up", l, e)]], writes=[b_w[wi]])
            P.dma("sp", wd[wi][:], C.wb["w_down"][l, e].rearrange("(k p) d -> p k d", p=128), b_w[wi],
                  reads=[C.wbuf[("w_down", l, e)]], writes=[b_w[wi]])
        tiles = []
        for ji, (sset, e) in enumerate(jobs):
            nsb = 8 if sset == 0 else 1
            for tb in range(0, nsb, 4):
                tiles.append((ji, sset, e, list(range(tb, min(tb + 4, nsb)))))

        def gathers(t):
            ji, sset, e, sbs = tiles[t]
            for j, sb in enumerate(sbs):
                k = (t % 2) * 4 + j
                idx_ap = C.idxu[:, sset, e, sb:sb + 1]
                _indirect(C, "g", xg[k][:], C.h2tok, idx_ap, b_xg[k], [C.b_idxu, C.b_h2tok], [b_xg[k]])
                _indirect(C, "g", ag[k][:], C.aff, idx_ap, b_ag[k], [C.b_idxu, C.b_aff], [b_ag[k]])
        load_w(0)
        gathers(0)
        last_ji = -1
        for t, (ji, sset, e, sbs) in enumerate(tiles):
            if ji != last_ji:
                last_ji = ji
                if ji + 1 < len(jobs):
                    load_w(ji + 1)
            if t + 1 < len(tiles):
                gathers(t + 1)
            wi = ji % 2
            n = len(sbs) * 128
            xi = t % 2
            for j, sb in enumerate(sbs):
                k = (t % 2) * 4 + j
                ti = nxt("t", 2)
                for c in range(8):
                    P.mm(I("transpose", ps_t[ti][:, c, :], xg[k][:, c * 128:(c + 1) * 128], identb[:]),
                         reads=[b_xg[k], b_ib], writes=[b_pst[ti]])
                if j % 2 == 0:
                    P.op("act", I("activation", out=xT[xi][:, :, j * 128:(j + 1) * 128], in_=ps_t[ti][:], func=AF.Copy),
                         reads=[b_pst[ti]], writes=[b_xT[xi]])
                else:
                    P.op("dve", I("tensor_copy", out=xT[xi][:, :, j * 128:(j + 1) * 128], in_=ps_t[ti][:]),
                         reads=[b_pst[ti]], writes=[b_xT[xi]])
            a_i = nxt("act", 2)
            for fc in range(4):
                pa = nxt("A", 2)
                for k in range(8):
                    P.mm(I("matmul", psA[pa][:, 0:n], lhsT=wg[wi][:, k, fc * 128:(fc + 1) * 128], rhs=xT[xi][:, k, 0:n],
                           start=(k == 0), stop=(k == 7)), reads=[b_w[wi], b_xT[xi]], writes=[b_psA[pa]])
                for k in range(8):
                    P.mm(I("matmul", psU[pa][:, 0:n], lhsT=wu[wi][:, k, fc * 128:(fc + 1) * 128], rhs=xT[xi][:, k, 0:n],
                           start=(k == 0), stop=(k == 7)), reads=[b_w[wi], b_xT[xi]], writes=[b_psU[pa]])
                sa = nxt("sA", 2)
                P.op("act", I("activation", out=sA[sa][:, 0:n], in_=psA[pa][:, 0:n], func=AF.Silu),
                     reads=[b_psA[pa]], writes=[b_sA[sa]])
                P.op("dve", I("tensor_tensor", out=act[a_i][:, fc, 0:n], in0=sA[sa][:, 0:n], in1=psU[pa][:, 0:n], op=ALU.mult),
                     reads=[b_sA[sa], b_psU[pa]], writes=[b_act[a_i]])
            for j, sb in enumerate(sbs):
                k = (t % 2) * 4 + j
                yi = nxt("y", 3)
                for hf in range(2):
                    py = nxt("Y", 2)
                    for fc in range(4):
                        P.mm(I("matmul", psY[py][:, :], lhsT=act[a_i][:, fc, j * 128:(j + 1) * 128],
                               rhs=wd[wi][:, fc, hf * 512:(hf + 1) * 512], start=(fc == 0), stop=(fc == 3)),
                             reads=[b_act[a_i], b_w[wi]], writes=[b_psY[py]])
                    P.op("dve", I("scalar_tensor_tensor", out=ysb[yi][:, hf * 512:(hf + 1) * 512], in0=psY[py][:, :],
                                  scalar=ag[k][:, e:e + 1], in1=C.gate_bc[1][:, sset, hf * 512:(hf + 1) * 512],
                                  op0=ALU.mult, op1=ALU.mult),
                         reads=[b_psY[py], b_ag[k], C.b_gate[1]], writes=[b_y[yi]])
                _indirect(C, "s", ysb[yi][:], C.xs, C.idxu[:, sset, e, sb:sb + 1], b_y[yi], [C.b_idxu, b_y[yi]], [C.b_xs])
        P.barrier()
        P.end_phase()


def phase_final(C):
    P, nc = C.P, C.nc
    with ExitStack() as st:
        xt = [_sb(st, nc, "f_x%d" % i, [128, 4, 1024], F32) for i in range(2)]
        nfb = _sb(st, nc, "f_nfb", [128, 1024], F32)
        ss = [_sb(st, nc, "f_ss%d" % i, [128, 4], F32) for i in range(2)]
        junk = _sb(st, nc, "f_junk", [128, 1024], BF16)
        b_x = [Buf("fx0"), Buf("fx1")]
        b_ss = [Buf("fss0"), Buf("fss1")]
        b_nfb, b_junk = Buf("fnfb"), Buf("fjunk")
        P.dma("sp", nfb[:], C.din["norm_final"].rearrange("(o d) -> o d", o=1).partition_broadcast(128), b_nfb, writes=[b_nfb])
        tl = [t for t in tiles_of(1) if t[2] == 0]

        def load(ti):
            t0, nb, r = tl[ti]
            P.dma("sp", xt[ti % 2][:], C.xs[t0:t0 + 512, :].rearrange("(b p) d -> p b d", p=128), b_x[ti % 2],
                  reads=[C.b_xs], writes=[b_x[ti % 2]])
        load(0)
        for ti, (t0, nb, r) in enumerate(tl):
            if ti + 1 < len(tl):
                load(ti + 1)
            i = ti % 2
            for b in range(4):
                P.op("act", I("activation", out=junk[:], in_=xt[i][:, b, :], func=AF.Square, accum_out=ss[i][:, b:b + 1]),
                     reads=[b_x[i]], writes=[b_junk, b_ss[i]])
            P.op("act", I("activation", out=ss[i][:], in_=ss[i][:], func=AF.Ln, scale=1.0 / D, bias=C.eps_col[:]),
                 reads=[b_ss[i]], writes=[b_ss[i]])
            P.op("act", I("activation", out=ss[i][:], in_=ss[i][:], func=AF.Exp, scale=-0.5), reads=[b_ss[i]], writes=[b_ss[i]])
            for b in range(4):
                eng = "dve" if b % 2 == 0 else "pool"
                if eng == "dve":
                    P.op("dve", I("scalar_tensor_tensor", out=xt[i][:, b, :], in0=xt[i][:, b, :], scalar=ss[i][:, b:b + 1], in1=nfb[:],
                                  op0=ALU.mult, op1=ALU.mult), reads=[b_x[i], b_ss[i], b_nfb], writes=[b_x[i]])
                else:
                    P.op("dve", I("scalar_tensor_tensor", out=xt[i][:, b, :], in0=xt[i][:, b, :], scalar=ss[i][:, b:b + 1], in1=nfb[:],
                                  op0=ALU.mult, op1=ALU.mult), reads=[b_x[i], b_ss[i], b_nfb], writes=[b_x[i]])
            P.dma("sp", C.out[t0 - CT:t0 - CT + 512, :].rearrange("(b p) d -> p b d", p=128), xt[i][:], b_x[i],
                  reads=[b_x[i]], writes=[C.b_out])
        P.barrier()
        P.end_phase()


IN_SPECS = [
    ("x", [SEQ, D], F32), ("ctx", [CT, D], F32), ("c", [D], F32), ("c_ctx", [D], F32),
    ("w_mod", [DEPTH, D, 6 * D], F32), ("b_mod", [DEPTH, 6 * D], F32),
    ("norm_attn", [DEPTH, D], F32), ("norm_ffn", [DEPTH, D], F32),
    ("w_in", [DEPTH, D, WIN_COLS], F32), ("sink", [DEPTH, 8], F32),
    ("q_norm", [DEPTH, 384], F32), ("kv_norm", [DEPTH, 256], F32),
    ("w_qu", [DEPTH, 384, 1024], F32), ("w_kvu", [DEPTH, 256, 1024], F32),
    ("w_out", [DEPTH, D, D], F32), ("w_router", [DEPTH, D, NE], F32),
    ("w_gate", [DEPTH, NE, D, DFF], F32), ("w_up", [DEPTH, NE, D, DFF], F32), ("w_down", [DEPTH, NE, DFF, D], F32),
    ("norm_final", [D], F32),
    ("rope", [128, 4, NT], F32),
    ("cst", [128, 1024], F32),
    ("cst2", [128, 2048], F32),
]


def build(upto="all", dumps=(), dense_moe=False):
    nc = bass.Bass("TRN2", target_bir_lowering=False)
    C = Ctx()
    C.dense_moe = dense_moe
    C.nc = nc
    C.din = {nm: nc.dram_tensor(nm, shp, dt, kind="ExternalInput").ap() for nm, shp, dt in IN_SPECS}

    def scratch(nm, shp, dt):
        kind = "ExternalOutput" if nm in dumps else "Internal"
        return nc.dram_tensor(nm, shp, dt, kind=kind).ap()
    C.out = nc.dram_tensor("out", [SEQ, D], F32, kind="ExternalOutput").ap()
    C.xs = scratch("xs", [NT + 128, D], F32)
    C.h2tok = scratch("h2tok", [NT + 128, D], BF16)
    C.mrow_d = scratch("mrow_d", [DEPTH, 2, 6 * D], F32)
    C.wb = {
        "w_in": scratch("wb_in", [DEPTH, D, WIN_COLS], BF16), "w_qu": scratch("wb_qu", [DEPTH, 384, 1024], BF16),
        "w_kvu": scratch("wb_kvu", [DEPTH, 256, 1024], BF16), "w_out": scratch("wb_out", [DEPTH, D, D], BF16),
        "w_gate": scratch("wb_gate", [DEPTH, NE, D, DFF], BF16), "w_up": scratch("wb_up", [DEPTH, NE, D, DFF], BF16),
        "w_down": scratch("wb_down", [DEPTH, NE, DFF, D], BF16),
    }
    C.wbuf = {}
    for l in range(DEPTH):
        bl = Buf("wcast%d" % l, persist=True)
        bl2 = Buf("wcastE%d" % l, persist=True)
        for nm in ("w_in", "w_qu", "w_kvu", "w_out"):
            C.wbuf[(nm, l)] = bl
        for nm in ("w_gate", "w_up", "w_down"):
            for e in range(NE):
                C.wbuf[(nm, l, e)] = bl2
    C.QaT = scratch("QaT", [2, 64, NBLK, 4, 128], BF16)
    C.KaT = scratch("KaT", [128, NT], BF16)
    C.Va = scratch("Va", [NT, 128], BF16)
    C.QbT = scratch("QbT", [8, 96, NT], BF16)
    C.KbT = scratch("KbT", [8, 96, NT], BF16)
    C.Vb = scratch("Vb", [NT, 512], BF16)
    C.OT = scratch("OT", [D, NT], BF16)
    C.h2T = scratch("h2T", [D, NT], BF16)
    C.aff = scratch("aff", [NT + 128, NE], F32)
    for nm in ("QaT", "KaT", "Va", "QbT", "KbT", "Vb", "OT", "h2T", "aff", "xs", "out", "h2tok", "mrow_d", "idxu"):
        setattr(C, "b_" + nm, Buf(nm))
    C.dbg = {}
    if "dbg_mod" in dumps:
        C.dbg["mod"] = nc.dram_tensor("dbg_mod", [128, 96], F32, kind="ExternalOutput").ap()

    with ExitStack() as st:
        P = Prog(nc, st)
        C.P = P
        C.ident = _sb(st, nc, "ident", [128, 128], F32)
        C.modT = _sb(st, nc, "modT", [128, 48, 2], F32)
        C.nrm = _sb(st, nc, "nrm", [128, 2, 8], F32)
        C.gcol = _sb(st, nc, "gcol", [128, 2, 2, 8], F32)
        C.gate_bc = [_sb(st, nc, "gate_bc%d" % i, [128, 2, 1024], F32) for i in range(2)]
        C.sel2 = _sb(st, nc, "sel2", [2, 2, 128], F32)
        C.eps_col = _sb(st, nc, "eps_col", [128, 1], F32)
        C.ones_q = _sb(st, nc, "ones_q", [128, 128], BF16)
        C.ones_kv = _sb(st, nc, "ones_kv", [128, 128], BF16)
        C.thr = _sb(st, nc, "thr", [128, 32], F32)
        C.idxu = _sb(st, nc, "idxu", [128, 2, NE, 8], U32)
        C.b_thr = Buf("thr")
        C.b_const, C.b_modT, C.b_nrm, C.b_gcol = Buf("const"), Buf("modT"), Buf("nrm"), Buf("gcol")
        C.b_gate = [Buf("gate0"), Buf("gate1")]
        block = st.enter_context(nc.Block())
        P.dma("sp", C.ident[:], C.din["cst"][:, 0:128], C.b_const, writes=[C.b_const])
        P.dma("sp", C.sel2[:].rearrange("k r m -> k (r m)"), C.din["cst"][0:2, 384:640], C.b_const, writes=[C.b_const])
        P.op("dve", I("memset", C.eps_col[:], EPS), writes=[C.b_const])
        P.op("dve", I("memset", C.ones_q[:], 1.0 / 384), writes=[C.b_const])
        P.op("dve", I("memset", C.ones_kv[:], 1.0 / 256), writes=[C.b_const])
        P.barrier()

        phase_init(C)
        phase_cast(C, 0, "small")
        for l in range(DEPTH):
            phase_mod(C, l)
            if upto == "mod":
                break
            phase_proj(C, l)
            if upto == "proj":
                break
            phase_attn(C, l)
            if upto == "attn":
                break
            phase_oproj(C, l)
            if upto == "oproj":
                break
            phase_thr(C, l)
            if upto == "thr":
                break
            if C.dense_moe:
                phase_moe(C, l)
            else:
                phase_idx(C, l)
                if upto == "idx":
                    break
                phase_moe2(C, l)
                if l == DEPTH - 1:
                    phase_final(C)
            if upto == "moe":
                break
        if "dbg_idx" in dumps:
            P.dma("sp", nc.dram_tensor("dbg_idx", [128, 256], U32, kind="ExternalOutput").ap(),
                  C.idxu[:].rearrange("p s e b -> p (s e b)"), C.b_idxu, reads=[C.b_idxu], writes=[Buf("x3")])
        if "dbg_thr" in dumps:
            P.dma("sp", nc.dram_tensor("dbg_thr", [128, 32], F32, kind="ExternalOutput").ap(), C.thr[:], C.b_thr, reads=[C.b_thr], writes=[Buf("x2")])
        if "mod" in C.dbg:
            P.dma("sp", C.dbg["mod"], C.modT[:].rearrange("p j r -> p (j r)"), C.b_modT, reads=[C.b_modT], writes=[Buf("x")])
        P.barrier()
        P.replay(block)
    print("instructions (incl waits):", P.ninstr, "dma sems:", len(P.semcnt))
    return nc


def _swap_idx(dh):
    nf = dh // 4
    idx = np.arange(dh)
    out = idx.copy()
    for base in (0, dh // 2):
        out[base:base + nf] = idx[base + nf:base + 2 * nf]
        out[base + nf:base + 2 * nf] = idx[base:base + nf]
    return out


def _rope_tables():
    t = np.arange(SEQ)
    rows, cols = t // 64, t % 64

    def tab(dh):
        da = dh // 2
        nf = da // 2
        inv = (10000.0 ** (-np.arange(nf, dtype=np.float32) / nf)).astype(np.float32)
        cos = np.ones((dh, NT), np.float32)
        sin = np.zeros((dh, NT), np.float32)
        for base, pos in ((0, rows), (da, cols)):
            ang = pos.astype(np.float32)[None, :] * inv[:, None]
            cos[base:base + nf, CT:] = np.cos(ang)
            cos[base + nf:base + 2 * nf, CT:] = np.cos(ang)
            sin[base:base + nf, CT:] = -np.sin(ang)
            sin[base + nf:base + 2 * nf, CT:] = np.sin(ang)
        return cos, sin
    ca, sa = tab(64)
    cb, sb = tab(32)
    out = np.zeros((128, 4, NT), np.float32)
    out[:, 0] = np.tile(ca, (2, 1))
    out[:, 1] = np.tile(sa, (2, 1))
    out[:, 2] = np.tile(cb, (4, 1))
    out[:, 3] = np.tile(sb, (4, 1))
    return out


def _consts():
    c = np.zeros((128, 1024), np.float32)
    c[:, 0:128] = np.eye(128, dtype=np.float32)
    s = np.arange(128)[:, None]
    q = np.arange(128)[None, :]
    c[:, 128:256] = (s >= q)
    c[:, 256:384] = (s <= q)
    c[0, 384:512] = 1.0
    c[1, 512:640] = 1.0
    c[:, 640] = np.arange(128)
    return c


def _consts2():
    c = np.zeros((128, 2048), np.float32)
    pp = np.arange(128)
    c[:, 0:128] = (pp[:, None] <= pp[None, :])
    bb = np.arange(NBLK)
    same = ((bb[:, None] < 2) == (bb[None, :] < 2))
    c[0:NBLK, 128:128 + NBLK] = ((bb[:, None] <= bb[None, :]) & same)
    c[0:NBLK, 256] = (bb >= 2)
    c[0:NBLK, 257] = (bb < 2)
    c[0:NBLK, 258] = bb * 128
    c[:, 259] = 1.0
    for sb in range(8):
        c[:, 260 + sb] = sb * 128 + pp
    c[:, 268] = NT + pp
    c[:, 1024:2048] = np.arange(1024)[None, :]
    return c


def prep_inputs(inp):
    f = lambda a: np.ascontiguousarray(np.asarray(a, dtype=np.float32))
    w_in = f(inp["w_in"])
    sa = _swap_idx(64)
    sb = _swap_idx(32)
    qa = w_in[:, :, 0:512]
    ka = w_in[:, :, 512:640]
    va = w_in[:, :, 640:768]
    cq = w_in[:, :, 768:1152]
    ckv = w_in[:, :, 1152:1408]
    kr = w_in[:, :, 1408:1440]
    qa_sw = qa.reshape(DEPTH, D, 8, 64)[..., sa].reshape(DEPTH, D, 512)
    ka_sw = ka.reshape(DEPTH, D, 2, 64)[..., sa].reshape(DEPTH, D, 128)
    kr_sw = kr[..., sb]
    w_in2 = np.concatenate([qa, qa_sw, ka, ka_sw, cq, ckv, kr, kr_sw, va], axis=-1)
    assert w_in2.shape[-1] == WIN_COLS
    wq = f(inp["w_q_up"]).reshape(DEPTH, 384, 8, 96)
    q_nope = wq[..., :64].reshape(DEPTH, 384, 512)
    q_rope = wq[..., 64:]
    w_qu = np.concatenate([q_nope, q_rope.reshape(DEPTH, 384, 256), q_rope[..., sb].reshape(DEPTH, 384, 256)], axis=-1)
    wkv = f(inp["w_kv_up"]).reshape(DEPTH, 256, 8, 128)
    w_kvu = np.concatenate([wkv[..., :64].reshape(DEPTH, 256, 512), wkv[..., 64:].reshape(DEPTH, 256, 512)], axis=-1)
    shared = {
        "c_ctx": f(inp["c_ctx"]), "w_mod": f(inp["w_mod"]), "b_mod": f(inp["b_mod"]),
        "norm_attn": f(inp["norm_attn"]), "norm_ffn": f(inp["norm_ffn"]),
        "w_in": np.ascontiguousarray(w_in2), "sink": f(inp["sink"]), "q_norm": f(inp["q_norm"]), "kv_norm": f(inp["kv_norm"]),
        "w_qu": np.ascontiguousarray(w_qu), "w_kvu": np.ascontiguousarray(w_kvu), "w_out": f(inp["w_out"]),
        "w_router": f(inp["w_router"]), "w_gate": f(inp["w_gate"]), "w_up": f(inp["w_up"]), "w_down": f(inp["w_down"]),
        "norm_final": f(inp["norm_final"]), "rope": _rope_tables(), "cst": _consts(), "cst2": _consts2(),
    }
    x = f(inp["x"])
    c = f(inp["c"])
    ctx = f(inp["ctx"])
    maps = []
    for core in range(8):
        b = core // 2
        m = dict(shared)
        m["x"] = x[b]
        m["ctx"] = ctx[b]
        m["c"] = c[b]
        maps.append(m)
    return maps


def kernel(**inputs):
    nc = build()
    maps = prep_inputs(inputs)
    res = run_bass_kernel_spmd(nc, maps, core_ids=list(range(8)))
    out = np.stack([res.results[2 * b]["out"] for b in range(4)], axis=0)
    return out.astype(np.float32)
```

```python
import numpy as np
import ml_dtypes
from contextlib import ExitStack
import concourse.bass as bass
import concourse.mybir as mybir
from concourse.bass_utils import run_bass_kernel_spmd

F32 = mybir.dt.float32
BF16 = mybir.dt.bfloat16
I32 = mybir.dt.int32
U32 = mybir.dt.uint32
AF = mybir.ActivationFunctionType
ALU = mybir.AluOpType
AX = mybir.AxisListType

D = 1024
SEQ = 8192
CT = 256
NT = SEQ + CT
NBLK = NT // 128
DEPTH = 2
NE = 16
DFF = 512
EPS = 1e-6
MLA_SCALE = 96 ** -0.5
C_QA, C_QAS, C_KA, C_KAS, C_CQ, C_CKV, C_KR, C_KRS, C_VA = 0, 512, 1024, 1152, 1280, 1664, 1920, 1952, 1984
WIN_COLS = 2112


class Buf:
    __slots__ = ("name", "w", "rd", "dsem", "persist")

    def __init__(self, name, persist=False):
        self.name = name
        self.w = None
        self.rd = []
        self.dsem = None
        self.persist = persist


class Prog:
    ENGS = ("pe", "act", "dve", "pool", "sp")

    def __init__(self, nc, stack):
        self.nc = nc
        self.stack = stack
        self.h = {"pe": nc.tensor, "act": nc.scalar, "dve": nc.vector, "pool": nc.gpsimd, "sp": nc.sync}
        self.esem = {e: stack.enter_context(nc.semaphore("s_" + e)) for e in self.ENGS}
        self.bar = stack.enter_context(nc.semaphore("s_bar"))
        self.nbar = 0
        self.ecnt = {e: 0 for e in self.ENGS}
        self.known = {e: {} for e in self.ENGS}
        self.sems = {("e", e): self.esem[e] for e in self.ENGS}
        self.q = {e: [] for e in self.ENGS}
        self.semcnt = {}
        self.free = {"sp": [], "pool": []}
        self.live = []
        self.ninstr = 0

    def _chan(self, chan, kind):
        if chan.dsem is None:
            chan.dsem = {}
        if kind not in chan.dsem:
            if self.free[kind]:
                chan.dsem[kind] = self.free[kind].pop()
            else:
                idx = len(self.semcnt)
                key = ("d", idx)
                self.sems[key] = self.stack.enter_context(self.nc.semaphore("d%d" % idx))
                self.semcnt[key] = 0
                chan.dsem[kind] = key
            if not getattr(chan, "persist", False):
                self.live.append((chan, kind))
        return chan.dsem[kind]

    def end_phase(self):
        for ch, kind in self.live:
            self.free[kind].append(ch.dsem.pop(kind))
        self.live = []

    def _deps(self, eng, reads, writes, skip_self=False):
        deps = {}

        def add(d):
            if d is None:
                return
            k, v = d
            if skip_self and k == ("e", eng):
                return
            if deps.get(k, 0) < v:
                deps[k] = v
        for b in reads:
            add(b.w)
        for b in writes:
            add(b.w)
            for r in b.rd:
                add(r)
        out = []
        kn = self.known[eng]
        for k, v in deps.items():
            if kn.get(k, 0) >= v:
                continue
            kn[k] = v
            out.append((self.sems[k], v))
        return out

    def _emit(self, eng, waits, fn, sem, inc):
        h = self.h[eng]
        self.ninstr += 1 + len(waits)

        def run():
            for s, v in waits:
                h.wait_ge(s, v)
            fn(h).then_inc(sem, inc)
        self.q[eng].append(run)

    def _record(self, me, reads, writes):
        for b in reads:
            if len(b.rd) > 24:
                b.rd = b.rd[-24:] if False else b.rd
            b.rd.append(me)
        for b in writes:
            b.w = me
            b.rd = []

    def op(self, eng, fn, reads=(), writes=()):
        waits = self._deps(eng, reads, writes, skip_self=(eng == "pe"))
        self.ecnt[eng] += 1
        me = (("e", eng), self.ecnt[eng])
        self._emit(eng, waits, fn, self.esem[eng], 1)
        self._record(me, reads, writes)

    def mm(self, fn, reads=(), writes=()):
        self.op("pe", fn, reads, writes)

    def dma(self, eng, out_ap, in_ap, chan, reads=(), writes=(), **kw):
        key = self._chan(chan, eng)
        waits = self._deps(eng, reads, writes)
        self.semcnt[key] += 16
        me = (key, self.semcnt[key])
        self._emit(eng, waits, I("dma_start", out=out_ap, in_=in_ap, **kw), self.sems[key], 16)
        self._record(me, reads, writes)

    def barrier(self):
        waits = []
        kn = self.known["sp"]
        for e in self.ENGS:
            if e != "sp" and self.ecnt[e] > kn.get(("e", e), 0):
                kn[("e", e)] = self.ecnt[e]
                waits.append((self.esem[e], self.ecnt[e]))
        for k, v in self.semcnt.items():
            if v > kn.get(k, 0):
                kn[k] = v
                waits.append((self.sems[k], v))
        self.nbar += 1
        n = self.nbar
        bar = self.bar
        hs = self.h["sp"]

        def run_sp():
            for s, v in waits:
                hs.wait_ge(s, v)
            hs.sem_inc(bar, 1)
        self.q["sp"].append(run_sp)
        for e in self.ENGS:
            if e == "sp":
                continue
            he = self.h[e]
            self.q[e].append(lambda he=he: he.wait_ge(bar, n))
            for e2 in self.ENGS:
                self.known[e][("e", e2)] = self.ecnt[e2]
            for k, v in self.semcnt.items():
                self.known[e][k] = v

    def replay(self, block):
        q = self.q

        @block.tensor
        def _(e):
            for f in q["pe"]:
                f()

        @block.scalar
        def _(e):
            for f in q["act"]:
                f()

        @block.vector
        def _(e):
            for f in q["dve"]:
                f()

        @block.gpsimd
        def _(e):
            for f in q["pool"]:
                f()

        @block.sync
        def _(e):
            for f in q["sp"]:
                f()


def I(method, *args, **kw):
    def fn(h):
        return getattr(h, method)(*args, **kw)
    return fn


class Ctx:
    pass


_UID = [0]


def _sb(st, nc, name, shape, dt):
    _UID[0] += 1
    return st.enter_context(nc.sbuf_tensor("%s_%d" % (name, _UID[0]), shape, dt))


def _ps(st, nc, name, shape, dt):
    _UID[0] += 1
    return st.enter_context(nc.psum_tensor("%s_%d" % (name, _UID[0]), shape, dt))


def phase_init(C):
    P, nc = C.P, C.nc
    with ExitStack() as st:
        zf = _sb(st, nc, "z_f", [128, 1024], F32)
        zb = _sb(st, nc, "z_b", [128, 1024], BF16)
        b_z = Buf("z")
        P.op("pool", I("memset", zf[:], 0.0), writes=[b_z])
        P.op("pool", I("memset", zb[:], 0.0), writes=[b_z])
        P.dma("sp", C.xs[NT:NT + 128, :], zf[:], b_z, reads=[b_z], writes=[C.b_xs])
        P.dma("sp", C.aff[NT:NT + 128, :], zf[:, 0:NE], b_z, reads=[b_z], writes=[C.b_aff])
        P.dma("sp", C.h2tok[NT:NT + 128, :], zb[:], b_z, reads=[b_z], writes=[C.b_h2tok])
        P.barrier()
        P.end_phase()


def phase_cast(C, l, part):
    P = C.P
    if part == "small":
        for nm in ("w_in", "w_qu", "w_kvu", "w_out"):
            b = C.wbuf[(nm, l)]
            P.dma("pool", C.wb[nm][l], C.din[nm][l], b, writes=[b])
    else:
        for e in range(NE):
            for nm in ("w_gate", "w_up", "w_down"):
                b = C.wbuf[(nm, l, e)]
                P.dma("pool", C.wb[nm][l, e], C.din[nm][l, e], b, writes=[b])


def phase_mod(C, l):
    P, nc = C.P, C.nc
    with ExitStack() as st:
        cs = _sb(st, nc, "m_cs", [128, 8, 2], F32)
        wm = [_sb(st, nc, "m_wm%d" % i, [128, 8, 512], F32) for i in range(2)]
        brow = _sb(st, nc, "m_brow", [2, 6144], F32)
        mrow = _sb(st, nc, "m_mrow", [2, 6144], F32)
        ps_r = [_ps(st, nc, "m_psr%d" % i, [2, 512], F32) for i in range(2)]
        ps_t = _ps(st, nc, "m_pst", [128, 96], F32)
        ps_b = [_ps(st, nc, "m_psb%d" % i, [128, 512], F32) for i in range(2)]
        b_cs, b_brow, b_mrow, b_pst = Buf("cs"), Buf("brow"), Buf("mrow"), Buf("pst")
        b_wm = [Buf("wm0"), Buf("wm1")]
        b_psr = [Buf("psr0"), Buf("psr1")]
        b_psb = [Buf("psb0"), Buf("psb1")]
        P.dma("sp", cs[:, :, 0], C.din["c"].rearrange("(k p) -> p k", p=128), b_cs, writes=[b_cs],
              allow_slow_non_contiguous=True)
        P.dma("sp", cs[:, :, 1], C.din["c_ctx"].rearrange("(k p) -> p k", p=128), b_cs, writes=[b_cs],
              allow_slow_non_contiguous=True)
        P.op("act", I("activation", out=cs[:], in_=cs[:], func=AF.Silu), reads=[b_cs], writes=[b_cs])
        for r in range(2):
            P.dma("sp", brow[r:r + 1, :], C.din["b_mod"][l:l + 1, :], b_brow, writes=[b_brow])
        wsrc = C.din["w_mod"][l].rearrange("(k p) c -> p k c", p=128)
        for cb in range(12):
            i = cb % 2
            P.dma("sp", wm[i][:], wsrc[:, :, cb * 512:(cb + 1) * 512], b_wm[i], writes=[b_wm[i]])
            for k in range(8):
                P.mm(I("matmul", ps_r[i][:], lhsT=cs[:, k, :], rhs=wm[i][:, k, :],
                                                 start=(k == 0), stop=(k == 7)),
                     reads=[b_cs, b_wm[i]], writes=[b_psr[i]])
            P.op("dve", I("tensor_tensor", out=mrow[:, cb * 512:(cb + 1) * 512], in0=ps_r[i][:],
                                                             in1=brow[:, cb * 512:(cb + 1) * 512], op=ALU.add),
                 reads=[b_psr[i], b_brow], writes=[b_mrow])
        P.dma("sp", C.mrow_d[l], mrow[:], b_mrow, reads=[b_mrow], writes=[C.b_mrow_d])
        for j in range(48):
            P.mm(I("transpose", ps_t[:, 2 * j:2 * j + 2], mrow[:, j * 128:(j + 1) * 128], C.ident[0:2, 0:2]),
                 reads=[b_mrow], writes=[b_pst])
        P.op("dve", I("tensor_copy", out=C.modT[:].rearrange("p j r -> p (j r)"), in_=ps_t[:]),
             reads=[b_pst], writes=[C.b_modT])
        P.dma("sp", C.nrm[:, 0, :], C.din["norm_attn"][l].rearrange("(k p) -> p k", p=128), C.b_nrm, writes=[C.b_nrm],
              allow_slow_non_contiguous=True)
        P.dma("sp", C.nrm[:, 1, :], C.din["norm_ffn"][l].rearrange("(k p) -> p k", p=128), C.b_nrm, writes=[C.b_nrm],
              allow_slow_non_contiguous=True)
        for r in range(2):
            for which, j0 in ((0, 8), (1, 32)):
                P.op("dve", I("scalar_tensor_tensor",
                    out=C.gcol[:, which, r, :], in0=C.modT[:, j0:j0 + 8, r], scalar=1.0, in1=C.nrm[:, which, :],
                    op0=ALU.add, op1=ALU.mult),
                    reads=[C.b_modT, C.b_nrm], writes=[C.b_gcol])
        for which, c0 in ((0, 2048), (1, 5120)):
            for r in range(2):
                for hf in range(2):
                    i = (r * 2 + hf) % 2
                    P.mm(I("matmul",
                        ps_b[i][:], lhsT=C.sel2[0:2, r, :], rhs=mrow[:, c0 + hf * 512:c0 + (hf + 1) * 512],
                        start=True, stop=True), reads=[b_mrow], writes=[b_psb[i]])
                    P.op("act", I("activation",
                        out=C.gate_bc[which][:, r, hf * 512:(hf + 1) * 512], in_=ps_b[i][:], func=AF.Copy),
                        reads=[b_psb[i]], writes=[C.b_gate[which]])
        P.barrier()
        P.end_phase()


def norm_mod_T(C, T, x_sb, b_x, nblk, which, r, out_b=None, b_out_b=None, want_b=True):
    P = C.P
    dst_t = T["hT_f"] if out_b is None else out_b
    b_dst = T["b_hT_f"] if out_b is None else b_out_b
    ss, rstd, xn = T["ss"], T["rstd"], T["xn"]
    junk = T["junk"]
    for b in range(nblk):
        P.op("act", I("activation", out=junk[:], in_=x_sb[:, b, :], func=AF.Square,
                                                accum_out=ss[:, b:b + 1]),
             reads=[b_x], writes=[T["b_junk"], T["b_ss"]])
    P.op("act", I("activation", out=rstd[:, 0:nblk], in_=ss[:, 0:nblk], func=AF.Ln, scale=1.0 / D, bias=C.eps_col[:]),
         reads=[T["b_ss"]], writes=[T["b_rstd"]])
    P.op("act", I("activation", out=rstd[:, 0:nblk], in_=rstd[:, 0:nblk], func=AF.Exp, scale=-0.5),
         reads=[T["b_rstd"]], writes=[T["b_rstd"]])
    for b in range(nblk):
        P.op("dve", I("tensor_scalar", out=xn[:, b, :], in0=x_sb[:, b, :], scalar1=rstd[:, b:b + 1],
                                                   scalar2=None, op0=ALU.mult),
             reads=[b_x, T["b_rstd"]], writes=[T["b_xn"]])
    ntok = nblk * 128
    for hf in range(2):
        for cc in range(4):
            c = hf * 4 + cc
            pt = T["ps_tr"][cc]
            for b in range(nblk):
                P.mm(I("transpose", pt[:, b * 128:(b + 1) * 128], xn[:, b, c * 128:(c + 1) * 128],
                                                            C.ident[:]),
                     reads=[T["b_xn"]], writes=[T["b_ps_tr"][cc]])
            eng = "dve" if cc % 2 == 0 else "act"
            if eng == "dve":
                P.op("dve", I("tensor_scalar",
                    out=dst_t[:, c, 0:ntok], in0=pt[:, 0:ntok], scalar1=C.gcol[:, which, r, c:c + 1],
                    scalar2=C.modT[:, (0 if which == 0 else 24) + c, r:r + 1], op0=ALU.mult, op1=ALU.add),
                    reads=[T["b_ps_tr"][cc], C.b_gcol, C.b_modT], writes=[b_dst])
            else:
                P.op("act", I("activation",
                    out=dst_t[:, c, 0:ntok], in_=pt[:, 0:ntok], func=AF.Identity,
                    scale=C.gcol[:, which, r, c:c + 1], bias=C.modT[:, (0 if which == 0 else 24) + c, r:r + 1]),
                    reads=[T["b_ps_tr"][cc], C.b_gcol, C.b_modT], writes=[b_dst])
    if out_b is None and want_b:
        P.op("pool", I("tensor_copy", out=T["hT_b"][:, :, 0:ntok], in_=T["hT_f"][:, :, 0:ntok]),
             reads=[T["b_hT_f"]], writes=[T["b_hT_b"]])


def alloc_norm_tiles(C, st, pfx, with_out=True):
    nc = C.nc
    T = {}
    T["ss"] = _sb(st, nc, pfx + "ss", [128, 4], F32)
    T["rstd"] = _sb(st, nc, pfx + "rstd", [128, 4], F32)
    T["xn"] = _sb(st, nc, pfx + "xn", [128, 4, 1024], F32)
    T["junk"] = _sb(st, nc, pfx + "junk", [128, 1024], BF16)
    if with_out:
        T["hT_f"] = _sb(st, nc, pfx + "hTf", [128, 8, 512], F32)
        T["hT_b"] = _sb(st, nc, pfx + "hTb", [128, 8, 512], BF16)
    T["ps_tr"] = [_ps(st, nc, pfx + "pstr%d" % i, [128, 512], F32) for i in range(4)]
    for k in ("ss", "rstd", "xn", "junk", "hT_f", "hT_b"):
        T["b_" + k] = Buf(pfx + k)
    T["b_ps_tr"] = [Buf(pfx + "pstr%d" % i) for i in range(4)]
    return T


def x_rows(C, l, t0, n):
    if l == 0:
        if t0 < CT:
            return C.din["ctx"][t0:t0 + n, :]
        return C.din["x"][t0 - CT:t0 - CT + n, :]
    return C.xs[t0:t0 + n, :]


def tiles_of(l):
    tl = [(0, 2, 1)]
    for i in range(SEQ // 512):
        tl.append((CT + i * 512, 4, 0))
    return tl


def phase_proj(C, l):
    P, nc = C.P, C.nc
    with ExitStack() as st:
        T = alloc_norm_tiles(C, st, "p_", with_out=False)
        hTb = [_sb(st, nc, "p_hTb%d" % i, [128, 8, 512], BF16) for i in range(2)]
        b_hTb = [Buf("p_hTb0"), Buf("p_hTb1")]
        w_in = _sb(st, nc, "p_win", [128, 8, WIN_COLS], BF16)
        w_qu = _sb(st, nc, "p_wqu", [128, 3, 1024], BF16)
        w_kvu = _sb(st, nc, "p_wkvu", [128, 2, 1024], BF16)
        qn_col = _sb(st, nc, "p_qn", [128, 3], F32)
        kvn_col = _sb(st, nc, "p_kvn", [128, 2], F32)
        xt = [_sb(st, nc, "p_x%d" % i, [128, 4, 1024], F32) for i in range(2)]
        rope = [_sb(st, nc, "p_rope%d" % i, [128, 4, 512], F32) for i in range(2)]
        ev = [_sb(st, nc, "p_ev%d" % i, [128, 512], F32) for i in range(4)]
        ob = [_sb(st, nc, "p_ob%d" % i, [128, 512], BF16) for i in range(6)]
        cq_f = _sb(st, nc, "p_cqf", [128, 5, 512], F32)
        cq_sq = _sb(st, nc, "p_cqsq", [128, 5, 512], BF16)
        cq_n = _sb(st, nc, "p_cqn", [128, 5, 512], BF16)
        rq = _sb(st, nc, "p_rq", [128, 2, 512], F32)
        ps = [_ps(st, nc, "p_ps%d" % i, [128, 512], F32) for i in range(4)]
        b_w = Buf("p_w")
        b_xt = [Buf("p_x0"), Buf("p_x1")]
        b_rope = [Buf("p_rope0"), Buf("p_rope1")]
        b_ev = [Buf("p_ev%d" % i) for i in range(4)]
        b_ob = [Buf("p_ob%d" % i) for i in range(6)]
        b_cqf, b_cqsq, b_cqn, b_rq = Buf("cqf"), Buf("cqsq"), Buf("cqn"), Buf("rq")
        b_ps = [Buf("p_ps%d" % i) for i in range(4)]
        cnt = {"ps": 0, "ev": 0, "ob": 0}

        def nxt(k, n):
            i = cnt[k] % n
            cnt[k] += 1
            return i

        P.dma("sp", w_in[:], C.wb["w_in"][l].rearrange("(k p) c -> p k c", p=128), b_w,
              reads=[C.wbuf[("w_in", l)]], writes=[b_w])
        P.dma("sp", w_qu[:], C.wb["w_qu"][l].rearrange("(k p) c -> p k c", p=128), b_w,
              reads=[C.wbuf[("w_qu", l)]], writes=[b_w])
        P.dma("sp", w_kvu[:], C.wb["w_kvu"][l].rearrange("(k p) c -> p k c", p=128), b_w,
              reads=[C.wbuf[("w_kvu", l)]], writes=[b_w])
        P.dma("sp", qn_col[:], C.din["q_norm"][l].rearrange("(k p) -> p k", p=128), b_w, writes=[b_w],
              allow_slow_non_contiguous=True)
        P.dma("sp", kvn_col[:], C.din["kv_norm"][l].rearrange("(k p) -> p k", p=128), b_w, writes=[b_w],
              allow_slow_non_contiguous=True)

        tl = tiles_of(l)

        def loadx(ti):
            t0, nb, r = tl[ti]
            i = ti % 2
            P.dma("sp", xt[i][:, 0:nb, :], x_rows(C, l, t0, nb * 128).rearrange("(b p) d -> p b d", p=128), b_xt[i],
                  reads=([C.b_xs] if l > 0 else []), writes=[b_xt[i]])

        def loadr(ti):
            t0, nb, r = tl[ti]
            i = ti % 2
            P.dma("sp", rope[i][:, :, 0:nb * 128], C.din["rope"][:, :, t0:t0 + nb * 128], b_rope[i], writes=[b_rope[i]])

        def front(ti):
            t0, nb, r = tl[ti]
            i = ti % 2
            norm_mod_T(C, T, xt[i], b_xt[i], nb, 0, r, out_b=hTb[i], b_out_b=b_hTb[i])

        loadx(0)
        loadx(1)
        loadr(0)
        front(0)
        for ti, (t0, nb, r) in enumerate(tl):
            if ti + 1 < len(tl):
                loadr(ti + 1)
                front(ti + 1)
            if ti + 2 < len(tl):
                loadx(ti + 2)
            i = ti % 2
            n = nb * 128
            hT = hTb[i]
            b_h = b_hTb[i]
            rp = rope[i]
            b_rp = b_rope[i]

            def proj(c0, m, pi):
                for k in range(8):
                    P.mm(I("matmul", ps[pi][0:m, 0:n], lhsT=w_in[:, k, c0:c0 + m], rhs=hT[:, k, 0:n],
                                                 start=(k == 0), stop=(k == 7)),
                         reads=[b_w, b_h], writes=[b_ps[pi]])

            def roped(c0, c0s, m, tab, store):
                pa, pb = nxt("ps", 4), nxt("ps", 4)
                proj(c0, m, pa)
                proj(c0s, m, pb)
                ea, eb, o = nxt("ev", 4), nxt("ev", 4), nxt("ob", 6)
                P.op("dve", I("tensor_tensor", out=ev[ea][0:m, 0:n], in0=ps[pa][0:m, 0:n],
                                                      in1=rp[0:m, tab, 0:n], op=ALU.mult),
                     reads=[b_ps[pa], b_rp], writes=[b_ev[ea]])
                P.op("dve", I("tensor_tensor", out=ev[eb][0:m, 0:n], in0=ps[pb][0:m, 0:n],
                                                      in1=rp[0:m, tab + 1, 0:n], op=ALU.mult),
                     reads=[b_ps[pb], b_rp], writes=[b_ev[eb]])
                P.op("pool", I("tensor_tensor", out=ob[o][0:m, 0:n], in0=ev[ea][0:m, 0:n],
                                                       in1=ev[eb][0:m, 0:n], op=ALU.add),
                     reads=[b_ev[ea], b_ev[eb]], writes=[b_ob[o]])
                store(ob[o], b_ob[o])

            for c in range(4):
                def st_q(o, bo, c=c):
                    for hh in range(2):
                        hq = 2 * c + hh
                        g, j = hq // 4, hq % 4
                        dst = C.QaT[g, :, t0 // 128:t0 // 128 + nb, j, :]
                        P.dma("sp", dst, o[hh * 64:(hh + 1) * 64, 0:n].rearrange("p (b q) -> p b q", q=128),
                              bo, reads=[bo], writes=[C.b_QaT])
                roped(C_QA + c * 128, C_QAS + c * 128, 128, 0, st_q)
            def st_k(o, bo):
                P.dma("sp", C.KaT[:, t0:t0 + n], o[:, 0:n], bo, reads=[bo], writes=[C.b_KaT])
            roped(C_KA, C_KAS, 128, 0, st_k)
            def st_kr(o, bo):
                for hh in range(8):
                    P.dma("sp", C.KbT[hh, 64:96, t0:t0 + n], o[0:32, 0:n], bo, reads=[bo], writes=[C.b_KbT])
            roped(C_KR, C_KRS, 32, 2, st_kr)
            pv = nxt("ps", 4)
            for b in range(nb):
                for k in range(8):
                    P.mm(I("matmul", ps[pv][:, b * 128:(b + 1) * 128], lhsT=hT[:, k, b * 128:(b + 1) * 128],
                                                      rhs=w_in[:, k, C_VA:C_VA + 128], start=(k == 0), stop=(k == 7)),
                         reads=[b_w, b_h], writes=[b_ps[pv]])
            o = nxt("ob", 6)
            P.op("act", I("activation", out=ob[o][:, 0:n], in_=ps[pv][:, 0:n], func=AF.Copy),
                 reads=[b_ps[pv]], writes=[b_ob[o]])
            P.dma("sp", C.Va[t0:t0 + n, :].rearrange("(b p) d -> p b d", p=128),
                  ob[o][:, 0:n].rearrange("p (b d) -> p b d", d=128), b_ob[o], reads=[b_ob[o]], writes=[C.b_Va])
            for c in range(5):
                pi = nxt("ps", 4)
                proj(C_CQ + c * 128, 128, pi)
                P.op("act", I("activation", out=cq_f[:, c, 0:n], in_=ps[pi][:, 0:n], func=AF.Copy),
                     reads=[b_ps[pi]], writes=[b_cqf])
                P.op("act", I("activation", out=cq_sq[:, c, 0:n], in_=ps[pi][:, 0:n], func=AF.Square),
                     reads=[b_ps[pi]], writes=[b_cqsq])
            for (grp, c0, nch, ones) in ((0, 0, 3, C.ones_q), (1, 3, 2, C.ones_kv)):
                pi = nxt("ps", 4)
                for c in range(nch):
                    P.mm(I("matmul", ps[pi][:, 0:n], lhsT=ones[:], rhs=cq_sq[:, c0 + c, 0:n],
                                                 start=(c == 0), stop=(c == nch - 1)),
                         reads=[b_cqsq], writes=[b_ps[pi]])
                P.op("act", I("activation", out=rq[:, grp, 0:n], in_=ps[pi][:, 0:n], func=AF.Ln,
                                                                   bias=C.eps_col[:]),
                     reads=[b_ps[pi]], writes=[b_rq])
                P.op("act", I("activation", out=rq[:, grp, 0:n], in_=rq[:, grp, 0:n], func=AF.Exp, scale=-0.5),
                     reads=[b_rq], writes=[b_rq])
                for c in range(nch):
                    col = qn_col[:, c:c + 1] if grp == 0 else kvn_col[:, c:c + 1]
                    P.op("dve", I("scalar_tensor_tensor",
                        out=cq_n[:, c0 + c, 0:n], in0=cq_f[:, c0 + c, 0:n], scalar=col, in1=rq[:, grp, 0:n],
                        op0=ALU.mult, op1=ALU.mult), reads=[b_cqf, b_rq, b_w], writes=[b_cqn])
            for c in range(4):
                pi = nxt("ps", 4)
                for k in range(3):
                    P.mm(I("matmul", ps[pi][:, 0:n], lhsT=w_qu[:, k, c * 128:(c + 1) * 128],
                                                      rhs=cq_n[:, k, 0:n], start=(k == 0), stop=(k == 2)),
                         reads=[b_w, b_cqn], writes=[b_ps[pi]])
                o = nxt("ob", 6)
                P.op("act", I("activation", out=ob[o][:, 0:n], in_=ps[pi][:, 0:n], func=AF.Copy),
                     reads=[b_ps[pi]], writes=[b_ob[o]])
                for hh in range(2):
                    P.dma("sp", C.QbT[2 * c + hh, 0:64, t0:t0 + n], ob[o][hh * 64:(hh + 1) * 64, 0:n], b_ob[o],
                          reads=[b_ob[o]], writes=[C.b_QbT])
            for c in range(2):
                pa, pb = nxt("ps", 4), nxt("ps", 4)
                for (pi, cc0) in ((pa, 512), (pb, 768)):
                    for k in range(3):
                        P.mm(I("matmul", ps[pi][:, 0:n], lhsT=w_qu[:, k, cc0 + c * 128:cc0 + (c + 1) * 128],
                                                                     rhs=cq_n[:, k, 0:n], start=(k == 0), stop=(k == 2)),
                             reads=[b_w, b_cqn], writes=[b_ps[pi]])
                ea, eb, o = nxt("ev", 4), nxt("ev", 4), nxt("ob", 6)
                P.op("dve", I("tensor_tensor", out=ev[ea][:, 0:n], in0=ps[pa][:, 0:n], in1=rp[:, 2, 0:n], op=ALU.mult),
                     reads=[b_ps[pa], b_rp], writes=[b_ev[ea]])
                P.op("dve", I("tensor_tensor", out=ev[eb][:, 0:n], in0=ps[pb][:, 0:n], in1=rp[:, 3, 0:n], op=ALU.mult),
                     reads=[b_ps[pb], b_rp], writes=[b_ev[eb]])
                P.op("pool", I("tensor_tensor", out=ob[o][:, 0:n], in0=ev[ea][:, 0:n], in1=ev[eb][:, 0:n], op=ALU.add),
                     reads=[b_ev[ea], b_ev[eb]], writes=[b_ob[o]])
                for hh in range(4):
                    P.dma("sp", C.QbT[4 * c + hh, 64:96, t0:t0 + n], ob[o][hh * 32:(hh + 1) * 32, 0:n], b_ob[o],
                          reads=[b_ob[o]], writes=[C.b_QbT])
            for c in range(4):
                pi = nxt("ps", 4)
                for k in range(2):
                    P.mm(I("matmul", ps[pi][:, 0:n], lhsT=w_kvu[:, k, c * 128:(c + 1) * 128],
                                                      rhs=cq_n[:, 3 + k, 0:n], start=(k == 0), stop=(k == 1)),
                         reads=[b_w, b_cqn], writes=[b_ps[pi]])
                o = nxt("ob", 6)
                P.op("act", I("activation", out=ob[o][:, 0:n], in_=ps[pi][:, 0:n], func=AF.Copy),
                     reads=[b_ps[pi]], writes=[b_ob[o]])
                for hh in range(2):
                    P.dma("sp", C.KbT[2 * c + hh, 0:64, t0:t0 + n], ob[o][hh * 64:(hh + 1) * 64, 0:n], b_ob[o],
                          reads=[b_ob[o]], writes=[C.b_KbT])
            for b in range(nb):
                pi = nxt("ps", 4)
                for k in range(2):
                    P.mm(I("matmul", ps[pi][:, :], lhsT=cq_n[:, 3 + k, b * 128:(b + 1) * 128],
                                                      rhs=w_kvu[:, k, 512:1024], start=(k == 0), stop=(k == 1)),
                         reads=[b_w, b_cqn], writes=[b_ps[pi]])
                o = nxt("ob", 6)
                P.op("dve", I("tensor_copy", out=ob[o][:, :], in_=ps[pi][:, :]),
                     reads=[b_ps[pi]], writes=[b_ob[o]])
                P.dma("sp", C.Vb[t0 + b * 128:t0 + (b + 1) * 128, :], ob[o][:, :], b_ob[o], reads=[b_ob[o]], writes=[C.b_Vb])
        P.barrier()
        P.end_phase()


def phase_attn(C, l):
    P, nc = C.P, C.nc
    LA = 2
    with ExitStack() as st:
        kT = [_sb(st, nc, "a_kT%d" % i, [128, NT], BF16) for i in range(2)]
        vv = [_sb(st, nc, "a_v%d" % i, [128, NBLK, 128], BF16) for i in range(2)]
        qq = [_sb(st, nc, "a_q%d" % i, [128, 512], BF16) for i in range(3)]
        pt = [_sb(st, nc, "a_pt%d" % i, [128, 512], BF16) for i in range(4)]
        rc = [_sb(st, nc, "a_rc%d" % i, [128, 512], F32) for i in range(2)]
        bcs = _sb(st, nc, "a_bcs", [128, 512], F32)
        obf = [_sb(st, nc, "a_obf%d" % i, [128, 512], BF16) for i in range(2)]
        mask = _sb(st, nc, "a_mask", [128, 2, 4, 128], BF16)
        maskf = _sb(st, nc, "a_maskf", [128, 256], F32)
        es8 = _sb(st, nc, "a_es8", [128, 8], F32)
        esink = _sb(st, nc, "a_esink", [128, 8, 128], F32)
        den = _sb(st, nc, "a_den", [128, 512], F32)
        b_den = Buf("den")
        ones_r = _sb(st, nc, "a_ones", [128, 64], F32)
        ps_s = [_ps(st, nc, "a_pss%d" % i, [128, 512], F32) for i in range(4)]
        ps_o = [_ps(st, nc, "a_pso%d" % i, [128, 512], F32) for i in range(2)]
        ps_b = _ps(st, nc, "a_psb", [128, 512], F32)
        b_kT = [Buf("kT0"), Buf("kT1")]
        b_v = [Buf("v0"), Buf("v1")]
        b_q = [Buf("q%d" % i) for i in range(3)]
        b_pt = [Buf("pt%d" % i) for i in range(4)]
        b_bcs, b_cst, b_psb = Buf("bcs"), Buf("acst"), Buf("psb")
        b_rc = [Buf("rc0"), Buf("rc1")]
        b_obf = [Buf("obf0"), Buf("obf1")]
        b_pss = [Buf("pss%d" % i) for i in range(4)]
        b_pso = [Buf("pso0"), Buf("pso1")]
        cnt = {"ob": 0}

        P.dma("sp", maskf[:], C.din["cst"][:, 128:384], b_cst, writes=[b_cst])
        for m in range(2):
            P.op("dve", I("tensor_copy", out=mask[:, m, :, :],
                          in_=maskf[:, m * 128:(m + 1) * 128].rearrange("p (o q) -> p o q", o=1).to_broadcast([128, 4, 128])),
                 reads=[b_cst], writes=[b_cst])
        P.dma("sp", es8[64:65, :], C.din["sink"][l:l + 1, :], b_cst, writes=[b_cst])
        P.op("act", I("activation", out=es8[64:65, :], in_=es8[64:65, :], func=AF.Exp), reads=[b_cst], writes=[b_cst])
        P.op("dve", I("tensor_copy", out=esink[64:65, :, :],
                      in_=es8[64:65, :].rearrange("p (e o) -> p e o", o=1).to_broadcast([1, 8, 128])),
             reads=[b_cst], writes=[b_cst])
        P.op("dve", I("memset", ones_r[:], 1.0), writes=[b_cst])
        for i in range(2):
            P.op("pool", I("memset", vv[i][:, :, 64:128], 0.0), writes=[b_v[i]])
            P.op("pool", I("memset", vv[i][:, :, 64:65], 1.0), writes=[b_v[i]])
        for i in range(2):
            P.op("pool", I("memset", kT[i][64:128, :], 0.0), writes=[b_kT[i]])
        for i in range(3):
            P.op("pool", I("memset", qq[i][64:128, :], 0.0), writes=[b_q[i]])
        phase_cast(C, l, "experts")
        if l + 1 < DEPTH:
            phase_cast(C, l + 1, "small")

        pending = []

        def finish(po, n, dst_fn, sink_g=None):
            flush()
            ri = cnt["ob"] % 2
            if sink_g is None:
                P.op("dve", I("reciprocal", out=rc[ri][64:65, 0:n], in_=ps_o[po][64:65, 0:n]), reads=[b_pso[po]], writes=[b_rc[ri]])
            else:
                P.op("dve", I("tensor_tensor", out=den[64:65, :], in0=ps_o[po][64:65, :],
                              in1=esink[64:65, sink_g * 4:(sink_g + 1) * 4, :].rearrange("p e q -> p (e q)"), op=ALU.add),
                     reads=[b_pso[po], b_cst], writes=[b_den])
                P.op("dve", I("reciprocal", out=rc[ri][64:65, 0:n], in_=den[64:65, 0:n]), reads=[b_den], writes=[b_rc[ri]])
            pending.append((po, n, dst_fn, ri))

        def flush():
            while pending:
                finish_b(*pending.pop(0))

        def finish_b(po, n, dst_fn, ri):
            P.mm(I("matmul", ps_b[0:64, 0:n], lhsT=ones_r[64:65, 0:64], rhs=rc[ri][64:65, 0:n], start=True, stop=True),
                 reads=[b_rc[ri], b_cst], writes=[b_psb])
            P.op("dve", I("tensor_copy", out=bcs[0:64, 0:n], in_=ps_b[0:64, 0:n]), reads=[b_psb], writes=[b_bcs])
            o = cnt["ob"] % 2
            cnt["ob"] += 1
            P.op("dve", I("tensor_tensor", out=obf[o][0:64, 0:n], in0=ps_o[po][0:64, 0:n], in1=bcs[0:64, 0:n], op=ALU.mult),
                 reads=[b_pso[po], b_bcs], writes=[b_obf[o]])
            dst_fn(obf[o], b_obf[o])

        srcs = [("w", g) for g in range(2)] + [("m", hh) for hh in range(8)]
        groups = []
        for si, (kind, idx) in enumerate(srcs):
            if kind == "w":
                for qb in range(0 if l == 0 else 2, NBLK):
                    if qb < 2:
                        kbs = [(0, None), (1, None)]
                    else:
                        kbs = [(0, None), (1, None)]
                        if qb - 1 >= 2:
                            kbs.append((qb - 1, 0))
                        kbs.append((qb, None))
                        if qb + 1 < NBLK:
                            kbs.append((qb + 1, 1))
                    groups.append(dict(si=si, kind="w", idx=idx, qb=qb, n=512, kbs=kbs))
            else:
                for (t0, nb, r) in tiles_of(l):
                    if l > 0 and r == 1:
                        continue
                    nkb = 2 if r == 1 else NBLK
                    groups.append(dict(si=si, kind="m", idx=idx, t0=t0, n=nb * 128, kbs=[(kb, None) for kb in range(nkb)]))

        def load_src(si):
            kind, idx = srcs[si]
            i = si % 2
            if kind == "w":
                P.dma("sp", kT[i][0:64, :], C.KaT[idx * 64:(idx + 1) * 64, :], b_kT[i], reads=[C.b_KaT], writes=[b_kT[i]])
                P.dma("sp", vv[i][:, :, 0:64], C.Va.rearrange("(b p) c -> p b c", p=128)[:, :, idx * 64:(idx + 1) * 64], b_v[i],
                      reads=[C.b_Va], writes=[b_v[i]])
            else:
                P.dma("sp", kT[i][0:96, :], C.KbT[idx], b_kT[i], reads=[C.b_KbT], writes=[b_kT[i]])
                P.dma("sp", vv[i][:, :, 0:64], C.Vb.rearrange("(b p) c -> p b c", p=128)[:, :, idx * 64:(idx + 1) * 64], b_v[i],
                      reads=[C.b_Vb], writes=[b_v[i]])

        def load_q(gi):
            G = groups[gi]
            qi = gi % 3
            if G["kind"] == "w":
                P.dma("sp", qq[qi][0:64, :], C.QaT[G["idx"], :, G["qb"], :, :].rearrange("d j q -> d (j q)"), b_q[qi],
                      reads=[C.b_QaT], writes=[b_q[qi]])
            else:
                P.dma("sp", qq[qi][0:96, 0:G["n"]], C.QbT[G["idx"], :, G["t0"]:G["t0"] + G["n"]], b_q[qi],
                      reads=[C.b_QbT], writes=[b_q[qi]])

        steps = []
        for gi, G in enumerate(groups):
            for ki, (kb, mk) in enumerate(G["kbs"]):
                steps.append((gi, ki, kb, mk))

        def stage_A(sidx):
            gi, ki, kb, mk = steps[sidx]
            G = groups[gi]
            if ki == 0:
                if gi == 0:
                    load_src(0)
                    load_q(0)
                if gi + 1 < len(groups):
                    load_q(gi + 1)
            i = G["si"] % 2
            qi = gi % 3
            kd = 128 if G["kind"] == "w" else 96
            n = G["n"]
            s = sidx % 4
            P.mm(I("matmul", ps_s[s][:, 0:n], lhsT=kT[i][0:kd, kb * 128:(kb + 1) * 128], rhs=qq[qi][0:kd, 0:n],
                   start=True, stop=True), reads=[b_kT[i], b_q[qi]], writes=[b_pss[s]])
            p = sidx % 4
            scale = 0.125 if G["kind"] == "w" else MLA_SCALE
            P.op("act", I("activation", out=pt[p][:, 0:n], in_=ps_s[s][:, 0:n], func=AF.Exp, scale=scale),
                 reads=[b_pss[s]], writes=[b_pt[p]])
            if mk is not None:
                P.op("dve", I("tensor_tensor", out=pt[p][:, :], in0=pt[p][:, :],
                              in1=mask[:, mk, :, :].rearrange("p j q -> p (j q)"), op=ALU.mult),
                     reads=[b_pt[p], b_cst], writes=[b_pt[p]])

        def stage_C(sidx):
            gi, ki, kb, mk = steps[sidx]
            G = groups[gi]
            i = G["si"] % 2
            n = G["n"]
            po = gi % 2
            p = sidx % 4
            if ki == 0 and (gi == 0 or groups[gi - 1]["si"] != G["si"]) and G["si"] + 1 < len(srcs):
                load_src(G["si"] + 1)
            first = (ki == 0)
            lastk = (ki == len(G["kbs"]) - 1)
            P.mm(I("matmul", ps_o[po][:, 0:n], lhsT=vv[i][:, kb, :], rhs=pt[p][:, 0:n], start=first, stop=lastk),
                 reads=[b_v[i], b_pt[p]], writes=[b_pso[po]])
            if lastk:
                if G["kind"] == "w":
                    g, qb = G["idx"], G["qb"]

                    def dst_w(o, bo):
                        dst = C.OT[g * 256:(g + 1) * 256, qb * 128:(qb + 1) * 128].rearrange("(j d) q -> d j q", d=64)
                        P.dma("sp", dst, o[0:64, :].rearrange("p (j q) -> p j q", q=128), bo, reads=[bo], writes=[C.b_OT])
                    finish(po, 512, dst_w, sink_g=g)
                else:
                    hh, t0 = G["idx"], G["t0"]

                    def dst_m(o, bo):
                        P.dma("sp", C.OT[512 + hh * 64:512 + (hh + 1) * 64, t0:t0 + n], o[0:64, 0:n], bo, reads=[bo], writes=[C.b_OT])
                    finish(po, n, dst_m)

        ns = len(steps)
        for i in range(ns + LA):
            if i < ns:
                stage_A(i)
            if i - LA >= 0:
                stage_C(i - LA)
        flush()
        P.barrier()
        P.end_phase()


def phase_oproj(C, l):
    P, nc = C.P, C.nc
    last = (l == DEPTH - 1)
    with ExitStack() as st:
        T = alloc_norm_tiles(C, st, "o_")
        w_out = _sb(st, nc, "o_wout", [128, 8, 1024], BF16)
        w_r = _sb(st, nc, "o_wr", [128, 8, NE], F32)
        ot = [_sb(st, nc, "o_ot%d" % i, [128, 8, 512], BF16) for i in range(2)]
        xt = [_sb(st, nc, "o_x%d" % i, [128, 4, 1024], F32) for i in range(2)]
        tmp = [_sb(st, nc, "o_tmp%d" % i, [128, 512], F32) for i in range(2)]
        lg = _sb(st, nc, "o_lg", [128, 4, NE], F32)
        mx = _sb(st, nc, "o_mx", [128, 4], F32)
        sm = _sb(st, nc, "o_sm", [128, 4], F32)
        af = [_sb(st, nc, "o_af%d" % i, [128, 4, NE], F32) for i in range(2)]
        ps = [_ps(st, nc, "o_ps%d" % i, [128, 512], F32) for i in range(2)]
        ps_r = _ps(st, nc, "o_psr", [128, 4, NE], F32)
        b_w = Buf("o_w")
        b_ot = [Buf("o_ot0"), Buf("o_ot1")]
        b_xt = [Buf("o_x0"), Buf("o_x1")]
        b_tmp = [Buf("o_tmp0"), Buf("o_tmp1")]
        b_lg, b_mx, b_sm, b_psr = Buf("lg"), Buf("mx"), Buf("sm"), Buf("psr")
        b_af = [Buf("af0"), Buf("af1")]
        b_ps = [Buf("o_ps0"), Buf("o_ps1")]
        cnt = {"ps": 0, "tmp": 0}

        def nxt(k, n):
            i = cnt[k] % n
            cnt[k] += 1
            return i
        P.dma("sp", w_out[:], C.wb["w_out"][l].rearrange("(k p) c -> p k c", p=128), b_w,
              reads=[C.wbuf[("w_out", l)]], writes=[b_w])
        P.dma("sp", w_r[:], C.din["w_router"][l].rearrange("(k p) e -> p k e", p=128), b_w, writes=[b_w])
        rowF = _sb(st, nc, "o_rowF", [128, 2, 2, 1024], F32)
        nfb = _sb(st, nc, "o_nfb", [128, 1024], F32)
        h2t = [_sb(st, nc, "o_h2t%d" % i, [128, 4, 1024], BF16) for i in range(2)]
        b_rowF = Buf("rowF")
        b_h2t = [Buf("h2t0"), Buf("h2t1")]
        P.dma("sp", nfb[:], C.din["norm_ffn"][l:l + 1, :].partition_broadcast(128), b_rowF, writes=[b_rowF])
        for r in range(2):
            P.dma("sp", rowF[:, r, 0, :], C.mrow_d[l, r:r + 1, 4096:5120].partition_broadcast(128), b_rowF,
                  reads=[C.b_mrow_d], writes=[b_rowF])
            P.dma("sp", rowF[:, r, 1, :], C.mrow_d[l, r:r + 1, 3072:4096].partition_broadcast(128), b_rowF,
                  reads=[C.b_mrow_d], writes=[b_rowF])
            P.op("dve", I("scalar_tensor_tensor", out=rowF[:, r, 0, :], in0=rowF[:, r, 0, :], scalar=1.0, in1=nfb[:],
                          op0=ALU.add, op1=ALU.mult), reads=[b_rowF], writes=[b_rowF])
        tl = [t for t in tiles_of(l) if not (last and t[2] == 1)]

        def load(ti):
            t0, nb, r = tl[ti]
            i = ti % 2
            n = nb * 128
            P.dma("sp", ot[i][:, :, 0:n], C.OT.rearrange("(k p) t -> p k t", p=128)[:, :, t0:t0 + n], b_ot[i],
                  reads=[C.b_OT], writes=[b_ot[i]])
            P.dma("sp", xt[i][:, 0:nb, :], x_rows(C, l, t0, n).rearrange("(b p) d -> p b d", p=128), b_xt[i],
                  reads=([C.b_xs] if l > 0 else []), writes=[b_xt[i]])
        load(0)
        for ti, (t0, nb, r) in enumerate(tl):
            if ti + 1 < len(tl):
                load(ti + 1)
            i = ti % 2
            n = nb * 128
            for b in range(nb):
                for hf in range(2):
                    pi = nxt("ps", 2)
                    for k in range(8):
                        P.mm(I("matmul", ps[pi][:, :], lhsT=ot[i][:, k, b * 128:(b + 1) * 128],
                               rhs=w_out[:, k, hf * 512:(hf + 1) * 512], start=(k == 0), stop=(k == 7)),
                             reads=[b_ot[i], b_w], writes=[b_ps[pi]])
                    tp = nxt("tmp", 2)
                    P.op("dve", I("tensor_tensor", out=tmp[tp][:, :], in0=ps[pi][:, :],
                                  in1=C.gate_bc[0][:, r, hf * 512:(hf + 1) * 512], op=ALU.mult),
                         reads=[b_ps[pi], C.b_gate[0]], writes=[b_tmp[tp]])
                    P.op("pool", I("tensor_tensor", out=xt[i][:, b, hf * 512:(hf + 1) * 512], in0=tmp[tp][:, :],
                                   in1=xt[i][:, b, hf * 512:(hf + 1) * 512], op=ALU.add),
                         reads=[b_tmp[tp], b_xt[i]], writes=[b_xt[i]])
            P.dma("sp", C.xs[t0:t0 + n, :].rearrange("(b p) d -> p b d", p=128), xt[i][:, 0:nb, :], b_xt[i],
                  reads=[b_xt[i]], writes=[C.b_xs])
            norm_mod_T(C, T, xt[i], b_xt[i], nb, 1, r, want_b=False)
            for b in range(nb):
                P.op("dve", I("tensor_tensor", out=T["xn"][:, b, :], in0=T["xn"][:, b, :], in1=rowF[:, r, 0, :], op=ALU.mult),
                     reads=[T["b_xn"], b_rowF], writes=[T["b_xn"]])
                P.op("pool" if b % 2 == 0 else "dve", I("tensor_tensor", out=h2t[i][:, b, :], in0=T["xn"][:, b, :], in1=rowF[:, r, 1, :], op=ALU.add),
                     reads=[T["b_xn"], b_rowF], writes=[b_h2t[i]])
            P.dma("sp", C.h2tok[t0:t0 + n, :].rearrange("(b p) d -> p b d", p=128), h2t[i][:, 0:nb, :], b_h2t[i],
                  reads=[b_h2t[i]], writes=[C.b_h2tok])
            for b in range(nb):
                for k in range(8):
                    P.mm(I("matmul", ps_r[:, b, :], lhsT=T["hT_f"][:, k, b * 128:(b + 1) * 128], rhs=w_r[:, k, :],
                           start=(k == 0), stop=(k == 7)), reads=[T["b_hT_f"], b_w], writes=[b_psr])
            a = ti % 2
            P.op("dve", I("tensor_reduce", out=mx[:, 0:nb], in_=ps_r[:, 0:nb, :], axis=AX.X, op=ALU.max),
                 reads=[b_psr], writes=[b_mx])
            P.op("dve", I("tensor_tensor", out=lg[:, 0:nb, :], in0=ps_r[:, 0:nb, :],
                          in1=mx[:, 0:nb].rearrange("p (b o) -> p b o", o=1).to_broadcast([128, nb, NE]), op=ALU.subtract),
                 reads=[b_psr, b_mx], writes=[b_lg])
            P.op("act", I("activation", out=lg[:, 0:nb, :], in_=lg[:, 0:nb, :], func=AF.Exp), reads=[b_lg], writes=[b_lg])
            P.op("dve", I("tensor_reduce", out=sm[:, 0:nb], in_=lg[:, 0:nb, :], axis=AX.X, op=ALU.add),
                 reads=[b_lg], writes=[b_sm])
            P.op("dve", I("reciprocal", out=sm[:, 0:nb], in_=sm[:, 0:nb]), reads=[b_sm], writes=[b_sm])
            P.op("dve", I("tensor_tensor", out=af[a][:, 0:nb, :], in0=lg[:, 0:nb, :],
                          in1=sm[:, 0:nb].rearrange("p (b o) -> p b o", o=1).to_broadcast([128, nb, NE]), op=ALU.mult),
                 reads=[b_lg, b_sm], writes=[b_af[a]])
            P.dma("sp", C.aff[t0:t0 + n, :].rearrange("(b p) e -> p b e", p=128), af[a][:, 0:nb, :], b_af[a],
                  reads=[b_af[a]], writes=[C.b_aff])
        P.barrier()
        P.end_phase()


def phase_thr(C, l):
    P, nc = C.P, C.nc
    last = (l == DEPTH - 1)
    with ExitStack() as st:
        araw = _sb(st, nc, "t_araw", [128, 64, NE], F32)
        craw = _sb(st, nc, "t_craw", [128, 2, NE], F32)
        A = _sb(st, nc, "t_A", [128, 32, 64], F32)
        cmpt = _sb(st, nc, "t_cmp", [128, 32, 64], BF16)
        cntp = _sb(st, nc, "t_cnt", [128, 32], F32)
        ones = _sb(st, nc, "t_ones", [128, 128], F32)
        lo = _sb(st, nc, "t_lo", [128, 32], F32)
        hi = _sb(st, nc, "t_hi", [128, 32], F32)
        mid = _sb(st, nc, "t_mid", [128, 32], F32)
        cap = _sb(st, nc, "t_cap", [128, 32], F32)
        ge = _sb(st, nc, "t_ge", [128, 32], U32)
        lt = _sb(st, nc, "t_lt", [128, 32], U32)
        ps = _ps(st, nc, "t_ps", [128, 32], F32)
        b_araw, b_A, b_cmp, b_cnt, b_c = Buf("araw"), Buf("A"), Buf("cmp"), Buf("cnt"), Buf("tc")
        b_lo, b_hi, b_mid, b_ge, b_lt, b_ps = Buf("lo"), Buf("hi"), Buf("mid"), Buf("ge"), Buf("lt"), Buf("tps")
        P.dma("sp", araw[:], C.aff[CT:NT, :].rearrange("(p j) e -> p j e", p=128), b_araw, reads=[C.b_aff], writes=[b_araw])
        P.op("pool", I("memset", A[:], 0.0), writes=[b_A])
        P.op("dve", I("tensor_copy", out=A[:, 0:16, :], in_=araw[:].rearrange("p j e -> p e j")), reads=[b_araw], writes=[b_A])
        if not last:
            P.dma("sp", craw[:], C.aff[0:CT, :].rearrange("(p j) e -> p j e", p=128), b_araw, reads=[C.b_aff], writes=[b_araw])
            P.op("dve", I("tensor_copy", out=A[:, 16:32, 0:2], in_=craw[:].rearrange("p j e -> p e j")), reads=[b_araw], writes=[b_A])
        P.op("dve", I("memset", ones[:], 1.0), writes=[b_c])
        P.op("dve", I("memset", cap[:, 0:16], float(2 * SEQ // NE)), writes=[b_c])
        P.op("dve", I("memset", cap[:, 16:32], float(2 * CT // NE)), writes=[b_c])
        P.op("dve", I("memset", lo[:], 0.0), writes=[b_lo])
        P.op("dve", I("memset", hi[:], 1.0), writes=[b_hi])
        P.op("dve", I("memset", cntp[:], 0.0), writes=[b_cnt])
        for it in range(31):
            P.op("dve", I("tensor_tensor", out=mid[:], in0=lo[:], in1=hi[:], op=ALU.add), reads=[b_lo, b_hi], writes=[b_mid])
            P.op("dve", I("tensor_scalar", out=mid[:], in0=mid[:], scalar1=0.5, scalar2=None, op0=ALU.mult),
                 reads=[b_mid], writes=[b_mid])
            P.op("dve", I("tensor_tensor", out=cmpt[:, 0:16, :], in0=A[:, 0:16, :],
                          in1=mid[:, 0:16].rearrange("p (e o) -> p e o", o=1).to_broadcast([128, 16, 64]), op=ALU.is_gt),
                 reads=[b_A, b_mid], writes=[b_cmp])
            P.op("dve", I("tensor_reduce", out=cntp[:, 0:16], in_=cmpt[:, 0:16, :], axis=AX.X, op=ALU.add),
                 reads=[b_cmp], writes=[b_cnt])
            if not last:
                P.op("dve", I("tensor_tensor", out=cmpt[:, 16:32, 0:2], in0=A[:, 16:32, 0:2],
                              in1=mid[:, 16:32].rearrange("p (e o) -> p e o", o=1).to_broadcast([128, 16, 2]), op=ALU.is_gt),
                     reads=[b_A, b_mid], writes=[b_cmp])
                P.op("dve", I("tensor_reduce", out=cntp[:, 16:32], in_=cmpt[:, 16:32, 0:2], axis=AX.X, op=ALU.add),
                     reads=[b_cmp], writes=[b_cnt])
            P.mm(I("matmul", ps[:], lhsT=ones[:], rhs=cntp[:], start=True, stop=True), reads=[b_cnt, b_c], writes=[b_ps])
            P.op("dve", I("tensor_tensor", out=ge[:], in0=ps[:], in1=cap[:], op=ALU.is_ge), reads=[b_ps, b_c], writes=[b_ge])
            P.op("dve", I("tensor_tensor", out=lt[:], in0=ps[:], in1=cap[:], op=ALU.is_lt), reads=[b_ps, b_c], writes=[b_lt])
            P.op("dve", I("copy_predicated", lo[:], ge[:], mid[:]), reads=[b_ge, b_mid], writes=[b_lo])
            P.op("dve", I("copy_predicated", hi[:], lt[:], mid[:]), reads=[b_lt, b_mid], writes=[b_hi])
        P.op("dve", I("tensor_copy", out=C.thr[:], in_=lo[:]), reads=[b_lo], writes=[C.b_thr])
        P.barrier()
        P.end_phase()


def phase_moe(C, l):
    P, nc = C.P, C.nc
    last = (l == DEPTH - 1)
    with ExitStack() as st:
        yacc = _sb(st, nc, "e_yacc", [128, 8, 1024], F32)
        wg = [_sb(st, nc, "e_wg%d" % i, [128, 8, DFF], BF16) for i in range(2)]
        wu = [_sb(st, nc, "e_wu%d" % i, [128, 8, DFF], BF16) for i in range(2)]
        wd = [_sb(st, nc, "e_wd%d" % i, [128, 4, 1024], BF16) for i in range(2)]
        hT = [_sb(st, nc, "e_hT%d" % i, [128, 8, 512], BF16) for i in range(2)]
        afr = [_sb(st, nc, "e_af%d" % i, [128, 4, NE], F32) for i in range(2)]
        gt = [_sb(st, nc, "e_gt%d" % i, [128, 4, NE], F32) for i in range(2)]
        sA = [_sb(st, nc, "e_sA%d" % i, [128, 512], F32) for i in range(2)]
        act = [_sb(st, nc, "e_act%d" % i, [128, 4, 512], BF16) for i in range(2)]
        xt = _sb(st, nc, "e_x", [128, 4, 1024], F32)
        nfb = _sb(st, nc, "e_nfb", [128, 1024], F32)
        ss = _sb(st, nc, "e_ss", [128, 4], F32)
        junk = _sb(st, nc, "e_junk", [128, 1024], BF16)
        psA = [_ps(st, nc, "e_psA%d" % i, [128, 512], F32) for i in range(2)]
        psU = [_ps(st, nc, "e_psU%d" % i, [128, 512], F32) for i in range(2)]
        psY = [_ps(st, nc, "e_psY%d" % i, [128, 512], F32) for i in range(3)]
        b_yacc = [Buf("yacc%d" % i) for i in range(8)]
        b_w = [Buf("ew0"), Buf("ew1")]
        b_hT = [Buf("ehT0"), Buf("ehT1")]
        b_afr = [Buf("eaf0"), Buf("eaf1")]
        b_gt = [Buf("egt0"), Buf("egt1")]
        b_sA = [Buf("esA0"), Buf("esA1")]
        b_act = [Buf("eact0"), Buf("eact1")]
        b_x, b_nfb, b_ss, b_junk = Buf("ex"), Buf("enfb"), Buf("ess"), Buf("ejunk")
        b_psA = [Buf("psA0"), Buf("psA1")]
        b_psU = [Buf("psU0"), Buf("psU1")]
        b_psY = [Buf("psY%d" % i) for i in range(3)]
        cnt = {"A": 0, "U": 0, "Y": 0, "sA": 0, "act": 0, "w": 0}

        def nxt(k, n):
            i = cnt[k] % n
            cnt[k] += 1
            return i
        if last:
            P.dma("sp", nfb[:], C.din["norm_final"].rearrange("(o d) -> o d", o=1).partition_broadcast(128), b_nfb, writes=[b_nfb])
        tl = [t for t in tiles_of(l) if not (last and t[2] == 1)]
        sts = []
        if not last:
            sts.append(tl[0:2])
            rest = tl[2:]
        else:
            rest = tl
        for i in range(0, len(rest), 2):
            sts.append(rest[i:i + 2])
        for stl in sts:
            blk0 = []
            nb_tot = 0
            for si, (t0, nb, r) in enumerate(stl):
                n = nb * 128
                P.dma("sp", hT[si][:, :, 0:n], C.h2T.rearrange("(k p) t -> p k t", p=128)[:, :, t0:t0 + n], b_hT[si],
                      reads=[C.b_h2T], writes=[b_hT[si]])
                P.dma("sp", afr[si][:, 0:nb, :], C.aff[t0:t0 + n, :].rearrange("(b p) e -> p b e", p=128), b_afr[si],
                      reads=[C.b_aff], writes=[b_afr[si]])
                thr = C.thr[:, 16 * r:16 * r + 16].rearrange("p (o e) -> p o e", o=1).to_broadcast([128, nb, NE])
                P.op("dve", I("tensor_tensor", out=gt[si][:, 0:nb, :], in0=afr[si][:, 0:nb, :], in1=thr, op=ALU.is_gt),
                     reads=[b_afr[si], C.b_thr], writes=[b_gt[si]])
                P.op("dve", I("tensor_tensor", out=gt[si][:, 0:nb, :], in0=gt[si][:, 0:nb, :], in1=afr[si][:, 0:nb, :], op=ALU.mult),
                     reads=[b_gt[si], b_afr[si]], writes=[b_gt[si]])
                blk0.append(nb_tot)
                nb_tot += nb
            for e in range(NE):
                wi = nxt("w", 2)
                P.dma("sp", wg[wi][:], C.wb["w_gate"][l, e].rearrange("(k p) f -> p k f", p=128), b_w[wi],
                      reads=[C.wbuf[("w_gate", l, e)]], writes=[b_w[wi]])
                P.dma("sp", wu[wi][:], C.wb["w_up"][l, e].rearrange("(k p) f -> p k f", p=128), b_w[wi],
                      reads=[C.wbuf[("w_up", l, e)]], writes=[b_w[wi]])
                P.dma("sp", wd[wi][:], C.wb["w_down"][l, e].rearrange("(k p) d -> p k d", p=128), b_w[wi],
                      reads=[C.wbuf[("w_down", l, e)]], writes=[b_w[wi]])
                for si, (t0, nb, r) in enumerate(stl):
                    n = nb * 128
                    ai = nxt("act", 2)
                    for fc in range(4):
                        pa, pu = nxt("A", 2), nxt("U", 2)
                        for k in range(8):
                            P.mm(I("matmul", psA[pa][:, 0:n], lhsT=wg[wi][:, k, fc * 128:(fc + 1) * 128], rhs=hT[si][:, k, 0:n],
                                   start=(k == 0), stop=(k == 7)), reads=[b_w[wi], b_hT[si]], writes=[b_psA[pa]])
                        for k in range(8):
                            P.mm(I("matmul", psU[pu][:, 0:n], lhsT=wu[wi][:, k, fc * 128:(fc + 1) * 128], rhs=hT[si][:, k, 0:n],
                                   start=(k == 0), stop=(k == 7)), reads=[b_w[wi], b_hT[si]], writes=[b_psU[pu]])
                        sa = nxt("sA", 2)
                        P.op("act", I("activation", out=sA[sa][:, 0:n], in_=psA[pa][:, 0:n], func=AF.Silu),
                             reads=[b_psA[pa]], writes=[b_sA[sa]])
                        P.op("dve", I("tensor_tensor", out=act[ai][:, fc, 0:n], in0=sA[sa][:, 0:n], in1=psU[pu][:, 0:n], op=ALU.mult),
                             reads=[b_sA[sa], b_psU[pu]], writes=[b_act[ai]])
                    for b in range(nb):
                        blk = blk0[si] + b
                        for hf in range(2):
                            py = nxt("Y", 3)
                            for fc in range(4):
                                P.mm(I("matmul", psY[py][:, :], lhsT=act[ai][:, fc, b * 128:(b + 1) * 128],
                                       rhs=wd[wi][:, fc, hf * 512:(hf + 1) * 512], start=(fc == 0), stop=(fc == 3)),
                                     reads=[b_act[ai], b_w[wi]], writes=[b_psY[py]])
                            ya = yacc[:, blk, hf * 512:(hf + 1) * 512]
                            if e == 0:
                                P.op("dve", I("tensor_scalar", out=ya, in0=psY[py][:, :], scalar1=gt[si][:, b, e:e + 1],
                                              scalar2=None, op0=ALU.mult),
                                     reads=[b_psY[py], b_gt[si]], writes=[b_yacc[blk]])
                            else:
                                P.op("dve", I("scalar_tensor_tensor", out=ya, in0=psY[py][:, :], scalar=gt[si][:, b, e:e + 1],
                                              in1=ya, op0=ALU.mult, op1=ALU.add),
                                     reads=[b_psY[py], b_gt[si], b_yacc[blk]], writes=[b_yacc[blk]])
            for si, (t0, nb, r) in enumerate(stl):
                n = nb * 128
                P.dma("sp", xt[:, 0:nb, :], C.xs[t0:t0 + n, :].rearrange("(b p) d -> p b d", p=128), b_x,
                      reads=[C.b_xs], writes=[b_x])
                for b in range(nb):
                    blk = blk0[si] + b
                    P.op("dve", I("tensor_tensor", out=yacc[:, blk, :], in0=yacc[:, blk, :], in1=C.gate_bc[1][:, r, :], op=ALU.mult),
                         reads=[b_yacc[blk], C.b_gate[1]], writes=[b_yacc[blk]])
                    P.op("pool", I("tensor_tensor", out=xt[:, b, :], in0=xt[:, b, :], in1=yacc[:, blk, :], op=ALU.add),
                         reads=[b_x, b_yacc[blk]], writes=[b_x])
                if not last:
                    P.dma("sp", C.xs[t0:t0 + n, :].rearrange("(b p) d -> p b d", p=128), xt[:, 0:nb, :], b_x,
                          reads=[b_x], writes=[C.b_xs])
                else:
                    for b in range(nb):
                        P.op("act", I("activation", out=junk[:], in_=xt[:, b, :], func=AF.Square, accum_out=ss[:, b:b + 1]),
                             reads=[b_x], writes=[b_junk, b_ss])
                    P.op("act", I("activation", out=ss[:, 0:nb], in_=ss[:, 0:nb], func=AF.Ln, scale=1.0 / D, bias=C.eps_col[:]),
                         reads=[b_ss], writes=[b_ss])
                    P.op("act", I("activation", out=ss[:, 0:nb], in_=ss[:, 0:nb], func=AF.Exp, scale=-0.5), reads=[b_ss], writes=[b_ss])
                    for b in range(nb):
                        P.op("dve", I("scalar_tensor_tensor", out=xt[:, b, :], in0=xt[:, b, :], scalar=ss[:, b:b + 1], in1=nfb[:],
                                      op0=ALU.mult, op1=ALU.mult), reads=[b_x, b_ss, b_nfb], writes=[b_x])
                    P.dma("sp", C.out[t0 - CT:t0 - CT + n, :].rearrange("(b p) d -> p b d", p=128), xt[:, 0:nb, :], b_x,
                          reads=[b_x], writes=[C.b_out])
        P.barrier()
        P.end_phase()


def phase_idx(C, l):
    P, nc = C.P, C.nc
    last = (l == DEPTH - 1)
    nsets = 1 if last else 2
    with ExitStack() as st:
        c2 = _sb(st, nc, "i_c2", [128, 2048], F32)
        affp = _sb(st, nc, "i_affp", [128, NBLK, NE], F32)
        maskb = _sb(st, nc, "i_mask", [128, NBLK, NE], BF16)
        trib = _sb(st, nc, "i_trib", [128, 128], BF16)
        onesb = _sb(st, nc, "i_onesb", [128, 1], BF16)
        cum_sb = _sb(st, nc, "i_cum", [128, NBLK, NE], F32)
        tot = _sb(st, nc, "i_tot", [128, NE], F32)
        cumB = _sb(st, nc, "i_cumB", [128, NE], F32)
        offB = _sb(st, nc, "i_offB", [128, NE], F32)
        cumS = _sb(st, nc, "i_cumS", [128, 2, NE], F32)
        offS = _sb(st, nc, "i_offS", [128, 2, NE], F32)
        cumx = _sb(st, nc, "i_cumx", [128, NE, 132], F32)
        t1 = _sb(st, nc, "i_t1", [128, 1024], F32)
        oh = [_sb(st, nc, "i_oh%d" % i, [128, 1024], F32) for i in range(2)]
        junk3 = _sb(st, nc, "i_junk3", [128, 8, 128], F32)
        rr = _sb(st, nc, "i_rr", [128, 8], F32)
        idf = _sb(st, nc, "i_idf", [128, 8], F32)
        gfull = _sb(st, nc, "i_gfull", [128, 8, 132], F32)
        b_c2, b_affp, b_mask, b_cum, b_tot, b_cb, b_cs, b_cumx = (Buf("c2"), Buf("affp"), Buf("mask"), Buf("cum"), Buf("tot"),
                                                                 Buf("cb"), Buf("cs"), Buf("cumx"))
        b_t1, b_junk, b_rr, b_idf, b_gsb = Buf("t1"), Buf("junk"), Buf("rr"), Buf("idf"), Buf("gsb")
        b_oh = [Buf("oh0"), Buf("oh1")]
        P.dma("sp", c2[:], C.din["cst2"], b_c2, writes=[b_c2])
        P.dma("sp", affp[:], C.aff[0:NT, :].rearrange("(b p) e -> p b e", p=128), b_affp, reads=[C.b_aff], writes=[b_affp])
        P.op("dve", I("tensor_copy", out=trib[:], in_=c2[:, 0:128]), reads=[b_c2], writes=[b_c2])
        P.op("dve", I("memset", onesb[:], 1.0), writes=[b_c2])
        P.op("dve", I("tensor_tensor", out=maskb[:, 2:NBLK, :], in0=affp[:, 2:NBLK, :],
                      in1=C.thr[:, 0:16].rearrange("p (o e) -> p o e", o=1).to_broadcast([128, NBLK - 2, NE]), op=ALU.is_gt),
             reads=[b_affp, C.b_thr], writes=[b_mask])
        if last:
            P.op("dve", I("memset", maskb[:, 0:2, :], 0.0), writes=[b_mask])
        else:
            P.op("dve", I("tensor_tensor", out=maskb[:, 0:2, :], in0=affp[:, 0:2, :],
                          in1=C.thr[:, 16:32].rearrange("p (o e) -> p o e", o=1).to_broadcast([128, 2, NE]), op=ALU.is_gt),
                 reads=[b_affp, C.b_thr], writes=[b_mask])
        import os
        STOP = int(os.environ.get("IDX_STOP", "9"))
        if STOP <= 1:
            P.barrier()
            P.end_phase()
            return
        with ExitStack() as st2:
            ps_cum = [_ps(st2, nc, "i_pscum%d" % i, [128, 352], F32) for i in range(3)]
            ps_tot = _ps(st2, nc, "i_pstot", [128, NE], F32)
            ps_cb = _ps(st2, nc, "i_pscb", [128, NE], F32)
            b_pscum = [Buf("pscum%d" % i) for i in range(3)]
            b_pstot, b_pscb = Buf("pstot"), Buf("pscb")
            mflat = maskb[:].rearrange("p b e -> p (b e)")
            cflat = cum_sb[:].rearrange("p b e -> p (b e)")
            for j in range(3):
                P.mm(I("matmul", ps_cum[j][:, :], lhsT=trib[:], rhs=mflat[:, j * 352:(j + 1) * 352], start=True, stop=True),
                     reads=[b_mask, b_c2], writes=[b_pscum[j]])
                P.op("act", I("activation", out=cflat[:, j * 352:(j + 1) * 352], in_=ps_cum[j][:, :], func=AF.Copy),
                     reads=[b_pscum[j]], writes=[b_cum])
            for e in range(NE if STOP > 2 else 0):
                P.mm(I("matmul", ps_tot[0:NBLK, e:e + 1], lhsT=maskb[:, :, e], rhs=onesb[:], start=True, stop=True),
                     reads=[b_mask, b_c2], writes=[b_pstot])
            P.op("dve", I("tensor_copy", out=tot[0:NBLK, :], in_=ps_tot[0:NBLK, :]), reads=[b_pstot], writes=[b_tot])
            P.mm(I("matmul", ps_cb[0:NBLK, :], lhsT=c2[0:NBLK, 128:128 + NBLK], rhs=tot[0:NBLK, :], start=True, stop=True),
                 reads=[b_tot, b_c2], writes=[b_pscb])
            P.op("dve", I("tensor_copy", out=cumB[0:NBLK, :], in_=ps_cb[0:NBLK, :]), reads=[b_pscb], writes=[b_cb])
            P.op("dve", I("tensor_tensor", out=offB[0:NBLK, :], in0=cumB[0:NBLK, :], in1=tot[0:NBLK, :], op=ALU.subtract),
                 reads=[b_cb, b_tot], writes=[b_cb])
            for sset in range(2):
                col = c2[0:NBLK, 256 + sset:257 + sset]
                P.op("dve", I("tensor_scalar", out=cumS[0:NBLK, sset, :], in0=cumB[0:NBLK, :], scalar1=col, scalar2=None, op0=ALU.mult),
                     reads=[b_cb, b_c2], writes=[b_cs])
                P.op("dve", I("tensor_scalar", out=offS[0:NBLK, sset, :], in0=offB[0:NBLK, :], scalar1=col, scalar2=None, op0=ALU.mult),
                     reads=[b_cb, b_c2], writes=[b_cs])
            P.barrier()
        if STOP <= 3:
            P.end_phase()
            return
        with ExitStack() as st3:
            ps_tr = [_ps(st3, nc, "i_pstr%d" % i, [128, 512], F32) for i in range(2)]
            ps_g = [_ps(st3, nc, "i_psg%d" % i, [128, 512], F32) for i in range(2)]
            b_pstr = [Buf("pstr0"), Buf("pstr1")]
            b_psg = [Buf("psg0"), Buf("psg1")]
            for e in range(NE):
                bank = (e // 4) % 2
                reg = ps_tr[bank][0:NBLK, (e % 4) * 128:(e % 4 + 1) * 128]
                P.mm(I("transpose", reg, cum_sb[:, :, e], C.ident[:]), reads=[b_cum], writes=[b_pstr[bank]])
                P.op("dve", I("tensor_scalar", out=cumx[0:NBLK, e, 0:128], in0=reg, scalar1=offB[0:NBLK, e:e + 1], scalar2=None,
                              op0=ALU.add), reads=[b_pstr[bank], b_cb], writes=[b_cumx])
                P.op("pool", I("tensor_copy", out=cumx[0:NBLK, e, 128:130], in_=c2[0:NBLK, 258:260]), reads=[b_c2], writes=[b_cumx])
            it = 0
            for sset in range(nsets if STOP > 4 else 0):
                nsb = 8 if sset == 0 else 1
                ns = nsb * 128
                for e in range(NE):
                    o = it % 2
                    it += 1
                    iota_s = c2[0:NBLK, 1024:1024 + ns]
                    P.op("dve", I("tensor_scalar", out=t1[0:NBLK, 0:ns], in0=iota_s, scalar1=offS[0:NBLK, sset, e:e + 1], scalar2=None,
                                  op0=ALU.is_ge), reads=[b_c2, b_cs], writes=[b_t1])
                    P.op("dve", I("tensor_scalar", out=oh[o][0:NBLK, 0:ns], in0=iota_s, scalar1=cumS[0:NBLK, sset, e:e + 1], scalar2=None,
                                  op0=ALU.is_lt), reads=[b_c2, b_cs], writes=[b_oh[o]])
                    P.op("dve", I("tensor_tensor", out=oh[o][0:NBLK, 0:ns], in0=oh[o][0:NBLK, 0:ns], in1=t1[0:NBLK, 0:ns], op=ALU.mult),
                         reads=[b_oh[o], b_t1], writes=[b_oh[o]])
                    if STOP <= 5:
                        continue
                    for sb in range(nsb):
                        gi = sb % 2
                        P.mm(I("matmul", ps_g[gi][:, 0:130], lhsT=oh[o][0:NBLK, sb * 128:(sb + 1) * 128], rhs=cumx[0:NBLK, e, 0:130],
                               start=True, stop=True), reads=[b_oh[o], b_cumx], writes=[b_psg[gi]])
                        if STOP <= 6:
                            continue
                        P.op("act", I("activation", out=gfull[:, sb, 0:130], in_=ps_g[gi][:, 0:130], func=AF.Copy),
                             reads=[b_psg[gi]], writes=[b_gsb])
                    if STOP <= 7:
                        continue
                    P.op("dve", I("tensor_tensor", out=junk3[:, 0:nsb, :], in0=gfull[:, 0:nsb, 0:128],
                                  in1=c2[:, 260:260 + nsb].rearrange("p (b o) -> p b o", o=1).to_broadcast([128, nsb, 128]),
                                  op=ALU.is_le), reads=[b_gsb, b_c2], writes=[b_junk])
                    P.op("dve", I("tensor_reduce", out=rr[:, 0:nsb], in_=junk3[:, 0:nsb, :], axis=AX.X, op=ALU.add),
                         reads=[b_junk], writes=[b_rr])
                    P.op("dve", I("tensor_tensor", out=idf[:, 0:nsb], in0=rr[:, 0:nsb], in1=gfull[:, 0:nsb, 128], op=ALU.add),
                         reads=[b_rr, b_gsb], writes=[b_idf])
                    P.op("dve", I("tensor_scalar", out=idf[:, 0:nsb], in0=idf[:, 0:nsb], scalar1=c2[:, 268:269], scalar2=None,
                                  op0=ALU.subtract), reads=[b_idf, b_c2], writes=[b_idf])
                    P.op("dve", I("tensor_tensor", out=idf[:, 0:nsb], in0=idf[:, 0:nsb], in1=gfull[:, 0:nsb, 129], op=ALU.mult),
                         reads=[b_idf, b_gsb], writes=[b_idf])
                    P.op("dve", I("tensor_scalar", out=idf[:, 0:nsb], in0=idf[:, 0:nsb], scalar1=c2[:, 268:269], scalar2=None,
                                  op0=ALU.add), reads=[b_idf, b_c2], writes=[b_idf])
                    P.op("dve", I("tensor_copy", out=C.idxu[:, sset, e, 0:nsb], in_=idf[:, 0:nsb]), reads=[b_idf], writes=[C.b_idxu])
            P.barrier()
        P.end_phase()


def _indirect(C, kind, sb_ap, dram_ap, idx_ap, chan, reads, writes):
    P = C.P
    key = P._chan(chan, "pool")
    waits = P._deps("pool", reads, writes)
    P.semcnt[key] += 16
    me = (key, P.semcnt[key])
    if kind == "g":
        fn = I("indirect_dma_start", out=sb_ap, out_offset=None, in_=dram_ap,
               in_offset=bass.IndirectOffsetOnAxis(ap=idx_ap, axis=0))
    else:
        fn = I("indirect_dma_start", out=dram_ap, out_offset=bass.IndirectOffsetOnAxis(ap=idx_ap, axis=0),
               in_=sb_ap, in_offset=None, compute_op=ALU.add)
    P._emit("pool", waits, fn, P.sems[key], 16)
    P._record(me, reads, writes)


def phase_moe2(C, l):
    P, nc = C.P, C.nc
    last = (l == DEPTH - 1)
    nsets = 1 if last else 2
    with ExitStack() as st:
        wg = [_sb(st, nc, "e_wg%d" % i, [128, 8, DFF], BF16) for i in range(2)]
        wu = [_sb(st, nc, "e_wu%d" % i, [128, 8, DFF], BF16) for i in range(2)]
        wd = [_sb(st, nc, "e_wd%d" % i, [128, 4, 1024], BF16) for i in range(2)]
        xg = [_sb(st, nc, "e_xg%d" % i, [128, 1024], BF16) for i in range(8)]
        ag = [_sb(st, nc, "e_ag%d" % i, [128, NE], F32) for i in range(8)]
        xT = [_sb(st, nc, "e_xT%d" % i, [128, 8, 512], BF16) for i in range(2)]
        sA = [_sb(st, nc, "e_sA%d" % i, [128, 512], F32) for i in range(2)]
        act = [_sb(st, nc, "e_act%d" % i, [128, 4, 512], BF16) for i in range(2)]
        ysb = [_sb(st, nc, "e_y%d" % i, [128, 1024], F32) for i in range(3)]
        identb = _sb(st, nc, "e_identb", [128, 128], BF16)
        ps_t = [_ps(st, nc, "e_pst%d" % i, [128, 8, 128], BF16) for i in range(2)]
        psA = [_ps(st, nc, "e_psA%d" % i, [128, 512], F32) for i in range(2)]
        psU = [_ps(st, nc, "e_psU%d" % i, [128, 512], F32) for i in range(2)]
        psY = [_ps(st, nc, "e_psY%d" % i, [128, 512], F32) for i in range(2)]
        b_w = [Buf("ew0"), Buf("ew1")]
        b_xg = [Buf("xg%d" % i) for i in range(8)]
        b_ag = [Buf("ag%d" % i) for i in range(8)]
        b_xT = [Buf("xT0"), Buf("xT1")]
        b_sA = [Buf("sA0"), Buf("sA1")]
        b_act = [Buf("act0"), Buf("act1")]
        b_y = [Buf("y%d" % i) for i in range(3)]
        b_pst = [Buf("pst0"), Buf("pst1")]
        b_psA, b_psU = [Buf("psA0"), Buf("psA1")], [Buf("psU0"), Buf("psU1")]
        b_psY = [Buf("psY%d" % i) for i in range(2)]
        b_ib = Buf("identb")
        cnt = {"xg": 0, "ag": 0, "t": 0, "xT": 0, "sA": 0, "act": 0, "Y": 0, "y": 0, "w": 0, "A": 0}

        def nxt(k, n):
            i = cnt[k] % n
            cnt[k] += 1
            return i
        P.op("dve", I("tensor_copy", out=identb[:], in_=C.ident[:]), reads=[C.b_const], writes=[b_ib])
        jobs = [(sset, e) for sset in range(nsets) for e in range(NE)]

        def load_w(ji):
            sset, e = jobs[ji]
            wi = ji % 2
            P.dma("sp", wg[wi][:], C.wb["w_gate"][l, e].rearrange("(k p) f -> p k f", p=128), b_w[wi],
                  reads=[C.wbuf[("w_gate", l, e)]], writes=[b_w[wi]])
            P.dma("sp", wu[wi][:], C.wb["w_up"][l, e].rearrange("(k p) f -> p k f", p=128), b_w[wi],
                  reads=[C.wbuf[("w_up", l, e)]], writes=[b_w[wi]])
            P.dma("sp", wd[wi][:], C.wb["w_down"][l, e].rearrange("(k p) d -> p k d", p=128), b_w[wi],
                  reads=[C.wbuf[("w_down", l, e)]], writes=[b_w[wi]])
        tiles = []
        for ji, (sset, e) in enumerate(jobs):
            nsb = 8 if sset == 0 else 1
            for tb in range(0, nsb, 4):
                tiles.append((ji, sset, e, list(range(tb, min(tb + 4, nsb)))))

        def gathers(t):
            ji, sset, e, sbs = tiles[t]
            for j, sb in enumerate(sbs):
                k = (t % 2) * 4 + j
                idx_ap = C.idxu[:, sset, e, sb:sb + 1]
                _indirect(C, "g", xg[k][:], C.h2tok, idx_ap, b_xg[k], [C.b_idxu, C.b_h2tok], [b_xg[k]])
                _indirect(C, "g", ag[k][:], C.aff, idx_ap, b_ag[k], [C.b_idxu, C.b_aff], [b_ag[k]])
        load_w(0)
        gathers(0)
        last_ji = -1
        for t, (ji, sset, e, sbs) in enumerate(tiles):
            if ji != last_ji:
                last_ji = ji
                if ji + 1 < len(jobs):
                    load_w(ji + 1)
            if t + 1 < len(tiles):
                gathers(t + 1)
            wi = ji % 2
            n = len(sbs) * 128
            xi = t % 2
            for j, sb in enumerate(sbs):
                k = (t % 2) * 4 + j
                ti = nxt("t", 2)
                for c in range(8):
                    P.mm(I("transpose", ps_t[ti][:, c, :], xg[k][:, c * 128:(c + 1) * 128], identb[:]),
                         reads=[b_xg[k], b_ib], writes=[b_pst[ti]])
                if j % 2 == 0:
                    P.op("act", I("activation", out=xT[xi][:, :, j * 128:(j + 1) * 128], in_=ps_t[ti][:], func=AF.Copy),
                         reads=[b_pst[ti]], writes=[b_xT[xi]])
                else:
                    P.op("dve", I("tensor_copy", out=xT[xi][:, :, j * 128:(j + 1) * 128], in_=ps_t[ti][:]),
                         reads=[b_pst[ti]], writes=[b_xT[xi]])
            a_i = nxt("act", 2)
            for fc in range(4):
                pa = nxt("A", 2)
                for k in range(8):
                    P.mm(I("matmul", psA[pa][:, 0:n], lhsT=wg[wi][:, k, fc * 128:(fc + 1) * 128], rhs=xT[xi][:, k, 0:n],
                           start=(k == 0), stop=(k == 7)), reads=[b_w[wi], b_xT[xi]], writes=[b_psA[pa]])
                for k in range(8):
                    P.mm(I("matmul", psU[pa][:, 0:n], lhsT=wu[wi][:, k, fc * 128:(fc + 1) * 128], rhs=xT[xi][:, k, 0:n],
                           start=(k == 0), stop=(k == 7)), reads=[b_w[wi], b_xT[xi]], writes=[b_psU[pa]])
                sa = nxt("sA", 2)
                P.op("act", I("activation", out=sA[sa][:, 0:n], in_=psA[pa][:, 0:n], func=AF.Silu),
                     reads=[b_psA[pa]], writes=[b_sA[sa]])
                P.op("dve", I("tensor_tensor", out=act[a_i][:, fc, 0:n], in0=sA[sa][:, 0:n], in1=psU[pa][:, 0:n], op=ALU.mult),
                     reads=[b_sA[sa], b_psU[pa]], writes=[b_act[a_i]])
            for j, sb in enumerate(sbs):
                k = (t % 2) * 4 + j
                yi = nxt("y", 3)
                for hf in range(2):
                    py = nxt("Y", 2)
                    for fc in range(4):
                        P.mm(I("matmul", psY[py][:, :], lhsT=act[a_i][:, fc, j * 128:(j + 1) * 128],
                               rhs=wd[wi][:, fc, hf * 512:(hf + 1) * 512], start=(fc == 0), stop=(fc == 3)),
                             reads=[b_act[a_i], b_w[wi]], writes=[b_psY[py]])
                    P.op("dve", I("scalar_tensor_tensor", out=ysb[yi][:, hf * 512:(hf + 1) * 512], in0=psY[py][:, :],
                                  scalar=ag[k][:, e:e + 1], in1=C.gate_bc[1][:, sset, hf * 512:(hf + 1) * 512],
                                  op0=ALU.mult, op1=ALU.mult),
                         reads=[b_psY[py], b_ag[k], C.b_gate[1]], writes=[b_y[yi]])
                _indirect(C, "s", ysb[yi][:], C.xs, C.idxu[:, sset, e, sb:sb + 1], b_y[yi], [C.b_idxu, b_y[yi]], [C.b_xs])
        P.barrier()
        P.end_phase()


def phase_final(C):
    P, nc = C.P, C.nc
    with ExitStack() as st:
        xt = [_sb(st, nc, "f_x%d" % i, [128, 4, 1024], F32) for i in range(2)]
        nfb = _sb(st, nc, "f_nfb", [128, 1024], F32)
        ss = [_sb(st, nc, "f_ss%d" % i, [128, 4], F32) for i in range(2)]
        junk = _sb(st, nc, "f_junk", [128, 1024], BF16)
        b_x = [Buf("fx0"), Buf("fx1")]
        b_ss = [Buf("fss0"), Buf("fss1")]
        b_nfb, b_junk = Buf("fnfb"), Buf("fjunk")
        P.dma("sp", nfb[:], C.din["norm_final"].rearrange("(o d) -> o d", o=1).partition_broadcast(128), b_nfb, writes=[b_nfb])
        tl = [t for t in tiles_of(1) if t[2] == 0]

        def load(ti):
            t0, nb, r = tl[ti]
            P.dma("sp", xt[ti % 2][:], C.xs[t0:t0 + 512, :].rearrange("(b p) d -> p b d", p=128), b_x[ti % 2],
                  reads=[C.b_xs], writes=[b_x[ti % 2]])
        load(0)
        for ti, (t0, nb, r) in enumerate(tl):
            if ti + 1 < len(tl):
                load(ti + 1)
            i = ti % 2
            for b in range(4):
                P.op("act", I("activation", out=junk[:], in_=xt[i][:, b, :], func=AF.Square, accum_out=ss[i][:, b:b + 1]),
                     reads=[b_x[i]], writes=[b_junk, b_ss[i]])
            P.op("act", I("activation", out=ss[i][:], in_=ss[i][:], func=AF.Ln, scale=1.0 / D, bias=C.eps_col[:]),
                 reads=[b_ss[i]], writes=[b_ss[i]])
            P.op("act", I("activation", out=ss[i][:], in_=ss[i][:], func=AF.Exp, scale=-0.5), reads=[b_ss[i]], writes=[b_ss[i]])
            for b in range(4):
                eng = "dve" if b % 2 == 0 else "pool"
                if eng == "dve":
                    P.op("dve", I("scalar_tensor_tensor", out=xt[i][:, b, :], in0=xt[i][:, b, :], scalar=ss[i][:, b:b + 1], in1=nfb[:],
                                  op0=ALU.mult, op1=ALU.mult), reads=[b_x[i], b_ss[i], b_nfb], writes=[b_x[i]])
                else:
                    P.op("dve", I("scalar_tensor_tensor", out=xt[i][:, b, :], in0=xt[i][:, b, :], scalar=ss[i][:, b:b + 1], in1=nfb[:],
                                  op0=ALU.mult, op1=ALU.mult), reads=[b_x[i], b_ss[i], b_nfb], writes=[b_x[i]])
            P.dma("sp", C.out[t0 - CT:t0 - CT + 512, :].rearrange("(b p) d -> p b d", p=128), xt[i][:], b_x[i],
                  reads=[b_x[i]], writes=[C.b_out])
        P.barrier()
        P.end_phase()


IN_SPECS = [
    ("x", [SEQ, D], F32), ("ctx", [CT, D], F32), ("c", [D], F32), ("c_ctx", [D], F32),
    ("w_mod", [DEPTH, D, 6 * D], F32), ("b_mod", [DEPTH, 6 * D], F32),
    ("norm_attn", [DEPTH, D], F32), ("norm_ffn", [DEPTH, D], F32),
    ("w_in", [DEPTH, D, WIN_COLS], F32), ("sink", [DEPTH, 8], F32),
    ("q_norm", [DEPTH, 384], F32), ("kv_norm", [DEPTH, 256], F32),
    ("w_qu", [DEPTH, 384, 1024], F32), ("w_kvu", [DEPTH, 256, 1024], F32),
    ("w_out", [DEPTH, D, D], F32), ("w_router", [DEPTH, D, NE], F32),
    ("w_gate", [DEPTH, NE, D, DFF], F32), ("w_up", [DEPTH, NE, D, DFF], F32), ("w_down", [DEPTH, NE, DFF, D], F32),
    ("norm_final", [D], F32),
    ("rope", [128, 4, NT], F32),
    ("cst", [128, 1024], F32),
    ("cst2", [128, 2048], F32),
]


def build(upto="all", dumps=(), dense_moe=False):
    nc = bass.Bass("TRN2", target_bir_lowering=False)
    C = Ctx()
    C.dense_moe = dense_moe
    C.nc = nc
    C.din = {nm: nc.dram_tensor(nm, shp, dt, kind="ExternalInput").ap() for nm, shp, dt in IN_SPECS}

    def scratch(nm, shp, dt):
        kind = "ExternalOutput" if nm in dumps else "Internal"
        return nc.dram_tensor(nm, shp, dt, kind=kind).ap()
    C.out = nc.dram_tensor("out", [SEQ, D], F32, kind="ExternalOutput").ap()
    C.xs = scratch("xs", [NT + 128, D], F32)
    C.h2tok = scratch("h2tok", [NT + 128, D], BF16)
    C.mrow_d = scratch("mrow_d", [DEPTH, 2, 6 * D], F32)
    C.wb = {
        "w_in": scratch("wb_in", [DEPTH, D, WIN_COLS], BF16), "w_qu": scratch("wb_qu", [DEPTH, 384, 1024], BF16),
        "w_kvu": scratch("wb_kvu", [DEPTH, 256, 1024], BF16), "w_out": scratch("wb_out", [DEPTH, D, D], BF16),
        "w_gate": scratch("wb_gate", [DEPTH, NE, D, DFF], BF16), "w_up": scratch("wb_up", [DEPTH, NE, D, DFF], BF16),
        "w_down": scratch("wb_down", [DEPTH, NE, DFF, D], BF16),
    }
    C.wbuf = {}
    for l in range(DEPTH):
        bl = Buf("wcast%d" % l, persist=True)
        bl2 = Buf("wcastE%d" % l, persist=True)
        for nm in ("w_in", "w_qu", "w_kvu", "w_out"):
            C.wbuf[(nm, l)] = bl
        for nm in ("w_gate", "w_up", "w_down"):
            for e in range(NE):
                C.wbuf[(nm, l, e)] = bl2
    C.QaT = scratch("QaT", [2, 64, NBLK, 4, 128], BF16)
    C.KaT = scratch("KaT", [128, NT], BF16)
    C.Va = scratch("Va", [NT, 128], BF16)
    C.QbT = scratch("QbT", [8, 96, NT], BF16)
    C.KbT = scratch("KbT", [8, 96, NT], BF16)
    C.Vb = scratch("Vb", [NT, 512], BF16)
    C.OT = scratch("OT", [D, NT], BF16)
    C.h2T = scratch("h2T", [D, NT], BF16)
    C.aff = scratch("aff", [NT + 128, NE], F32)
    for nm in ("QaT", "KaT", "Va", "QbT", "KbT", "Vb", "OT", "h2T", "aff", "xs", "out", "h2tok", "mrow_d", "idxu"):
        setattr(C, "b_" + nm, Buf(nm))
    C.dbg = {}
    if "dbg_mod" in dumps:
        C.dbg["mod"] = nc.dram_tensor("dbg_mod", [128, 96], F32, kind="ExternalOutput").ap()

    with ExitStack() as st:
        P = Prog(nc, st)
        C.P = P
        C.ident = _sb(st, nc, "ident", [128, 128], F32)
        C.modT = _sb(st, nc, "modT", [128, 48, 2], F32)
        C.nrm = _sb(st, nc, "nrm", [128, 2, 8], F32)
        C.gcol = _sb(st, nc, "gcol", [128, 2, 2, 8], F32)
        C.gate_bc = [_sb(st, nc, "gate_bc%d" % i, [128, 2, 1024], F32) for i in range(2)]
        C.sel2 = _sb(st, nc, "sel2", [2, 2, 128], F32)
        C.eps_col = _sb(st, nc, "eps_col", [128, 1], F32)
        C.ones_q = _sb(st, nc, "ones_q", [128, 128], BF16)
        C.ones_kv = _sb(st, nc, "ones_kv", [128, 128], BF16)
        C.thr = _sb(st, nc, "thr", [128, 32], F32)
        C.idxu = _sb(st, nc, "idxu", [128, 2, NE, 8], U32)
        C.b_thr = Buf("thr")
        C.b_const, C.b_modT, C.b_nrm, C.b_gcol = Buf("const"), Buf("modT"), Buf("nrm"), Buf("gcol")
        C.b_gate = [Buf("gate0"), Buf("gate1")]
        block = st.enter_context(nc.Block())
        P.dma("sp", C.ident[:], C.din["cst"][:, 0:128], C.b_const, writes=[C.b_const])
        P.dma("sp", C.sel2[:].rearrange("k r m -> k (r m)"), C.din["cst"][0:2, 384:640], C.b_const, writes=[C.b_const])
        P.op("dve", I("memset", C.eps_col[:], EPS), writes=[C.b_const])
        P.op("dve", I("memset", C.ones_q[:], 1.0 / 384), writes=[C.b_const])
        P.op("dve", I("memset", C.ones_kv[:], 1.0 / 256), writes=[C.b_const])
        P.barrier()

        phase_init(C)
        phase_cast(C, 0, "small")
        for l in range(DEPTH):
            phase_mod(C, l)
            if upto == "mod":
                break
            phase_proj(C, l)
            if upto == "proj":
                break
            phase_attn(C, l)
            if upto == "attn":
                break
            phase_oproj(C, l)
            if upto == "oproj":
                break
            phase_thr(C, l)
            if upto == "thr":
                break
            if C.dense_moe:
                phase_moe(C, l)
            else:
                phase_idx(C, l)
                if upto == "idx":
                    break
                phase_moe2(C, l)
                if l == DEPTH - 1:
                    phase_final(C)
            if upto == "moe":
                break
        if "dbg_idx" in dumps:
            P.dma("sp", nc.dram_tensor("dbg_idx", [128, 256], U32, kind="ExternalOutput").ap(),
                  C.idxu[:].rearrange("p s e b -> p (s e b)"), C.b_idxu, reads=[C.b_idxu], writes=[Buf("x3")])
        if "dbg_thr" in dumps:
            P.dma("sp", nc.dram_tensor("dbg_thr", [128, 32], F32, kind="ExternalOutput").ap(), C.thr[:], C.b_thr, reads=[C.b_thr], writes=[Buf("x2")])
        if "mod" in C.dbg:
            P.dma("sp", C.dbg["mod"], C.modT[:].rearrange("p j r -> p (j r)"), C.b_modT, reads=[C.b_modT], writes=[Buf("x")])
        P.barrier()
        P.replay(block)
    print("instructions (incl waits):", P.ninstr, "dma sems:", len(P.semcnt))
    return nc


def _swap_idx(dh):
    nf = dh // 4
    idx = np.arange(dh)
    out = idx.copy()
    for base in (0, dh // 2):
        out[base:base + nf] = idx[base + nf:base + 2 * nf]
        out[base + nf:base + 2 * nf] = idx[base:base + nf]
    return out


def _rope_tables():
    t = np.arange(SEQ)
    rows, cols = t // 64, t % 64

    def tab(dh):
        da = dh // 2
        nf = da // 2
        inv = (10000.0 ** (-np.arange(nf, dtype=np.float32) / nf)).astype(np.float32)
        cos = np.ones((dh, NT), np.float32)
        sin = np.zeros((dh, NT), np.float32)
        for base, pos in ((0, rows), (da, cols)):
            ang = pos.astype(np.float32)[None, :] * inv[:, None]
            cos[base:base + nf, CT:] = np.cos(ang)
            cos[base + nf:base + 2 * nf, CT:] = np.cos(ang)
            sin[base:base + nf, CT:] = -np.sin(ang)
            sin[base + nf:base + 2 * nf, CT:] = np.sin(ang)
        return cos, sin
    ca, sa = tab(64)
    cb, sb = tab(32)
    out = np.zeros((128, 4, NT), np.float32)
    out[:, 0] = np.tile(ca, (2, 1))
    out[:, 1] = np.tile(sa, (2, 1))
    out[:, 2] = np.tile(cb, (4, 1))
    out[:, 3] = np.tile(sb, (4, 1))
    return out


def _consts():
    c = np.zeros((128, 1024), np.float32)
    c[:, 0:128] = np.eye(128, dtype=np.float32)
    s = np.arange(128)[:, None]
    q = np.arange(128)[None, :]
    c[:, 128:256] = (s >= q)
    c[:, 256:384] = (s <= q)
    c[0, 384:512] = 1.0
    c[1, 512:640] = 1.0
    c[:, 640] = np.arange(128)
    return c


def _consts2():
    c = np.zeros((128, 2048), np.float32)
    pp = np.arange(128)
    c[:, 0:128] = (pp[:, None] <= pp[None, :])
    bb = np.arange(NBLK)
    same = ((bb[:, None] < 2) == (bb[None, :] < 2))
    c[0:NBLK, 128:128 + NBLK] = ((bb[:, None] <= bb[None, :]) & same)
    c[0:NBLK, 256] = (bb >= 2)
    c[0:NBLK, 257] = (bb < 2)
    c[0:NBLK, 258] = bb * 128
    c[:, 259] = 1.0
    for sb in range(8):
        c[:, 260 + sb] = sb * 128 + pp
    c[:, 268] = NT + pp
    c[:, 1024:2048] = np.arange(1024)[None, :]
    return c


def prep_inputs(inp):
    f = lambda a: np.ascontiguousarray(np.asarray(a, dtype=np.float32))
    w_in = f(inp["w_in"])
    sa = _swap_idx(64)
    sb = _swap_idx(32)
    qa = w_in[:, :, 0:512]
    ka = w_in[:, :, 512:640]
    va = w_in[:, :, 640:768]
    cq = w_in[:, :, 768:1152]
    ckv = w_in[:, :, 1152:1408]
    kr = w_in[:, :, 1408:1440]
    qa_sw = qa.reshape(DEPTH, D, 8, 64)[..., sa].reshape(DEPTH, D, 512)
    ka_sw = ka.reshape(DEPTH, D, 2, 64)[..., sa].reshape(DEPTH, D, 128)
    kr_sw = kr[..., sb]
    w_in2 = np.concatenate([qa, qa_sw, ka, ka_sw, cq, ckv, kr, kr_sw, va], axis=-1)
    assert w_in2.shape[-1] == WIN_COLS
    wq = f(inp["w_q_up"]).reshape(DEPTH, 384, 8, 96)
    q_nope = wq[..., :64].reshape(DEPTH, 384, 512)
    q_rope = wq[..., 64:]
    w_qu = np.concatenate([q_nope, q_rope.reshape(DEPTH, 384, 256), q_rope[..., sb].reshape(DEPTH, 384, 256)], axis=-1)
    wkv = f(inp["w_kv_up"]).reshape(DEPTH, 256, 8, 128)
    w_kvu = np.concatenate([wkv[..., :64].reshape(DEPTH, 256, 512), wkv[..., 64:].reshape(DEPTH, 256, 512)], axis=-1)
    shared = {
        "c_ctx": f(inp["c_ctx"]), "w_mod": f(inp["w_mod"]), "b_mod": f(inp["b_mod"]),
        "norm_attn": f(inp["norm_attn"]), "norm_ffn": f(inp["norm_ffn"]),
        "w_in": np.ascontiguousarray(w_in2), "sink": f(inp["sink"]), "q_norm": f(inp["q_norm"]), "kv_norm": f(inp["kv_norm"]),
        "w_qu": np.ascontiguousarray(w_qu), "w_kvu": np.ascontiguousarray(w_kvu), "w_out": f(inp["w_out"]),
        "w_router": f(inp["w_router"]), "w_gate": f(inp["w_gate"]), "w_up": f(inp["w_up"]), "w_down": f(inp["w_down"]),
        "norm_final": f(inp["norm_final"]), "rope": _rope_tables(), "cst": _consts(), "cst2": _consts2(),
    }
    x = f(inp["x"])
    c = f(inp["c"])
    ctx = f(inp["ctx"])
    maps = []
    for core in range(8):
        b = core // 2
        m = dict(shared)
        m["x"] = x[b]
        m["ctx"] = ctx[b]
        m["c"] = c[b]
        maps.append(m)
    return maps


def kernel(**inputs):
    nc = build()
    maps = prep_inputs(inputs)
    res = run_bass_kernel_spmd(nc, maps, core_ids=list(range(8)))
    out = np.stack([res.results[2 * b]["out"] for b in range(4)], axis=0)
    return out.astype(np.float32)
```

```python
import numpy as np
import ml_dtypes
from contextlib import ExitStack
import concourse.bass as bass
import concourse.mybir as mybir
from concourse.bass_utils import run_bass_kernel_spmd

F32 = mybir.dt.float32
BF16 = mybir.dt.bfloat16
I32 = mybir.dt.int32
U32 = mybir.dt.uint32
AF = mybir.ActivationFunctionType
ALU = mybir.AluOpType
AX = mybir.AxisListType

D = 1024
SEQ = 8192
CT = 256
NT = SEQ + CT
NBLK = NT // 128
DEPTH = 2
NE = 16
DFF = 512
EPS = 1e-6
MLA_SCALE = 96 ** -0.5
C_QA, C_QAS, C_KA, C_KAS, C_CQ, C_CKV, C_KR, C_KRS, C_VA = 0, 512, 1024, 1152, 1280, 1664, 1920, 1952, 1984
WIN_COLS = 2112


class Buf:
    __slots__ = ("name", "w", "rd", "dsem", "persist")

    def __init__(self, name, persist=False):
        self.name = name
        self.w = None
        self.rd = []
        self.dsem = None
        self.persist = persist


class Prog:
    ENGS = ("pe", "act", "dve", "pool", "sp")

    def __init__(self, nc, stack):
        self.nc = nc
        self.stack = stack
        self.h = {"pe": nc.tensor, "act": nc.scalar, "dve": nc.vector, "pool": nc.gpsimd, "sp": nc.sync}
        self.esem = {e: stack.enter_context(nc.semaphore("s_" + e)) for e in self.ENGS}
        self.bar = stack.enter_context(nc.semaphore("s_bar"))
        self.nbar = 0
        self.ecnt = {e: 0 for e in self.ENGS}
        self.known = {e: {} for e in self.ENGS}
        self.sems = {("e", e): self.esem[e] for e in self.ENGS}
        self.q = {e: [] for e in self.ENGS}
        self.semcnt = {}
        self.free = {"sp": [], "pool": []}
        self.live = []
        self.ninstr = 0

    def _chan(self, chan, kind):
        if chan.dsem is None:
            chan.dsem = {}
        if kind not in chan.dsem:
            if self.free[kind]:
                chan.dsem[kind] = self.free[kind].pop()
            else:
                idx = len(self.semcnt)
                key = ("d", idx)
                self.sems[key] = self.stack.enter_context(self.nc.semaphore("d%d" % idx))
                self.semcnt[key] = 0
                chan.dsem[kind] = key
            if not getattr(chan, "persist", False):
                self.live.append((chan, kind))
        return chan.dsem[kind]

    def end_phase(self):
        for ch, kind in self.live:
            self.free[kind].append(ch.dsem.pop(kind))
        self.live = []

    def _deps(self, eng, reads, writes, skip_self=False):
        deps = {}

        def add(d):
            if d is None:
                return
            k, v = d
            if skip_self and k == ("e", eng):
                return
            if deps.get(k, 0) < v:
                deps[k] = v
        for b in reads:
            add(b.w)
        for b in writes:
            add(b.w)
            for r in b.rd:
                add(r)
        out = []
        kn = self.known[eng]
        for k, v in deps.items():
            if kn.get(k, 0) >= v:
                continue
            kn[k] = v
            out.append((self.sems[k], v))
        return out

    def _emit(self, eng, waits, fn, sem, inc):
        h = self.h[eng]
        self.ninstr += 1 + len(waits)

        def run():
            for s, v in waits:
                h.wait_ge(s, v)
            fn(h).then_inc(sem, inc)
        self.q[eng].append(run)

    def _record(self, me, reads, writes):
        for b in reads:
            if len(b.rd) > 24:
                b.rd = b.rd[-24:] if False else b.rd
            b.rd.append(me)
        for b in writes:
            b.w = me
            b.rd = []

    def op(self, eng, fn, reads=(), writes=()):
        waits = self._deps(eng, reads, writes, skip_self=(eng == "pe"))
        self.ecnt[eng] += 1
        me = (("e", eng), self.ecnt[eng])
        self._emit(eng, waits, fn, self.esem[eng], 1)
        self._record(me, reads, writes)

    def mm(self, fn, reads=(), writes=()):
        self.op("pe", fn, reads, writes)

    def dma(self, eng, out_ap, in_ap, chan, reads=(), writes=(), **kw):
        key = self._chan(chan, eng)
        waits = self._deps(eng, reads, writes)
        self.semcnt[key] += 16
        me = (key, self.semcnt[key])
        self._emit(eng, waits, I("dma_start", out=out_ap, in_=in_ap, **kw), self.sems[key], 16)
        self._record(me, reads, writes)

    def barrier(self):
        waits = []
        kn = self.known["sp"]
        for e in self.ENGS:
            if e != "sp" and self.ecnt[e] > kn.get(("e", e), 0):
                kn[("e", e)] = self.ecnt[e]
                waits.append((self.esem[e], self.ecnt[e]))
        for k, v in self.semcnt.items():
            if v > kn.get(k, 0):
                kn[k] = v
                waits.append((self.sems[k], v))
        self.nbar += 1
        n = self.nbar
        bar = self.bar
        hs = self.h["sp"]

        def run_sp():
            for s, v in waits:
                hs.wait_ge(s, v)
            hs.sem_inc(bar, 1)
        self.q["sp"].append(run_sp)
        for e in self.ENGS:
            if e == "sp":
                continue
            he = self.h[e]
            self.q[e].append(lambda he=he: he.wait_ge(bar, n))
            for e2 in self.ENGS:
                self.known[e][("e", e2)] = self.ecnt[e2]
            for k, v in self.semcnt.items():
                self.known[e][k] = v

    def replay(self, block):
        q = self.q

        @block.tensor
        def _(e):
            for f in q["pe"]:
                f()

        @block.scalar
        def _(e):
            for f in q["act"]:
                f()

        @block.vector
        def _(e):
            for f in q["dve"]:
                f()

        @block.gpsimd
        def _(e):
            for f in q["pool"]:
                f()

        @block.sync
        def _(e):
            for f in q["sp"]:
                f()


def I(method, *args, **kw):
    def fn(h):
        return getattr(h, method)(*args, **kw)
    return fn


class Ctx:
    pass


_UID = [0]


def _sb(st, nc, name, shape, dt):
    _UID[0] += 1
    return st.enter_context(nc.sbuf_tensor("%s_%d" % (name, _UID[0]), shape, dt))


def _ps(st, nc, name, shape, dt):
    _UID[0] += 1
    return st.enter_context(nc.psum_tensor("%s_%d" % (name, _UID[0]), shape, dt))


def phase_init(C):
    P, nc = C.P, C.nc
    with ExitStack() as st:
        zf = _sb(st, nc, "z_f", [128, 1024], F32)
        zb = _sb(st, nc, "z_b", [128, 1024], BF16)
        b_z = Buf("z")
        P.op("pool", I("memset", zf[:], 0.0), writes=[b_z])
        P.op("pool", I("memset", zb[:], 0.0), writes=[b_z])
        P.dma("sp", C.xs[NT:NT + 128, :], zf[:], b_z, reads=[b_z], writes=[C.b_xs])
        P.dma("sp", C.aff[NT:NT + 128, :], zf[:, 0:NE], b_z, reads=[b_z], writes=[C.b_aff])
        P.dma("sp", C.h2tok[NT:NT + 128, :], zb[:], b_z, reads=[b_z], writes=[C.b_h2tok])
        P.barrier()
        P.end_phase()


def phase_cast(C, l, part):
    P = C.P
    if part == "small":
        for nm in ("w_in", "w_qu", "w_kvu", "w_out"):
            b = C.wbuf[(nm, l)]
            P.dma("pool", C.wb[nm][l], C.din[nm][l], b, writes=[b])
    else:
        for e in range(NE):
            for nm in ("w_gate", "w_up", "w_down"):
                b = C.wbuf[(nm, l, e)]
                P.dma("pool", C.wb[nm][l, e], C.din[nm][l, e], b, writes=[b])


def phase_mod(C, l):
    P, nc = C.P, C.nc
    with ExitStack() as st:
        cs = _sb(st, nc, "m_cs", [128, 8, 2], F32)
        wm = [_sb(st, nc, "m_wm%d" % i, [128, 8, 512], F32) for i in range(2)]
        brow = _sb(st, nc, "m_brow", [2, 6144], F32)
        mrow = _sb(st, nc, "m_mrow", [2, 6144], F32)
        ps_r = [_ps(st, nc, "m_psr%d" % i, [2, 512], F32) for i in range(2)]
        ps_t = _ps(st, nc, "m_pst", [128, 96], F32)
        ps_b = [_ps(st, nc, "m_psb%d" % i, [128, 512], F32) for i in range(2)]
        b_cs, b_brow, b_mrow, b_pst = Buf("cs"), Buf("brow"), Buf("mrow"), Buf("pst")
        b_wm = [Buf("wm0"), Buf("wm1")]
        b_psr = [Buf("psr0"), Buf("psr1")]
        b_psb = [Buf("psb0"), Buf("psb1")]
        P.dma("sp", cs[:, :, 0], C.din["c"].rearrange("(k p) -> p k", p=128), b_cs, writes=[b_cs],
              allow_slow_non_contiguous=True)
        P.dma("sp", cs[:, :, 1], C.din["c_ctx"].rearrange("(k p) -> p k", p=128), b_cs, writes=[b_cs],
              allow_slow_non_contiguous=True)
        P.op("act", I("activation", out=cs[:], in_=cs[:], func=AF.Silu), reads=[b_cs], writes=[b_cs])
        for r in range(2):
            P.dma("sp", brow[r:r + 1, :], C.din["b_mod"][l:l + 1, :], b_brow, writes=[b_brow])
        wsrc = C.din["w_mod"][l].rearrange("(k p) c -> p k c", p=128)
        for cb in range(12):
            i = cb % 2
            P.dma("sp", wm[i][:], wsrc[:, :, cb * 512:(cb + 1) * 512], b_wm[i], writes=[b_wm[i]])
            for k in range(8):
                P.mm(I("matmul", ps_r[i][:], lhsT=cs[:, k, :], rhs=wm[i][:, k, :],
                                                 start=(k == 0), stop=(k == 7)),
                     reads=[b_cs, b_wm[i]], writes=[b_psr[i]])
            P.op("dve", I("tensor_tensor", out=mrow[:, cb * 512:(cb + 1) * 512], in0=ps_r[i][:],
                                                             in1=brow[:, cb * 512:(cb + 1) * 512], op=ALU.add),
                 reads=[b_psr[i], b_brow], writes=[b_mrow])
        P.dma("sp", C.mrow_d[l], mrow[:], b_mrow, reads=[b_mrow], writes=[C.b_mrow_d])
        for j in range(48):
            P.mm(I("transpose", ps_t[:, 2 * j:2 * j + 2], mrow[:, j * 128:(j + 1) * 128], C.ident[0:2, 0:2]),
                 reads=[b_mrow], writes=[b_pst])
        P.op("dve", I("tensor_copy", out=C.modT[:].rearrange("p j r -> p (j r)"), in_=ps_t[:]),
             reads=[b_pst], writes=[C.b_modT])
        P.dma("sp", C.nrm[:, 0, :], C.din["norm_attn"][l].rearrange("(k p) -> p k", p=128), C.b_nrm, writes=[C.b_nrm],
              allow_slow_non_contiguous=True)
        P.dma("sp", C.nrm[:, 1, :], C.din["norm_ffn"][l].rearrange("(k p) -> p k", p=128), C.b_nrm, writes=[C.b_nrm],
              allow_slow_non_contiguous=True)
        for r in range(2):
            for which, j0 in ((0, 8), (1, 32)):
                P.op("dve", I("scalar_tensor_tensor",
                    out=C.gcol[:, which, r, :], in0=C.modT[:, j0:j0 + 8, r], scalar=1.0, in1=C.nrm[:, which, :],
                    op0=ALU.add, op1=ALU.mult),
                    reads=[C.b_modT, C.b_nrm], writes=[C.b_gcol])
        for which, c0 in ((0, 2048), (1, 5120)):
            for r in range(2):
                for hf in range(2):
                    i = (r * 2 + hf) % 2
                    P.mm(I("matmul",
                        ps_b[i][:], lhsT=C.sel2[0:2, r, :], rhs=mrow[:, c0 + hf * 512:c0 + (hf + 1) * 512],
                        start=True, stop=True), reads=[b_mrow], writes=[b_psb[i]])
                    P.op("act", I("activation",
                        out=C.gate_bc[which][:, r, hf * 512:(hf + 1) * 512], in_=ps_b[i][:], func=AF.Copy),
                        reads=[b_psb[i]], writes=[C.b_gate[which]])
        P.barrier()
        P.end_phase()


def norm_mod_T(C, T, x_sb, b_x, nblk, which, r, out_b=None, b_out_b=None, want_b=True):
    P = C.P
    dst_t = T["hT_f"] if out_b is None else out_b
    b_dst = T["b_hT_f"] if out_b is None else b_out_b
    ss, rstd, xn = T["ss"], T["rstd"], T["xn"]
    junk = T["junk"]
    for b in range(nblk):
        P.op("act", I("activation", out=junk[:], in_=x_sb[:, b, :], func=AF.Square,
                                                accum_out=ss[:, b:b + 1]),
             reads=[b_x], writes=[T["b_junk"], T["b_ss"]])
    P.op("act", I("activation", out=rstd[:, 0:nblk], in_=ss[:, 0:nblk], func=AF.Ln, scale=1.0 / D, bias=C.eps_col[:]),
         reads=[T["b_ss"]], writes=[T["b_rstd"]])
    P.op("act", I("activation", out=rstd[:, 0:nblk], in_=rstd[:, 0:nblk], func=AF.Exp, scale=-0.5),
         reads=[T["b_rstd"]], writes=[T["b_rstd"]])
    for b in range(nblk):
        P.op("dve", I("tensor_scalar", out=xn[:, b, :], in0=x_sb[:, b, :], scalar1=rstd[:, b:b + 1],
                                                   scalar2=None, op0=ALU.mult),
             reads=[b_x, T["b_rstd"]], writes=[T["b_xn"]])
    ntok = nblk * 128
    for hf in range(2):
        for cc in range(4):
            c = hf * 4 + cc
            pt = T["ps_tr"][cc]
            for b in range(nblk):
                P.mm(I("transpose", pt[:, b * 128:(b + 1) * 128], xn[:, b, c * 128:(c + 1) * 128],
                                                            C.ident[:]),
                     reads=[T["b_xn"]], writes=[T["b_ps_tr"][cc]])
            eng = "dve" if cc % 2 == 0 else "act"
            if eng == "dve":
                P.op("dve", I("tensor_scalar",
                    out=dst_t[:, c, 0:ntok], in0=pt[:, 0:ntok], scalar1=C.gcol[:, which, r, c:c + 1],
                    scalar2=C.modT[:, (0 if which == 0 else 24) + c, r:r + 1], op0=ALU.mult, op1=ALU.add),
                    reads=[T["b_ps_tr"][cc], C.b_gcol, C.b_modT], writes=[b_dst])
            else:
                P.op("act", I("activation",
                    out=dst_t[:, c, 0:ntok], in_=pt[:, 0:ntok], func=AF.Identity,
                    scale=C.gcol[:, which, r, c:c + 1], bias=C.modT[:, (0 if which == 0 else 24) + c, r:r + 1]),
                    reads=[T["b_ps_tr"][cc], C.b_gcol, C.b_modT], writes=[b_dst])
    if out_b is None and want_b:
        P.op("pool", I("tensor_copy", out=T["hT_b"][:, :, 0:ntok], in_=T["hT_f"][:, :, 0:ntok]),
             reads=[T["b_hT_f"]], writes=[T["b_hT_b"]])


def alloc_norm_tiles(C, st, pfx, with_out=True):
    nc = C.nc
    T = {}
    T["ss"] = _sb(st, nc, pfx + "ss", [128, 4], F32)
    T["rstd"] = _sb(st, nc, pfx + "rstd", [128, 4], F32)
    T["xn"] = _sb(st, nc, pfx + "xn", [128, 4, 1024], F32)
    T["junk"] = _sb(st, nc, pfx + "junk", [128, 1024], BF16)
    if with_out:
        T["hT_f"] = _sb(st, nc, pfx + "hTf", [128, 8, 512], F32)
        T["hT_b"] = _sb(st, nc, pfx + "hTb", [128, 8, 512], BF16)
    T["ps_tr"] = [_ps(st, nc, pfx + "pstr%d" % i, [128, 512], F32) for i in range(4)]
    for k in ("ss", "rstd", "xn", "junk", "hT_f", "hT_b"):
        T["b_" + k] = Buf(pfx + k)
    T["b_ps_tr"] = [Buf(pfx + "pstr%d" % i) for i in range(4)]
    return T


def x_rows(C, l, t0, n):
    if l == 0:
        if t0 < CT:
            return C.din["ctx"][t0:t0 + n, :]
        return C.din["x"][t0 - CT:t0 - CT + n, :]
    return C.xs[t0:t0 + n, :]


def tiles_of(l):
    tl = [(0, 2, 1)]
    for i in range(SEQ // 512):
        tl.append((CT + i * 512, 4, 0))
    return tl


def phase_proj(C, l):
    P, nc = C.P, C.nc
    with ExitStack() as st:
        T = alloc_norm_tiles(C, st, "p_", with_out=False)
        hTb = [_sb(st, nc, "p_hTb%d" % i, [128, 8, 512], BF16) for i in range(2)]
        b_hTb = [Buf("p_hTb0"), Buf("p_hTb1")]
        w_in = _sb(st, nc, "p_win", [128, 8, WIN_COLS], BF16)
        w_qu = _sb(st, nc, "p_wqu", [128, 3, 1024], BF16)
        w_kvu = _sb(st, nc, "p_wkvu", [128, 2, 1024], BF16)
        qn_col = _sb(st, nc, "p_qn", [128, 3], F32)
        kvn_col = _sb(st, nc, "p_kvn", [128, 2], F32)
        xt = [_sb(st, nc, "p_x%d" % i, [128, 4, 1024], F32) for i in range(2)]
        rope = [_sb(st, nc, "p_rope%d" % i, [128, 4, 512], F32) for i in range(2)]
        ev = [_sb(st, nc, "p_ev%d" % i, [128, 512], F32) for i in range(4)]
        ob = [_sb(st, nc, "p_ob%d" % i, [128, 512], BF16) for i in range(6)]
        cq_f = _sb(st, nc, "p_cqf", [128, 5, 512], F32)
        cq_sq = _sb(st, nc, "p_cqsq", [128, 5, 512], BF16)
        cq_n = _sb(st, nc, "p_cqn", [128, 5, 512], BF16)
        rq = _sb(st, nc, "p_rq", [128, 2, 512], F32)
        ps = [_ps(st, nc, "p_ps%d" % i, [128, 512], F32) for i in range(4)]
        b_w = Buf("p_w")
        b_xt = [Buf("p_x0"), Buf("p_x1")]
        b_rope = [Buf("p_rope0"), Buf("p_rope1")]
        b_ev = [Buf("p_ev%d" % i) for i in range(4)]
        b_ob = [Buf("p_ob%d" % i) for i in range(6)]
        b_cqf, b_cqsq, b_cqn, b_rq = Buf("cqf"), Buf("cqsq"), Buf("cqn"), Buf("rq")
        b_ps = [Buf("p_ps%d" % i) for i in range(4)]
        cnt = {"ps": 0, "ev": 0, "ob": 0}

        def nxt(k, n):
            i = cnt[k] % n
            cnt[k] += 1
            return i

        P.dma("sp", w_in[:], C.wb["w_in"][l].rearrange("(k p) c -> p k c", p=128), b_w,
              reads=[C.wbuf[("w_in", l)]], writes=[b_w])
        P.dma("sp", w_qu[:], C.wb["w_qu"][l].rearrange("(k p) c -> p k c", p=128), b_w,
              reads=[C.wbuf[("w_qu", l)]], writes=[b_w])
        P.dma("sp", w_kvu[:], C.wb["w_kvu"][l].rearrange("(k p) c -> p k c", p=128), b_w,
              reads=[C.wbuf[("w_kvu", l)]], writes=[b_w])
        P.dma("sp", qn_col[:], C.din["q_norm"][l].rearrange("(k p) -> p k", p=128), b_w, writes=[b_w],
              allow_slow_non_contiguous=True)
        P.dma("sp", kvn_col[:], C.din["kv_norm"][l].rearrange("(k p) -> p k", p=128), b_w, writes=[b_w],
              allow_slow_non_contiguous=True)

        tl = tiles_of(l)

        def loadx(ti):
            t0, nb, r = tl[ti]
            i = ti % 2
            P.dma("sp", xt[i][:, 0:nb, :], x_rows(C, l, t0, nb * 128).rearrange("(b p) d -> p b d", p=128), b_xt[i],
                  reads=([C.b_xs] if l > 0 else []), writes=[b_xt[i]])

        def loadr(ti):
            t0, nb, r = tl[ti]
            i = ti % 2
            P.dma("sp", rope[i][:, :, 0:nb * 128], C.din["rope"][:, :, t0:t0 + nb * 128], b_rope[i], writes=[b_rope[i]])

        def front(ti):
            t0, nb, r = tl[ti]
            i = ti % 2
            norm_mod_T(C, T, xt[i], b_xt[i], nb, 0, r, out_b=hTb[i], b_out_b=b_hTb[i])

        loadx(0)
        loadx(1)
        loadr(0)
        front(0)
        for ti, (t0, nb, r) in enumerate(tl):
            if ti + 1 < len(tl):
                loadr(ti + 1)
                front(ti + 1)
            if ti + 2 < len(tl):
                loadx(ti + 2)
            i = ti % 2
            n = nb * 128
            hT = hTb[i]
            b_h = b_hTb[i]
            rp = rope[i]
            b_rp = b_rope[i]

            def proj(c0, m, pi):
                for k in range(8):
                    P.mm(I("matmul", ps[pi][0:m, 0:n], lhsT=w_in[:, k, c0:c0 + m], rhs=hT[:, k, 0:n],
                                                 start=(k == 0), stop=(k == 7)),
                         reads=[b_w, b_h], writes=[b_ps[pi]])

            def roped(c0, c0s, m, tab, store):
                pa, pb = nxt("ps", 4), nxt("ps", 4)
                proj(c0, m, pa)
                proj(c0s, m, pb)
                ea, eb, o = nxt("ev", 4), nxt("ev", 4), nxt("ob", 6)
                P.op("dve", I("tensor_tensor", out=ev[ea][0:m, 0:n], in0=ps[pa][0:m, 0:n],
                                                      in1=rp[0:m, tab, 0:n], op=ALU.mult),
                     reads=[b_ps[pa], b_rp], writes=[b_ev[ea]])
                P.op("dve", I("tensor_tensor", out=ev[eb][0:m, 0:n], in0=ps[pb][0:m, 0:n],
                                                      in1=rp[0:m, tab + 1, 0:n], op=ALU.mult),
                     reads=[b_ps[pb], b_rp], writes=[b_ev[eb]])
                P.op("pool", I("tensor_tensor", out=ob[o][0:m, 0:n], in0=ev[ea][0:m, 0:n],
                                                       in1=ev[eb][0:m, 0:n], op=ALU.add),
                     reads=[b_ev[ea], b_ev[eb]], writes=[b_ob[o]])
                store(ob[o], b_ob[o])

            for c in range(4):
                def st_q(o, bo, c=c):
                    for hh in range(2):
                        hq = 2 * c + hh
                        g, j = hq // 4, hq % 4
                        dst = C.QaT[g, :, t0 // 128:t0 // 128 + nb, j, :]
                        P.dma("sp", dst, o[hh * 64:(hh + 1) * 64, 0:n].rearrange("p (b q) -> p b q", q=128),
                              bo, reads=[bo], writes=[C.b_QaT])
                roped(C_QA + c * 128, C_QAS + c * 128, 128, 0, st_q)
            def st_k(o, bo):
                P.dma("sp", C.KaT[:, t0:t0 + n], o[:, 0:n], bo, reads=[bo], writes=[C.b_KaT])
            roped(C_KA, C_KAS, 128, 0, st_k)
            def st_kr(o, bo):
                for hh in range(8):
                    P.dma("sp", C.KbT[hh, 64:96, t0:t0 + n], o[0:32, 0:n], bo, reads=[bo], writes=[C.b_KbT])
            roped(C_KR, C_KRS, 32, 2, st_kr)
            pv = nxt("ps", 4)
            for b in range(nb):
                for k in range(8):
                    P.mm(I("matmul", ps[pv][:, b * 128:(b + 1) * 128], lhsT=hT[:, k, b * 128:(b + 1) * 128],
                                                      rhs=w_in[:, k, C_VA:C_VA + 128], start=(k == 0), stop=(k == 7)),
                         reads=[b_w, b_h], writes=[b_ps[pv]])
            o = nxt("ob", 6)
            P.op("act", I("activation", out=ob[o][:, 0:n], in_=ps[pv][:, 0:n], func=AF.Copy),
                 reads=[b_ps[pv]], writes=[b_ob[o]])
            P.dma("sp", C.Va[t0:t0 + n, :].rearrange("(b p) d -> p b d", p=128),
                  ob[o][:, 0:n].rearrange("p (b d) -> p b d", d=128), b_ob[o], reads=[b_ob[o]], writes=[C.b_Va])
            for c in range(5):
                pi = nxt("ps", 4)
                proj(C_CQ + c * 128, 128, pi)
                P.op("act", I("activation", out=cq_f[:, c, 0:n], in_=ps[pi][:, 0:n], func=AF.Copy),
                     reads=[b_ps[pi]], writes=[b_cqf])
                P.op("act", I("activation", out=cq_sq[:, c, 0:n], in_=ps[pi][:, 0:n], func=AF.Square),
                     reads=[b_ps[pi]], writes=[b_cqsq])
            for (grp, c0, nch, ones) in ((0, 0, 3, C.ones_q), (1, 3, 2, C.ones_kv)):
                pi = nxt("ps", 4)
                for c in range(nch):
                    P.mm(I("matmul", ps[pi][:, 0:n], lhsT=ones[:], rhs=cq_sq[:, c0 + c, 0:n],
                                                 start=(c == 0), stop=(c == nch - 1)),
                         reads=[b_cqsq], writes=[b_ps[pi]])
                P.op("act", I("activation", out=rq[:, grp, 0:n], in_=ps[pi][:, 0:n], func=AF.Ln,
                                                                   bias=C.eps_col[:]),
                     reads=[b_ps[pi]], writes=[b_rq])
                P.op("act", I("activation", out=rq[:, grp, 0:n], in_=rq[:, grp, 0:n], func=AF.Exp, scale=-0.5),
                     reads=[b_rq], writes=[b_rq])
                for c in range(nch):
                    col = qn_col[:, c:c + 1] if grp == 0 else kvn_col[:, c:c + 1]
                    P.op("dve", I("scalar_tensor_tensor",
                        out=cq_n[:, c0 + c, 0:n], in0=cq_f[:, c0 + c, 0:n], scalar=col, in1=rq[:, grp, 0:n],
                        op0=ALU.mult, op1=ALU.mult), reads=[b_cqf, b_rq, b_w], writes=[b_cqn])
            for c in range(4):
                pi = nxt("ps", 4)
                for k in range(3):
                    P.mm(I("matmul", ps[pi][:, 0:n], lhsT=w_qu[:, k, c * 128:(c + 1) * 128],
                                                      rhs=cq_n[:, k, 0:n], start=(k == 0), stop=(k == 2)),
                         reads=[b_w, b_cqn], writes=[b_ps[pi]])
                o = nxt("ob", 6)
                P.op("act", I("activation", out=ob[o][:, 0:n], in_=ps[pi][:, 0:n], func=AF.Copy),
                     reads=[b_ps[pi]], writes=[b_ob[o]])
                for hh in range(2):
                    P.dma("sp", C.QbT[2 * c + hh, 0:64, t0:t0 + n], ob[o][hh * 64:(hh + 1) * 64, 0:n], b_ob[o],
                          reads=[b_ob[o]], writes=[C.b_QbT])
            for c in range(2):
                pa, pb = nxt("ps", 4), nxt("ps", 4)
                for (pi, cc0) in ((pa, 512), (pb, 768)):
                    for k in range(3):
                        P.mm(I("matmul", ps[pi][:, 0:n], lhsT=w_qu[:, k, cc0 + c * 128:cc0 + (c + 1) * 128],
                                                                     rhs=cq_n[:, k, 0:n], start=(k == 0), stop=(k == 2)),
                             reads=[b_w, b_cqn], writes=[b_ps[pi]])
                ea, eb, o = nxt("ev", 4), nxt("ev", 4), nxt("ob", 6)
                P.op("dve", I("tensor_tensor", out=ev[ea][:, 0:n], in0=ps[pa][:, 0:n], in1=rp[:, 2, 0:n], op=ALU.mult),
                     reads=[b_ps[pa], b_rp], writes=[b_ev[ea]])
                P.op("dve", I("tensor_tensor", out=ev[eb][:, 0:n], in0=ps[pb][:, 0:n], in1=rp[:, 3, 0:n], op=ALU.mult),
                     reads=[b_ps[pb], b_rp], writes=[b_ev[eb]])
                P.op("pool", I("tensor_tensor", out=ob[o][:, 0:n], in0=ev[ea][:, 0:n], in1=ev[eb][:, 0:n], op=ALU.add),
                     reads=[b_ev[ea], b_ev[eb]], writes=[b_ob[o]])
                for hh in range(4):
                    P.dma("sp", C.QbT[4 * c + hh, 64:96, t0:t0 + n], ob[o][hh * 32:(hh + 1) * 32, 0:n], b_ob[o],
                          reads=[b_ob[o]], writes=[C.b_QbT])
            for c in range(4):
                pi = nxt("ps", 4)
                for k in range(2):
                    P.mm(I("matmul", ps[pi][:, 0:n], lhsT=w_kvu[:, k, c * 128:(c + 1) * 128],
                                                      rhs=cq_n[:, 3 + k, 0:n], start=(k == 0), stop=(k == 1)),
                         reads=[b_w, b_cqn], writes=[b_ps[pi]])
                o = nxt("ob", 6)
                P.op("act", I("activation", out=ob[o][:, 0:n], in_=ps[pi][:, 0:n], func=AF.Copy),
                     reads=[b_ps[pi]], writes=[b_ob[o]])
                for hh in range(2):
                    P.dma("sp", C.KbT[2 * c + hh, 0:64, t0:t0 + n], ob[o][hh * 64:(hh + 1) * 64, 0:n], b_ob[o],
                          reads=[b_ob[o]], writes=[C.b_KbT])
            for b in range(nb):
                pi = nxt("ps", 4)
                for k in range(2):
                    P.mm(I("matmul", ps[pi][:, :], lhsT=cq_n[:, 3 + k, b * 128:(b + 1) * 128],
                                                      rhs=w_kvu[:, k, 512:1024], start=(k == 0), stop=(k == 1)),
                         reads=[b_w, b_cqn], writes=[b_ps[pi]])
                o = nxt("ob", 6)
                P.op("dve", I("tensor_copy", out=ob[o][:, :], in_=ps[pi][:, :]),
                     reads=[b_ps[pi]], writes=[b_ob[o]])
                P.dma("sp", C.Vb[t0 + b * 128:t0 + (b + 1) * 128, :], ob[o][:, :], b_ob[o], reads=[b_ob[o]], writes=[C.b_Vb])
        P.barrier()
        P.end_phase()


def phase_attn(C, l):
    P, nc = C.P, C.nc
    LA = 3
    with ExitStack() as st:
        kT = [_sb(st, nc, "a_kT%d" % i, [128, NT], BF16) for i in range(2)]
        vv = [_sb(st, nc, "a_v%d" % i, [128, NBLK, 128], BF16) for i in range(2)]
        qq = [_sb(st, nc, "a_q%d" % i, [128, 512], BF16) for i in range(3)]
        pt = [_sb(st, nc, "a_pt%d" % i, [128, 512], BF16) for i in range(4)]
        rc = [_sb(st, nc, "a_rc%d" % i, [128, 512], F32) for i in range(2)]
        bcs = _sb(st, nc, "a_bcs", [128, 512], F32)
        obf = [_sb(st, nc, "a_obf%d" % i, [128, 512], BF16) for i in range(2)]
        mask = _sb(st, nc, "a_mask", [128, 2, 4, 128], BF16)
        maskf = _sb(st, nc, "a_maskf", [128, 256], F32)
        es8 = _sb(st, nc, "a_es8", [128, 8], F32)
        esink = _sb(st, nc, "a_esink", [128, 8, 128], F32)
        den = _sb(st, nc, "a_den", [128, 512], F32)
        b_den = Buf("den")
        ones_r = _sb(st, nc, "a_ones", [128, 64], F32)
        ps_s = [_ps(st, nc, "a_pss%d" % i, [128, 512], F32) for i in range(4)]
        ps_o = [_ps(st, nc, "a_pso%d" % i, [128, 512], F32) for i in range(2)]
        ps_b = _ps(st, nc, "a_psb", [128, 512], F32)
        b_kT = [Buf("kT0"), Buf("kT1")]
        b_v = [Buf("v0"), Buf("v1")]
        b_q = [Buf("q%d" % i) for i in range(3)]
        b_pt = [Buf("pt%d" % i) for i in range(4)]
        b_bcs, b_cst, b_psb = Buf("bcs"), Buf("acst"), Buf("psb")
        b_rc = [Buf("rc0"), Buf("rc1")]
        b_obf = [Buf("obf0"), Buf("obf1")]
        b_pss = [Buf("pss%d" % i) for i in range(4)]
        b_pso = [Buf("pso0"), Buf("pso1")]
        cnt = {"ob": 0}

        P.dma("sp", maskf[:], C.din["cst"][:, 128:384], b_cst, writes=[b_cst])
        for m in range(2):
            P.op("dve", I("tensor_copy", out=mask[:, m, :, :],
                          in_=maskf[:, m * 128:(m + 1) * 128].rearrange("p (o q) -> p o q", o=1).to_broadcast([128, 4, 128])),
                 reads=[b_cst], writes=[b_cst])
        P.dma("sp", es8[64:65, :], C.din["sink"][l:l + 1, :], b_cst, writes=[b_cst])
        P.op("act", I("activation", out=es8[64:65, :], in_=es8[64:65, :], func=AF.Exp), reads=[b_cst], writes=[b_cst])
        P.op("dve", I("tensor_copy", out=esink[64:65, :, :],
                      in_=es8[64:65, :].rearrange("p (e o) -> p e o", o=1).to_broadcast([1, 8, 128])),
             reads=[b_cst], writes=[b_cst])
        P.op("dve", I("memset", ones_r[:], 1.0), writes=[b_cst])
        for i in range(2):
            P.op("pool", I("memset", vv[i][:, :, 64:128], 0.0), writes=[b_v[i]])
            P.op("pool", I("memset", vv[i][:, :, 64:65], 1.0), writes=[b_v[i]])
        for i in range(2):
            P.op("pool", I("memset", kT[i][64:128, :], 0.0), writes=[b_kT[i]])
        for i in range(3):
            P.op("pool", I("memset", qq[i][64:128, :], 0.0), writes=[b_q[i]])
        phase_cast(C, l, "experts")
        if l + 1 < DEPTH:
            phase_cast(C, l + 1, "small")

        pending = []

        def finish(po, n, dst_fn, sink_g=None):
            flush()
            ri = cnt["ob"] % 2
            if sink_g is None:
                P.op("dve", I("reciprocal", out=rc[ri][64:65, 0:n], in_=ps_o[po][64:65, 0:n]), reads=[b_pso[po]], writes=[b_rc[ri]])
            else:
                P.op("dve", I("tensor_tensor", out=den[64:65, :], in0=ps_o[po][64:65, :],
                              in1=esink[64:65, sink_g * 4:(sink_g + 1) * 4, :].rearrange("p e q -> p (e q)"), op=ALU.add),
                     reads=[b_pso[po], b_cst], writes=[b_den])
                P.op("dve", I("reciprocal", out=rc[ri][64:65, 0:n], in_=den[64:65, 0:n]), reads=[b_den], writes=[b_rc[ri]])
            pending.append((po, n, dst_fn, ri))

        def flush():
            while pending:
                finish_b(*pending.pop(0))

        def finish_b(po, n, dst_fn, ri):
            P.mm(I("matmul", ps_b[0:64, 0:n], lhsT=ones_r[64:65, 0:64], rhs=rc[ri][64:65, 0:n], start=True, stop=True),
                 reads=[b_rc[ri], b_cst], writes=[b_psb])
            P.op("dve", I("tensor_copy", out=bcs[0:64, 0:n], in_=ps_b[0:64, 0:n]), reads=[b_psb], writes=[b_bcs])
            o = cnt["ob"] % 2
            cnt["ob"] += 1
            P.op("dve", I("tensor_tensor", out=obf[o][0:64, 0:n], in0=ps_o[po][0:64, 0:n], in1=bcs[0:64, 0:n], op=ALU.mult),
                 reads=[b_pso[po], b_bcs], writes=[b_obf[o]])
            dst_fn(obf[o], b_obf[o])

        srcs = [("w", g) for g in range(2)] + [("m", hh) for hh in range(8)]
        groups = []
        for si, (kind, idx) in enumerate(srcs):
            if kind == "w":
                for qb in range(0 if l == 0 else 2, NBLK):
                    if qb < 2:
                        kbs = [(0, None), (1, None)]
                    else:
                        kbs = [(0, None), (1, None)]
                        if qb - 1 >= 2:
                            kbs.append((qb - 1, 0))
                        kbs.append((qb, None))
                        if qb + 1 < NBLK:
                            kbs.append((qb + 1, 1))
                    groups.append(dict(si=si, kind="w", idx=idx, qb=qb, n=512, kbs=kbs))
            else:
                for (t0, nb, r) in tiles_of(l):
                    if l > 0 and r == 1:
                        continue
                    nkb = 2 if r == 1 else NBLK
                    groups.append(dict(si=si, kind="m", idx=idx, t0=t0, n=nb * 128, kbs=[(kb, None) for kb in range(nkb)]))

        def load_src(si):
            kind, idx = srcs[si]
            i = si % 2
            if kind == "w":
                P.dma("sp", kT[i][0:64, :], C.KaT[idx * 64:(idx + 1) * 64, :], b_kT[i], reads=[C.b_KaT], writes=[b_kT[i]])
                P.dma("sp", vv[i][:, :, 0:64], C.Va.rearrange("(b p) c -> p b c", p=128)[:, :, idx * 64:(idx + 1) * 64], b_v[i],
                      reads=[C.b_Va], writes=[b_v[i]])
            else:
                P.dma("sp", kT[i][0:96, :], C.KbT[idx], b_kT[i], reads=[C.b_KbT], writes=[b_kT[i]])
                P.dma("sp", vv[i][:, :, 0:64], C.Vb.rearrange("(b p) c -> p b c", p=128)[:, :, idx * 64:(idx + 1) * 64], b_v[i],
                      reads=[C.b_Vb], writes=[b_v[i]])

        def load_q(gi):
            G = groups[gi]
            qi = gi % 3
            if G["kind"] == "w":
                P.dma("sp", qq[qi][0:64, :], C.QaT[G["idx"], :, G["qb"], :, :].rearrange("d j q -> d (j q)"), b_q[qi],
                      reads=[C.b_QaT], writes=[b_q[qi]])
            else:
                P.dma("sp", qq[qi][0:96, 0:G["n"]], C.QbT[G["idx"], :, G["t0"]:G["t0"] + G["n"]], b_q[qi],
                      reads=[C.b_QbT], writes=[b_q[qi]])

        steps = []
        for gi, G in enumerate(groups):
            for ki, (kb, mk) in enumerate(G["kbs"]):
                steps.append((gi, ki, kb, mk))

        def stage_A(sidx):
            gi, ki, kb, mk = steps[sidx]
            G = groups[gi]
            if ki == 0:
                if gi == 0:
                    load_src(0)
                    load_q(0)
                if gi + 1 < len(groups):
                    load_q(gi + 1)
            i = G["si"] % 2
            qi = gi % 3
            kd = 128 if G["kind"] == "w" else 96
            n = G["n"]
            s = sidx % 4
            P.mm(I("matmul", ps_s[s][:, 0:n], lhsT=kT[i][0:kd, kb * 128:(kb + 1) * 128], rhs=qq[qi][0:kd, 0:n],
                   start=True, stop=True), reads=[b_kT[i], b_q[qi]], writes=[b_pss[s]])
            p = sidx % 4
            scale = 0.125 if G["kind"] == "w" else MLA_SCALE
            P.op("act", I("activation", out=pt[p][:, 0:n], in_=ps_s[s][:, 0:n], func=AF.Exp, scale=scale),
                 reads=[b_pss[s]], writes=[b_pt[p]])
            if mk is not None:
                P.op("dve", I("tensor_tensor", out=pt[p][:, :], in0=pt[p][:, :],
                              in1=mask[:, mk, :, :].rearrange("p j q -> p (j q)"), op=ALU.mult),
                     reads=[b_pt[p], b_cst], writes=[b_pt[p]])

        def stage_C(sidx):
            gi, ki, kb, mk = steps[sidx]
            G = groups[gi]
            i = G["si"] % 2
            n = G["n"]
            po = gi % 2
            p = sidx % 4
            if ki == 0 and (gi == 0 or groups[gi - 1]["si"] != G["si"]) and G["si"] + 1 < len(srcs):
                load_src(G["si"] + 1)
            first = (ki == 0)
            lastk = (ki == len(G["kbs"]) - 1)
            P.mm(I("matmul", ps_o[po][:, 0:n], lhsT=vv[i][:, kb, :], rhs=pt[p][:, 0:n], start=first, stop=lastk),
                 reads=[b_v[i], b_pt[p]], writes=[b_pso[po]])
            if lastk:
                if G["kind"] == "w":
                    g, qb = G["idx"], G["qb"]

                    def dst_w(o, bo):
                        dst = C.OT[g * 256:(g + 1) * 256, qb * 128:(qb + 1) * 128].rearrange("(j d) q -> d j q", d=64)
                        P.dma("sp", dst, o[0:64, :].rearrange("p (j q) -> p j q", q=128), bo, reads=[bo], writes=[C.b_OT])
                    finish(po, 512, dst_w, sink_g=g)
                else:
                    hh, t0 = G["idx"], G["t0"]

                    def dst_m(o, bo):
                        P.dma("sp", C.OT[512 + hh * 64:512 + (hh + 1) * 64, t0:t0 + n], o[0:64, 0:n], bo, reads=[bo], writes=[C.b_OT])
                    finish(po, n, dst_m)

        ns = len(steps)
        for i in range(ns + LA):
            if i < ns:
                stage_A(i)
            if i - LA >= 0:
                stage_C(i - LA)
        flush()
        P.barrier()
        P.end_phase()


def phase_oproj(C, l):
    P, nc = C.P, C.nc
    last = (l == DEPTH - 1)
    with ExitStack() as st:
        T = alloc_norm_tiles(C, st, "o_")
        w_out = _sb(st, nc, "o_wout", [128, 8, 1024], BF16)
        w_r = _sb(st, nc, "o_wr", [128, 8, NE], F32)
        ot = [_sb(st, nc, "o_ot%d" % i, [128, 8, 512], BF16) for i in range(2)]
        xt = [_sb(st, nc, "o_x%d" % i, [128, 4, 1024], F32) for i in range(2)]
        tmp = [_sb(st, nc, "o_tmp%d" % i, [128, 512], F32) for i in range(2)]
        lg = _sb(st, nc, "o_lg", [128, 4, NE], F32)
        mx = _sb(st, nc, "o_mx", [128, 4], F32)
        sm = _sb(st, nc, "o_sm", [128, 4], F32)
        af = [_sb(st, nc, "o_af%d" % i, [128, 4, NE], F32) for i in range(2)]
        ps = [_ps(st, nc, "o_ps%d" % i, [128, 512], F32) for i in range(2)]
        ps_r = _ps(st, nc, "o_psr", [128, 4, NE], F32)
        b_w = Buf("o_w")
        b_ot = [Buf("o_ot0"), Buf("o_ot1")]
        b_xt = [Buf("o_x0"), Buf("o_x1")]
        b_tmp = [Buf("o_tmp0"), Buf("o_tmp1")]
        b_lg, b_mx, b_sm, b_psr = Buf("lg"), Buf("mx"), Buf("sm"), Buf("psr")
        b_af = [Buf("af0"), Buf("af1")]
        b_ps = [Buf("o_ps0"), Buf("o_ps1")]
        cnt = {"ps": 0, "tmp": 0}

        def nxt(k, n):
            i = cnt[k] % n
            cnt[k] += 1
            return i
        P.dma("sp", w_out[:], C.wb["w_out"][l].rearrange("(k p) c -> p k c", p=128), b_w,
              reads=[C.wbuf[("w_out", l)]], writes=[b_w])
        P.dma("sp", w_r[:], C.din["w_router"][l].rearrange("(k p) e -> p k e", p=128), b_w, writes=[b_w])
        rowF = _sb(st, nc, "o_rowF", [128, 2, 2, 1024], F32)
        nfb = _sb(st, nc, "o_nfb", [128, 1024], F32)
        h2t = [_sb(st, nc, "o_h2t%d" % i, [128, 4, 1024], BF16) for i in range(2)]
        b_rowF = Buf("rowF")
        b_h2t = [Buf("h2t0"), Buf("h2t1")]
        P.dma("sp", nfb[:], C.din["norm_ffn"][l:l + 1, :].partition_broadcast(128), b_rowF, writes=[b_rowF])
        for r in range(2):
            P.dma("sp", rowF[:, r, 0, :], C.mrow_d[l, r:r + 1, 4096:5120].partition_broadcast(128), b_rowF,
                  reads=[C.b_mrow_d], writes=[b_rowF])
            P.dma("sp", rowF[:, r, 1, :], C.mrow_d[l, r:r + 1, 3072:4096].partition_broadcast(128), b_rowF,
                  reads=[C.b_mrow_d], writes=[b_rowF])
            P.op("dve", I("scalar_tensor_tensor", out=rowF[:, r, 0, :], in0=rowF[:, r, 0, :], scalar=1.0, in1=nfb[:],
                          op0=ALU.add, op1=ALU.mult), reads=[b_rowF], writes=[b_rowF])
        tl = [t for t in tiles_of(l) if not (last and t[2] == 1)]

        def load(ti):
            t0, nb, r = tl[ti]
            i = ti % 2
            n = nb * 128
            P.dma("sp", ot[i][:, :, 0:n], C.OT.rearrange("(k p) t -> p k t", p=128)[:, :, t0:t0 + n], b_ot[i],
                  reads=[C.b_OT], writes=[b_ot[i]])
            P.dma("sp", xt[i][:, 0:nb, :], x_rows(C, l, t0, n).rearrange("(b p) d -> p b d", p=128), b_xt[i],
                  reads=([C.b_xs] if l > 0 else []), writes=[b_xt[i]])
        load(0)
        for ti, (t0, nb, r) in enumerate(tl):
            if ti + 1 < len(tl):
                load(ti + 1)
            i = ti % 2
            n = nb * 128
            for b in range(nb):
                for hf in range(2):
                    pi = nxt("ps", 2)
                    for k in range(8):
                        P.mm(I("matmul", ps[pi][:, :], lhsT=ot[i][:, k, b * 128:(b + 1) * 128],
                               rhs=w_out[:, k, hf * 512:(hf + 1) * 512], start=(k == 0), stop=(k == 7)),
                             reads=[b_ot[i], b_w], writes=[b_ps[pi]])
                    tp = nxt("tmp", 2)
                    P.op("dve", I("tensor_tensor", out=tmp[tp][:, :], in0=ps[pi][:, :],
                                  in1=C.gate_bc[0][:, r, hf * 512:(hf + 1) * 512], op=ALU.mult),
                         reads=[b_ps[pi], C.b_gate[0]], writes=[b_tmp[tp]])
                    P.op("pool", I("tensor_tensor", out=xt[i][:, b, hf * 512:(hf + 1) * 512], in0=tmp[tp][:, :],
                                   in1=xt[i][:, b, hf * 512:(hf + 1) * 512], op=ALU.add),
                         reads=[b_tmp[tp], b_xt[i]], writes=[b_xt[i]])
            P.dma("sp", C.xs[t0:t0 + n, :].rearrange("(b p) d -> p b d", p=128), xt[i][:, 0:nb, :], b_xt[i],
                  reads=[b_xt[i]], writes=[C.b_xs])
            norm_mod_T(C, T, xt[i], b_xt[i], nb, 1, r, want_b=False)
            for b in range(nb):
                P.op("dve", I("tensor_tensor", out=T["xn"][:, b, :], in0=T["xn"][:, b, :], in1=rowF[:, r, 0, :], op=ALU.mult),
                     reads=[T["b_xn"], b_rowF], writes=[T["b_xn"]])
                P.op("pool" if b % 2 == 0 else "dve", I("tensor_tensor", out=h2t[i][:, b, :], in0=T["xn"][:, b, :], in1=rowF[:, r, 1, :], op=ALU.add),
                     reads=[T["b_xn"], b_rowF], writes=[b_h2t[i]])
            P.dma("sp", C.h2tok[t0:t0 + n, :].rearrange("(b p) d -> p b d", p=128), h2t[i][:, 0:nb, :], b_h2t[i],
                  reads=[b_h2t[i]], writes=[C.b_h2tok])
            for b in range(nb):
                for k in range(8):
                    P.mm(I("matmul", ps_r[:, b, :], lhsT=T["hT_f"][:, k, b * 128:(b + 1) * 128], rhs=w_r[:, k, :],
                           start=(k == 0), stop=(k == 7)), reads=[T["b_hT_f"], b_w], writes=[b_psr])
            a = ti % 2
            P.op("dve", I("tensor_reduce", out=mx[:, 0:nb], in_=ps_r[:, 0:nb, :], axis=AX.X, op=ALU.max),
                 reads=[b_psr], writes=[b_mx])
            P.op("dve", I("tensor_tensor", out=lg[:, 0:nb, :], in0=ps_r[:, 0:nb, :],
                          in1=mx[:, 0:nb].rearrange("p (b o) -> p b o", o=1).to_broadcast([128, nb, NE]), op=ALU.subtract),
                 reads=[b_psr, b_mx], writes=[b_lg])
            P.op("act", I("activation", out=lg[:, 0:nb, :], in_=lg[:, 0:nb, :], func=AF.Exp), reads=[b_lg], writes=[b_lg])
            P.op("dve", I("tensor_reduce", out=sm[:, 0:nb], in_=lg[:, 0:nb, :], axis=AX.X, op=ALU.add),
                 reads=[b_lg], writes=[b_sm])
            P.op("dve", I("reciprocal", out=sm[:, 0:nb], in_=sm[:, 0:nb]), reads=[b_sm], writes=[b_sm])
            P.op("dve", I("tensor_tensor", out=af[a][:, 0:nb, :], in0=lg[:, 0:nb, :],
                          in1=sm[:, 0:nb].rearrange("p (b o) -> p b o", o=1).to_broadcast([128, nb, NE]), op=ALU.mult),
                 reads=[b_lg, b_sm], writes=[b_af[a]])
            P.dma("sp", C.aff[t0:t0 + n, :].rearrange("(b p) e -> p b e", p=128), af[a][:, 0:nb, :], b_af[a],
                  reads=[b_af[a]], writes=[C.b_aff])
        P.barrier()
        P.end_phase()


def phase_thr(C, l):
    P, nc = C.P, C.nc
    last = (l == DEPTH - 1)
    with ExitStack() as st:
        araw = _sb(st, nc, "t_araw", [128, 64, NE], F32)
        craw = _sb(st, nc, "t_craw", [128, 2, NE], F32)
        A = _sb(st, nc, "t_A", [128, 32, 64], F32)
        cmpt = _sb(st, nc, "t_cmp", [128, 32, 64], BF16)
        cntp = _sb(st, nc, "t_cnt", [128, 32], F32)
        ones = _sb(st, nc, "t_ones", [128, 128], F32)
        lo = _sb(st, nc, "t_lo", [128, 32], F32)
        hi = _sb(st, nc, "t_hi", [128, 32], F32)
        mid = _sb(st, nc, "t_mid", [128, 32], F32)
        cap = _sb(st, nc, "t_cap", [128, 32], F32)
        ge = _sb(st, nc, "t_ge", [128, 32], U32)
        lt = _sb(st, nc, "t_lt", [128, 32], U32)
        ps = _ps(st, nc, "t_ps", [128, 32], F32)
        b_araw, b_A, b_cmp, b_cnt, b_c = Buf("araw"), Buf("A"), Buf("cmp"), Buf("cnt"), Buf("tc")
        b_lo, b_hi, b_mid, b_ge, b_lt, b_ps = Buf("lo"), Buf("hi"), Buf("mid"), Buf("ge"), Buf("lt"), Buf("tps")
        P.dma("sp", araw[:], C.aff[CT:NT, :].rearrange("(p j) e -> p j e", p=128), b_araw, reads=[C.b_aff], writes=[b_araw])
        P.op("pool", I("memset", A[:], 0.0), writes=[b_A])
        P.op("dve", I("tensor_copy", out=A[:, 0:16, :], in_=araw[:].rearrange("p j e -> p e j")), reads=[b_araw], writes=[b_A])
        if not last:
            P.dma("sp", craw[:], C.aff[0:CT, :].rearrange("(p j) e -> p j e", p=128), b_araw, reads=[C.b_aff], writes=[b_araw])
            P.op("dve", I("tensor_copy", out=A[:, 16:32, 0:2], in_=craw[:].rearrange("p j e -> p e j")), reads=[b_araw], writes=[b_A])
        P.op("dve", I("memset", ones[:], 1.0), writes=[b_c])
        P.op("dve", I("memset", cap[:, 0:16], float(2 * SEQ // NE)), writes=[b_c])
        P.op("dve", I("memset", cap[:, 16:32], float(2 * CT // NE)), writes=[b_c])
        P.op("dve", I("memset", lo[:], 0.0), writes=[b_lo])
        P.op("dve", I("memset", hi[:], 1.0), writes=[b_hi])
        P.op("dve", I("memset", cntp[:], 0.0), writes=[b_cnt])
        for it in range(31):
            P.op("dve", I("tensor_tensor", out=mid[:], in0=lo[:], in1=hi[:], op=ALU.add), reads=[b_lo, b_hi], writes=[b_mid])
            P.op("dve", I("tensor_scalar", out=mid[:], in0=mid[:], scalar1=0.5, scalar2=None, op0=ALU.mult),
                 reads=[b_mid], writes=[b_mid])
            P.op("dve", I("tensor_tensor", out=cmpt[:, 0:16, :], in0=A[:, 0:16, :],
                          in1=mid[:, 0:16].rearrange("p (e o) -> p e o", o=1).to_broadcast([128, 16, 64]), op=ALU.is_gt),
                 reads=[b_A, b_mid], writes=[b_cmp])
            P.op("dve", I("tensor_reduce", out=cntp[:, 0:16], in_=cmpt[:, 0:16, :], axis=AX.X, op=ALU.add),
                 reads=[b_cmp], writes=[b_cnt])
            if not last:
                P.op("dve", I("tensor_tensor", out=cmpt[:, 16:32, 0:2], in0=A[:, 16:32, 0:2],
                              in1=mid[:, 16:32].rearrange("p (e o) -> p e o", o=1).to_broadcast([128, 16, 2]), op=ALU.is_gt),
                     reads=[b_A, b_mid], writes=[b_cmp])
                P.op("dve", I("tensor_reduce", out=cntp[:, 16:32], in_=cmpt[:, 16:32, 0:2], axis=AX.X, op=ALU.add),
                     reads=[b_cmp], writes=[b_cnt])
            P.mm(I("matmul", ps[:], lhsT=ones[:], rhs=cntp[:], start=True, stop=True), reads=[b_cnt, b_c], writes=[b_ps])
            P.op("dve", I("tensor_tensor", out=ge[:], in0=ps[:], in1=cap[:], op=ALU.is_ge), reads=[b_ps, b_c], writes=[b_ge])
            P.op("dve", I("tensor_tensor", out=lt[:], in0=ps[:], in1=cap[:], op=ALU.is_lt), reads=[b_ps, b_c], writes=[b_lt])
            P.op("dve", I("copy_predicated", lo[:], ge[:], mid[:]), reads=[b_ge, b_mid], writes=[b_lo])
            P.op("dve", I("copy_predicated", hi[:], lt[:], mid[:]), reads=[b_lt, b_mid], writes=[b_hi])
        P.op("dve", I("tensor_copy", out=C.thr[:], in_=lo[:]), reads=[b_lo], writes=[C.b_thr])
        P.barrier()
        P.end_phase()


def phase_moe(C, l):
    P, nc = C.P, C.nc
    last = (l == DEPTH - 1)
    with ExitStack() as st:
        yacc = _sb(st, nc, "e_yacc", [128, 8, 1024], F32)
        wg = [_sb(st, nc, "e_wg%d" % i, [128, 8, DFF], BF16) for i in range(2)]
        wu = [_sb(st, nc, "e_wu%d" % i, [128, 8, DFF], BF16) for i in range(2)]
        wd = [_sb(st, nc, "e_wd%d" % i, [128, 4, 1024], BF16) for i in range(2)]
        hT = [_sb(st, nc, "e_hT%d" % i, [128, 8, 512], BF16) for i in range(2)]
        afr = [_sb(st, nc, "e_af%d" % i, [128, 4, NE], F32) for i in range(2)]
        gt = [_sb(st, nc, "e_gt%d" % i, [128, 4, NE], F32) for i in range(2)]
        sA = [_sb(st, nc, "e_sA%d" % i, [128, 512], F32) for i in range(2)]
        act = [_sb(st, nc, "e_act%d" % i, [128, 4, 512], BF16) for i in range(2)]
        xt = _sb(st, nc, "e_x", [128, 4, 1024], F32)
        nfb = _sb(st, nc, "e_nfb", [128, 1024], F32)
        ss = _sb(st, nc, "e_ss", [128, 4], F32)
        junk = _sb(st, nc, "e_junk", [128, 1024], BF16)
        psA = [_ps(st, nc, "e_psA%d" % i, [128, 512], F32) for i in range(2)]
        psU = [_ps(st, nc, "e_psU%d" % i, [128, 512], F32) for i in range(2)]
        psY = [_ps(st, nc, "e_psY%d" % i, [128, 512], F32) for i in range(3)]
        b_yacc = [Buf("yacc%d" % i) for i in range(8)]
        b_w = [Buf("ew0"), Buf("ew1")]
        b_hT = [Buf("ehT0"), Buf("ehT1")]
        b_afr = [Buf("eaf0"), Buf("eaf1")]
        b_gt = [Buf("egt0"), Buf("egt1")]
        b_sA = [Buf("esA0"), Buf("esA1")]
        b_act = [Buf("eact0"), Buf("eact1")]
        b_x, b_nfb, b_ss, b_junk = Buf("ex"), Buf("enfb"), Buf("ess"), Buf("ejunk")
        b_psA = [Buf("psA0"), Buf("psA1")]
        b_psU = [Buf("psU0"), Buf("psU1")]
        b_psY = [Buf("psY%d" % i) for i in range(3)]
        cnt = {"A": 0, "U": 0, "Y": 0, "sA": 0, "act": 0, "w": 0}

        def nxt(k, n):
            i = cnt[k] % n
            cnt[k] += 1
            return i
        if last:
            P.dma("sp", nfb[:], C.din["norm_final"].rearrange("(o d) -> o d", o=1).partition_broadcast(128), b_nfb, writes=[b_nfb])
        tl = [t for t in tiles_of(l) if not (last and t[2] == 1)]
        sts = []
        if not last:
            sts.append(tl[0:2])
            rest = tl[2:]
        else:
            rest = tl
        for i in range(0, len(rest), 2):
            sts.append(rest[i:i + 2])
        for stl in sts:
            blk0 = []
            nb_tot = 0
            for si, (t0, nb, r) in enumerate(stl):
                n = nb * 128
                P.dma("sp", hT[si][:, :, 0:n], C.h2T.rearrange("(k p) t -> p k t", p=128)[:, :, t0:t0 + n], b_hT[si],
                      reads=[C.b_h2T], writes=[b_hT[si]])
                P.dma("sp", afr[si][:, 0:nb, :], C.aff[t0:t0 + n, :].rearrange("(b p) e -> p b e", p=128), b_afr[si],
                      reads=[C.b_aff], writes=[b_afr[si]])
                thr = C.thr[:, 16 * r:16 * r + 16].rearrange("p (o e) -> p o e", o=1).to_broadcast([128, nb, NE])
                P.op("dve", I("tensor_tensor", out=gt[si][:, 0:nb, :], in0=afr[si][:, 0:nb, :], in1=thr, op=ALU.is_gt),
                     reads=[b_afr[si], C.b_thr], writes=[b_gt[si]])
                P.op("dve", I("tensor_tensor", out=gt[si][:, 0:nb, :], in0=gt[si][:, 0:nb, :], in1=afr[si][:, 0:nb, :], op=ALU.mult),
                     reads=[b_gt[si], b_afr[si]], writes=[b_gt[si]])
                blk0.append(nb_tot)
                nb_tot += nb
            for e in range(NE):
                wi = nxt("w", 2)
                P.dma("sp", wg[wi][:], C.wb["w_gate"][l, e].rearrange("(k p) f -> p k f", p=128), b_w[wi],
                      reads=[C.wbuf[("w_gate", l, e)]], writes=[b_w[wi]])
                P.dma("sp", wu[wi][:], C.wb["w_up"][l, e].rearrange("(k p) f -> p k f", p=128), b_w[wi],
                      reads=[C.wbuf[("w_up", l, e)]], writes=[b_w[wi]])
                P.dma("sp", wd[wi][:], C.wb["w_down"][l, e].rearrange("(k p) d -> p k d", p=128), b_w[wi],
                      reads=[C.wbuf[("w_down", l, e)]], writes=[b_w[wi]])
                for si, (t0, nb, r) in enumerate(stl):
                    n = nb * 128
                    ai = nxt("act", 2)
                    for fc in range(4):
                        pa, pu = nxt("A", 2), nxt("U", 2)
                        for k in range(8):
                            P.mm(I("matmul", psA[pa][:, 0:n], lhsT=wg[wi][:, k, fc * 128:(fc + 1) * 128], rhs=hT[si][:, k, 0:n],
                                   start=(k == 0), stop=(k == 7)), reads=[b_w[wi], b_hT[si]], writes=[b_psA[pa]])
                        for k in range(8):
                            P.mm(I("matmul", psU[pu][:, 0:n], lhsT=wu[wi][:, k, fc * 128:(fc + 1) * 128], rhs=hT[si][:, k, 0:n],
                                   start=(k == 0), stop=(k == 7)), reads=[b_w[wi], b_hT[si]], writes=[b_psU[pu]])
                        sa = nxt("sA", 2)
                        P.op("act", I("activation", out=sA[sa][:, 0:n], in_=psA[pa][:, 0:n], func=AF.Silu),
                             reads=[b_psA[pa]], writes=[b_sA[sa]])
                        P.op("dve", I("tensor_tensor", out=act[ai][:, fc, 0:n], in0=sA[sa][:, 0:n], in1=psU[pu][:, 0:n], op=ALU.mult),
                             reads=[b_sA[sa], b_psU[pu]], writes=[b_act[ai]])
                    for b in range(nb):
                        blk = blk0[si] + b
                        for hf in range(2):
                            py = nxt("Y", 3)
                            for fc in range(4):
                                P.mm(I("matmul", psY[py][:, :], lhsT=act[ai][:, fc, b * 128:(b + 1) * 128],
                                       rhs=wd[wi][:, fc, hf * 512:(hf + 1) * 512], start=(fc == 0), stop=(fc == 3)),
                                     reads=[b_act[ai], b_w[wi]], writes=[b_psY[py]])
                            ya = yacc[:, blk, hf * 512:(hf + 1) * 512]
                            if e == 0:
                                P.op("dve", I("tensor_scalar", out=ya, in0=psY[py][:, :], scalar1=gt[si][:, b, e:e + 1],
                                              scalar2=None, op0=ALU.mult),
                                     reads=[b_psY[py], b_gt[si]], writes=[b_yacc[blk]])
                            else:
                                P.op("dve", I("scalar_tensor_tensor", out=ya, in0=psY[py][:, :], scalar=gt[si][:, b, e:e + 1],
                                              in1=ya, op0=ALU.mult, op1=ALU.add),
                                     reads=[b_psY[py], b_gt[si], b_yacc[blk]], writes=[b_yacc[blk]])
            for si, (t0, nb, r) in enumerate(stl):
                n = nb * 128
                P.dma("sp", xt[:, 0:nb, :], C.xs[t0:t0 + n, :].rearrange("(b p) d -> p b d", p=128), b_x,
                      reads=[C.b_xs], writes=[b_x])
                for b in range(nb):
                    blk = blk0[si] + b
                    P.op("dve", I("tensor_tensor", out=yacc[:, blk, :], in0=yacc[:, blk, :], in1=C.gate_bc[1][:, r, :], op=ALU.mult),
                         reads=[b_yacc[blk], C.b_gate[1]], writes=[b_yacc[blk]])
                    P.op("pool", I("tensor_tensor", out=xt[:, b, :], in0=xt[:, b, :], in1=yacc[:, blk, :], op=ALU.add),
                         reads=[b_x, b_yacc[blk]], writes=[b_x])
                if not last:
                    P.dma("sp", C.xs[t0:t0 + n, :].rearrange("(b p) d -> p b d", p=128), xt[:, 0:nb, :], b_x,
                          reads=[b_x], writes=[C.b_xs])
                else:
                    for b in range(nb):
                        P.op("act", I("activation", out=junk[:], in_=xt[:, b, :], func=AF.Square, accum_out=ss[:, b:b + 1]),
                             reads=[b_x], writes=[b_junk, b_ss])
                    P.op("act", I("activation", out=ss[:, 0:nb], in_=ss[:, 0:nb], func=AF.Ln, scale=1.0 / D, bias=C.eps_col[:]),
                         reads=[b_ss], writes=[b_ss])
                    P.op("act", I("activation", out=ss[:, 0:nb], in_=ss[:, 0:nb], func=AF.Exp, scale=-0.5), reads=[b_ss], writes=[b_ss])
                    for b in range(nb):
                        P.op("dve", I("scalar_tensor_tensor", out=xt[:, b, :], in0=xt[:, b, :], scalar=ss[:, b:b + 1], in1=nfb[:],
                                      op0=ALU.mult, op1=ALU.mult), reads=[b_x, b_ss, b_nfb], writes=[b_x])
                    P.dma("sp", C.out[t0 - CT:t0 - CT + n, :].rearrange("(b p) d -> p b d", p=128), xt[:, 0:nb, :], b_x,
                          reads=[b_x], writes=[C.b_out])
        P.barrier()
        P.end_phase()


def phase_idx(C, l):
    P, nc = C.P, C.nc
    last = (l == DEPTH - 1)
    nsets = 1 if last else 2
    with ExitStack() as st:
        c2 = _sb(st, nc, "i_c2", [128, 2048], F32)
        affp = _sb(st, nc, "i_affp", [128, NBLK, NE], F32)
        maskb = _sb(st, nc, "i_mask", [128, NBLK, NE], BF16)
        trib = _sb(st, nc, "i_trib", [128, 128], BF16)
        onesb = _sb(st, nc, "i_onesb", [128, 1], BF16)
        cum_sb = _sb(st, nc, "i_cum", [128, NBLK, NE], F32)
        tot = _sb(st, nc, "i_tot", [128, NE], F32)
        cumB = _sb(st, nc, "i_cumB", [128, NE], F32)
        offB = _sb(st, nc, "i_offB", [128, NE], F32)
        cumS = _sb(st, nc, "i_cumS", [128, 2, NE], F32)
        offS = _sb(st, nc, "i_offS", [128, 2, NE], F32)
        cumx = _sb(st, nc, "i_cumx", [128, NE, 132], F32)
        t1 = _sb(st, nc, "i_t1", [128, 1024], F32)
        oh = [_sb(st, nc, "i_oh%d" % i, [128, 1024], F32) for i in range(2)]
        junk3 = _sb(st, nc, "i_junk3", [128, 8, 128], F32)
        rr = _sb(st, nc, "i_rr", [128, 8], F32)
        idf = _sb(st, nc, "i_idf", [128, 8], F32)
        gfull = _sb(st, nc, "i_gfull", [128, 8, 132], F32)
        b_c2, b_affp, b_mask, b_cum, b_tot, b_cb, b_cs, b_cumx = (Buf("c2"), Buf("affp"), Buf("mask"), Buf("cum"), Buf("tot"),
                                                                 Buf("cb"), Buf("cs"), Buf("cumx"))
        b_t1, b_junk, b_rr, b_idf, b_gsb = Buf("t1"), Buf("junk"), Buf("rr"), Buf("idf"), Buf("gsb")
        b_oh = [Buf("oh0"), Buf("oh1")]
        P.dma("sp", c2[:], C.din["cst2"], b_c2, writes=[b_c2])
        P.dma("sp", affp[:], C.aff[0:NT, :].rearrange("(b p) e -> p b e", p=128), b_affp, reads=[C.b_aff], writes=[b_affp])
        P.op("dve", I("tensor_copy", out=trib[:], in_=c2[:, 0:128]), reads=[b_c2], writes=[b_c2])
        P.op("dve", I("memset", onesb[:], 1.0), writes=[b_c2])
        P.op("dve", I("tensor_tensor", out=maskb[:, 2:NBLK, :], in0=affp[:, 2:NBLK, :],
                      in1=C.thr[:, 0:16].rearrange("p (o e) -> p o e", o=1).to_broadcast([128, NBLK - 2, NE]), op=ALU.is_gt),
             reads=[b_affp, C.b_thr], writes=[b_mask])
        if last:
            P.op("dve", I("memset", maskb[:, 0:2, :], 0.0), writes=[b_mask])
        else:
            P.op("dve", I("tensor_tensor", out=maskb[:, 0:2, :], in0=affp[:, 0:2, :],
                          in1=C.thr[:, 16:32].rearrange("p (o e) -> p o e", o=1).to_broadcast([128, 2, NE]), op=ALU.is_gt),
                 reads=[b_affp, C.b_thr], writes=[b_mask])
        import os
        STOP = int(os.environ.get("IDX_STOP", "9"))
        if STOP <= 1:
            P.barrier()
            P.end_phase()
            return
        with ExitStack() as st2:
            ps_cum = [_ps(st2, nc, "i_pscum%d" % i, [128, 352], F32) for i in range(3)]
            ps_tot = _ps(st2, nc, "i_pstot", [128, NE], F32)
            ps_cb = _ps(st2, nc, "i_pscb", [128, NE], F32)
            b_pscum = [Buf("pscum%d" % i) for i in range(3)]
            b_pstot, b_pscb = Buf("pstot"), Buf("pscb")
            mflat = maskb[:].rearrange("p b e -> p (b e)")
            cflat = cum_sb[:].rearrange("p b e -> p (b e)")
            for j in range(3):
                P.mm(I("matmul", ps_cum[j][:, :], lhsT=trib[:], rhs=mflat[:, j * 352:(j + 1) * 352], start=True, stop=True),
                     reads=[b_mask, b_c2], writes=[b_pscum[j]])
                P.op("act", I("activation", out=cflat[:, j * 352:(j + 1) * 352], in_=ps_cum[j][:, :], func=AF.Copy),
                     reads=[b_pscum[j]], writes=[b_cum])
            for e in range(NE if STOP > 2 else 0):
                P.mm(I("matmul", ps_tot[0:NBLK, e:e + 1], lhsT=maskb[:, :, e], rhs=onesb[:], start=True, stop=True),
                     reads=[b_mask, b_c2], writes=[b_pstot])
            P.op("dve", I("tensor_copy", out=tot[0:NBLK, :], in_=ps_tot[0:NBLK, :]), reads=[b_pstot], writes=[b_tot])
            P.mm(I("matmul", ps_cb[0:NBLK, :], lhsT=c2[0:NBLK, 128:128 + NBLK], rhs=tot[0:NBLK, :], start=True, stop=True),
                 reads=[b_tot, b_c2], writes=[b_pscb])
            P.op("dve", I("tensor_copy", out=cumB[0:NBLK, :], in_=ps_cb[0:NBLK, :]), reads=[b_pscb], writes=[b_cb])
            P.op("dve", I("tensor_tensor", out=offB[0:NBLK, :], in0=cumB[0:NBLK, :], in1=tot[0:NBLK, :], op=ALU.subtract),
                 reads=[b_cb, b_tot], writes=[b_cb])
            for sset in range(2):
                col = c2[0:NBLK, 256 + sset:257 + sset]
                P.op("dve", I("tensor_scalar", out=cumS[0:NBLK, sset, :], in0=cumB[0:NBLK, :], scalar1=col, scalar2=None, op0=ALU.mult),
                     reads=[b_cb, b_c2], writes=[b_cs])
                P.op("dve", I("tensor_scalar", out=offS[0:NBLK, sset, :], in0=offB[0:NBLK, :], scalar1=col, scalar2=None, op0=ALU.mult),
                     reads=[b_cb, b_c2], writes=[b_cs])
            P.barrier()
        if STOP <= 3:
            P.end_phase()
            return
        with ExitStack() as st3:
            ps_tr = [_ps(st3, nc, "i_pstr%d" % i, [128, 512], F32) for i in range(2)]
            ps_g = [_ps(st3, nc, "i_psg%d" % i, [128, 512], F32) for i in range(2)]
            b_pstr = [Buf("pstr0"), Buf("pstr1")]
            b_psg = [Buf("psg0"), Buf("psg1")]
            for e in range(NE):
                bank = (e // 4) % 2
                reg = ps_tr[bank][0:NBLK, (e % 4) * 128:(e % 4 + 1) * 128]
                P.mm(I("transpose", reg, cum_sb[:, :, e], C.ident[:]), reads=[b_cum], writes=[b_pstr[bank]])
                P.op("dve", I("tensor_scalar", out=cumx[0:NBLK, e, 0:128], in0=reg, scalar1=offB[0:NBLK, e:e + 1], scalar2=None,
                              op0=ALU.add), reads=[b_pstr[bank], b_cb], writes=[b_cumx])
                P.op("pool", I("tensor_copy", out=cumx[0:NBLK, e, 128:130], in_=c2[0:NBLK, 258:260]), reads=[b_c2], writes=[b_cumx])
            it = 0
            for sset in range(nsets if STOP > 4 else 0):
                nsb = 8 if sset == 0 else 1
                ns = nsb * 128
                for e in range(NE):
                    o = it % 2
                    it += 1
                    iota_s = c2[0:NBLK, 1024:1024 + ns]
                    P.op("dve", I("tensor_scalar", out=t1[0:NBLK, 0:ns], in0=iota_s, scalar1=offS[0:NBLK, sset, e:e + 1], scalar2=None,
                                  op0=ALU.is_ge), reads=[b_c2, b_cs], writes=[b_t1])
                    P.op("dve", I("tensor_scalar", out=oh[o][0:NBLK, 0:ns], in0=iota_s, scalar1=cumS[0:NBLK, sset, e:e + 1], scalar2=None,
                                  op0=ALU.is_lt), reads=[b_c2, b_cs], writes=[b_oh[o]])
                    P.op("dve", I("tensor_tensor", out=oh[o][0:NBLK, 0:ns], in0=oh[o][0:NBLK, 0:ns], in1=t1[0:NBLK, 0:ns], op=ALU.mult),
                         reads=[b_oh[o], b_t1], writes=[b_oh[o]])
                    if STOP <= 5:
                        continue
                    for sb in range(nsb):
                        gi = sb % 2
                        P.mm(I("matmul", ps_g[gi][:, 0:130], lhsT=oh[o][0:NBLK, sb * 128:(sb + 1) * 128], rhs=cumx[0:NBLK, e, 0:130],
                               start=True, stop=True), reads=[b_oh[o], b_cumx], writes=[b_psg[gi]])
                        if STOP <= 6:
                            continue
                        P.op("act", I("activation", out=gfull[:, sb, 0:130], in_=ps_g[gi][:, 0:130], func=AF.Copy),
                             reads=[b_psg[gi]], writes=[b_gsb])
                    if STOP <= 7:
                        continue
                    P.op("dve", I("tensor_tensor", out=junk3[:, 0:nsb, :], in0=gfull[:, 0:nsb, 0:128],
                                  in1=c2[:, 260:260 + nsb].rearrange("p (b o) -> p b o", o=1).to_broadcast([128, nsb, 128]),
                                  op=ALU.is_le), reads=[b_gsb, b_c2], writes=[b_junk])
                    P.op("dve", I("tensor_reduce", out=rr[:, 0:nsb], in_=junk3[:, 0:nsb, :], axis=AX.X, op=ALU.add),
                         reads=[b_junk], writes=[b_rr])
                    P.op("dve", I("tensor_tensor", out=idf[:, 0:nsb], in0=rr[:, 0:nsb], in1=gfull[:, 0:nsb, 128], op=ALU.add),
                         reads=[b_rr, b_gsb], writes=[b_idf])
                    P.op("dve", I("tensor_scalar", out=idf[:, 0:nsb], in0=idf[:, 0:nsb], scalar1=c2[:, 268:269], scalar2=None,
                                  op0=ALU.subtract), reads=[b_idf, b_c2], writes=[b_idf])
                    P.op("dve", I("tensor_tensor", out=idf[:, 0:nsb], in0=idf[:, 0:nsb], in1=gfull[:, 0:nsb, 129], op=ALU.mult),
                         reads=[b_idf, b_gsb], writes=[b_idf])
                    P.op("dve", I("tensor_scalar", out=idf[:, 0:nsb], in0=idf[:, 0:nsb], scalar1=c2[:, 268:269], scalar2=None,
                                  op0=ALU.add), reads=[b_idf, b_c2], writes=[b_idf])
                    P.op("dve", I("tensor_copy", out=C.idxu[:, sset, e, 0:nsb], in_=idf[:, 0:nsb]), reads=[b_idf], writes=[C.b_idxu])
            P.barrier()
        P.end_phase()


def _indirect(C, kind, sb_ap, dram_ap, idx_ap, chan, reads, writes):
    P = C.P
    key = P._chan(chan, "pool")
    waits = P._deps("pool", reads, writes)
    P.semcnt[key] += 16
    me = (key, P.semcnt[key])
    if kind == "g":
        fn = I("indirect_dma_start", out=sb_ap, out_offset=None, in_=dram_ap,
               in_offset=bass.IndirectOffsetOnAxis(ap=idx_ap, axis=0))
    else:
        fn = I("indirect_dma_start", out=dram_ap, out_offset=bass.IndirectOffsetOnAxis(ap=idx_ap, axis=0),
               in_=sb_ap, in_offset=None, compute_op=ALU.add)
    P._emit("pool", waits, fn, P.sems[key], 16)
    P._record(me, reads, writes)


def phase_moe2(C, l):
    P, nc = C.P, C.nc
    last = (l == DEPTH - 1)
    nsets = 1 if last else 2
    with ExitStack() as st:
        wg = [_sb(st, nc, "e_wg%d" % i, [128, 8, DFF], BF16) for i in range(2)]
        wu = [_sb(st, nc, "e_wu%d" % i, [128, 8, DFF], BF16) for i in range(2)]
        wd = [_sb(st, nc, "e_wd%d" % i, [128, 4, 1024], BF16) for i in range(2)]
        xg = [_sb(st, nc, "e_xg%d" % i, [128, 1024], BF16) for i in range(8)]
        ag = [_sb(st, nc, "e_ag%d" % i, [128, NE], F32) for i in range(8)]
        xT = [_sb(st, nc, "e_xT%d" % i, [128, 8, 512], BF16) for i in range(2)]
        sA = [_sb(st, nc, "e_sA%d" % i, [128, 512], F32) for i in range(2)]
        act = [_sb(st, nc, "e_act%d" % i, [128, 4, 512], BF16) for i in range(2)]
        ysb = [_sb(st, nc, "e_y%d" % i, [128, 1024], F32) for i in range(3)]
        identb = _sb(st, nc, "e_identb", [128, 128], BF16)
        ps_t = [_ps(st, nc, "e_pst%d" % i, [128, 8, 128], BF16) for i in range(2)]
        psA = [_ps(st, nc, "e_psA%d" % i, [128, 512], F32) for i in range(2)]
        psU = [_ps(st, nc, "e_psU%d" % i, [128, 512], F32) for i in range(2)]
        psY = [_ps(st, nc, "e_psY%d" % i, [128, 512], F32) for i in range(2)]
        b_w = [Buf("ew0"), Buf("ew1")]
        b_xg = [Buf("xg%d" % i) for i in range(8)]
        b_ag = [Buf("ag%d" % i) for i in range(8)]
        b_xT = [Buf("xT0"), Buf("xT1")]
        b_sA = [Buf("sA0"), Buf("sA1")]
        b_act = [Buf("act0"), Buf("act1")]
        b_y = [Buf("y%d" % i) for i in range(3)]
        b_pst = [Buf("pst0"), Buf("pst1")]
        b_psA, b_psU = [Buf("psA0"), Buf("psA1")], [Buf("psU0"), Buf("psU1")]
        b_psY = [Buf("psY%d" % i) for i in range(2)]
        b_ib = Buf("identb")
        cnt = {"xg": 0, "ag": 0, "t": 0, "xT": 0, "sA": 0, "act": 0, "Y": 0, "y": 0, "w": 0, "A": 0}

        def nxt(k, n):
            i = cnt[k] % n
            cnt[k] += 1
            return i
        P.op("dve", I("tensor_copy", out=identb[:], in_=C.ident[:]), reads=[C.b_const], writes=[b_ib])
        jobs = [(sset, e) for sset in range(nsets) for e in range(NE)]

        def load_w(ji):
            sset, e = jobs[ji]
            wi = ji % 2
            P.dma("sp", wg[wi][:], C.wb["w_gate"][l, e].rearrange("(k p) f -> p k f", p=128), b_w[wi],
                  reads=[C.wbuf[("w_gate", l, e)]], writes=[b_w[wi]])
            P.dma("sp", wu[wi][:], C.wb["w_up"][l, e].rearrange("(k p) f -> p k f", p=128), b_w[wi],
                  reads=[C.wbuf[("w_up", l, e)]], writes=[b_w[wi]])
            P.dma("sp", wd[wi][:], C.wb["w_down"][l, e].rearrange("(k p) d -> p k d", p=128), b_w[wi],
                  reads=[C.wbuf[("w_down", l, e)]], writes=[b_w[wi]])
        tiles = []
        for ji, (sset, e) in enumerate(jobs):
            nsb = 8 if sset == 0 else 1
            for tb in range(0, nsb, 4):
                tiles.append((ji, sset, e, list(range(tb, min(tb + 4, nsb)))))

        def gathers(t):
            ji, sset, e, sbs = tiles[t]
            for j, sb in enumerate(sbs):
                k = (t % 2) * 4 + j
                idx_ap = C.idxu[:, sset, e, sb:sb + 1]
                _indirect(C, "g", xg[k][:], C.h2tok, idx_ap, b_xg[k], [C.b_idxu, C.b_h2tok], [b_xg[k]])
                _indirect(C, "g", ag[k][:], C.aff, idx_ap, b_ag[k], [C.b_idxu, C.b_aff], [b_ag[k]])
        load_w(0)
        gathers(0)
        last_ji = -1
        for t, (ji, sset, e, sbs) in enumerate(tiles):
            if ji != last_ji:
                last_ji = ji
                if ji + 1 < len(jobs):
                    load_w(ji + 1)
            if t + 1 < len(tiles):
                gathers(t + 1)
            wi = ji % 2
            n = len(sbs) * 128
            xi = t % 2
            for j, sb in enumerate(sbs):
                k = (t % 2) * 4 + j
                ti = nxt("t", 2)
                for c in range(8):
                    P.mm(I("transpose", ps_t[ti][:, c, :], xg[k][:, c * 128:(c + 1) * 128], identb[:]),
                         reads=[b_xg[k], b_ib], writes=[b_pst[ti]])
                if j % 2 == 0:
                    P.op("act", I("activation", out=xT[xi][:, :, j * 128:(j + 1) * 128], in_=ps_t[ti][:], func=AF.Copy),
                         reads=[b_pst[ti]], writes=[b_xT[xi]])
                else:
                    P.op("dve", I("tensor_copy", out=xT[xi][:, :, j * 128:(j + 1) * 128], in_=ps_t[ti][:]),
                         reads=[b_pst[ti]], writes=[b_xT[xi]])
            a_i = nxt("act", 2)
            for fc in range(4):
                pa = nxt("A", 2)
                for k in range(8):
                    P.mm(I("matmul", psA[pa][:, 0:n], lhsT=wg[wi][:, k, fc * 128:(fc + 1) * 128], rhs=xT[xi][:, k, 0:n],
                           start=(k == 0), stop=(k == 7)), reads=[b_w[wi], b_xT[xi]], writes=[b_psA[pa]])
                for k in range(8):
                    P.mm(I("matmul", psU[pa][:, 0:n], lhsT=wu[wi][:, k, fc * 128:(fc + 1) * 128], rhs=xT[xi][:, k, 0:n],
                           start=(k == 0), stop=(k == 7)), reads=[b_w[wi], b_xT[xi]], writes=[b_psU[pa]])
                sa = nxt("sA", 2)
                P.op("act", I("activation", out=sA[sa][:, 0:n], in_=psA[pa][:, 0:n], func=AF.Silu),
                     reads=[b_psA[pa]], writes=[b_sA[sa]])
                P.op("dve", I("tensor_tensor", out=act[a_i][:, fc, 0:n], in0=sA[sa][:, 0:n], in1=psU[pa][:, 0:n], op=ALU.mult),
                     reads=[b_sA[sa], b_psU[pa]], writes=[b_act[a_i]])
            for j, sb in enumerate(sbs):
                k = (t % 2) * 4 + j
                yi = nxt("y", 3)
                for hf in range(2):
                    py = nxt("Y", 2)
                    for fc in range(4):
                        P.mm(I("matmul", psY[py][:, :], lhsT=act[a_i][:, fc, j * 128:(j + 1) * 128],
                               rhs=wd[wi][:, fc, hf * 512:(hf + 1) * 512], start=(fc == 0), stop=(fc == 3)),
                             reads=[b_act[a_i], b_w[wi]], writes=[b_psY[py]])
                    P.op("dve", I("scalar_tensor_tensor", out=ysb[yi][:, hf * 512:(hf + 1) * 512], in0=psY[py][:, :],
                                  scalar=ag[k][:, e:e + 1], in1=C.gate_bc[1][:, sset, hf * 512:(hf + 1) * 512],
                                  op0=ALU.mult, op1=ALU.mult),
                         reads=[b_psY[py], b_ag[k], C.b_gate[1]], writes=[b_y[yi]])
                _indirect(C, "s", ysb[yi][:], C.xs, C.idxu[:, sset, e, sb:sb + 1], b_y[yi], [C.b_idxu, b_y[yi]], [C.b_xs])
        P.barrier()
        P.end_phase()


def phase_final(C):
    P, nc = C.P, C.nc
    with ExitStack() as st:
        xt = [_sb(st, nc, "f_x%d" % i, [128, 4, 1024], F32) for i in range(2)]
        nfb = _sb(st, nc, "f_nfb", [128, 1024], F32)
        ss = [_sb(st, nc, "f_ss%d" % i, [128, 4], F32) for i in range(2)]
        junk = _sb(st, nc, "f_junk", [128, 1024], BF16)
        b_x = [Buf("fx0"), Buf("fx1")]
        b_ss = [Buf("fss0"), Buf("fss1")]
        b_nfb, b_junk = Buf("fnfb"), Buf("fjunk")
        P.dma("sp", nfb[:], C.din["norm_final"].rearrange("(o d) -> o d", o=1).partition_broadcast(128), b_nfb, writes=[b_nfb])
        tl = [t for t in tiles_of(1) if t[2] == 0]

        def load(ti):
            t0, nb, r = tl[ti]
            P.dma("sp", xt[ti % 2][:], C.xs[t0:t0 + 512, :].rearrange("(b p) d -> p b d", p=128), b_x[ti % 2],
                  reads=[C.b_xs], writes=[b_x[ti % 2]])
        load(0)
        for ti, (t0, nb, r) in enumerate(tl):
            if ti + 1 < len(tl):
                load(ti + 1)
            i = ti % 2
            for b in range(4):
                P.op("act", I("activation", out=junk[:], in_=xt[i][:, b, :], func=AF.Square, accum_out=ss[i][:, b:b + 1]),
                     reads=[b_x[i]], writes=[b_junk, b_ss[i]])
            P.op("act", I("activation", out=ss[i][:], in_=ss[i][:], func=AF.Ln, scale=1.0 / D, bias=C.eps_col[:]),
                 reads=[b_ss[i]], writes=[b_ss[i]])
            P.op("act", I("activation", out=ss[i][:], in_=ss[i][:], func=AF.Exp, scale=-0.5), reads=[b_ss[i]], writes=[b_ss[i]])
            for b in range(4):
                eng = "dve" if b % 2 == 0 else "pool"
                if eng == "dve":
                    P.op("dve", I("scalar_tensor_tensor", out=xt[i][:, b, :], in0=xt[i][:, b, :], scalar=ss[i][:, b:b + 1], in1=nfb[:],
                                  op0=ALU.mult, op1=ALU.mult), reads=[b_x[i], b_ss[i], b_nfb], writes=[b_x[i]])
                else:
                    P.op("dve", I("scalar_tensor_tensor", out=xt[i][:, b, :], in0=xt[i][:, b, :], scalar=ss[i][:, b:b + 1], in1=nfb[:],
                                  op0=ALU.mult, op1=ALU.mult), reads=[b_x[i], b_ss[i], b_nfb], writes=[b_x[i]])
            P.dma("sp", C.out[t0 - CT:t0 - CT + 512, :].rearrange("(b p) d -> p b d", p=128), xt[i][:], b_x[i],
                  reads=[b_x[i]], writes=[C.b_out])
        P.barrier()
        P.end_phase()


IN_SPECS = [
    ("x", [SEQ, D], F32), ("ctx", [CT, D], F32), ("c", [D], F32), ("c_ctx", [D], F32),
    ("w_mod", [DEPTH, D, 6 * D], F32), ("b_mod", [DEPTH, 6 * D], F32),
    ("norm_attn", [DEPTH, D], F32), ("norm_ffn", [DEPTH, D], F32),
    ("w_in", [DEPTH, D, WIN_COLS], F32), ("sink", [DEPTH, 8], F32),
    ("q_norm", [DEPTH, 384], F32), ("kv_norm", [DEPTH, 256], F32),
    ("w_qu", [DEPTH, 384, 1024], F32), ("w_kvu", [DEPTH, 256, 1024], F32),
    ("w_out", [DEPTH, D, D], F32), ("w_router", [DEPTH, D, NE], F32),
    ("w_gate", [DEPTH, NE, D, DFF], F32), ("w_up", [DEPTH, NE, D, DFF], F32), ("w_down", [DEPTH, NE, DFF, D], F32),
    ("norm_final", [D], F32),
    ("rope", [128, 4, NT], F32),
    ("cst", [128, 1024], F32),
    ("cst2", [128, 2048], F32),
]


def build(upto="all", dumps=(), dense_moe=False):
    nc = bass.Bass("TRN2", target_bir_lowering=False)
    C = Ctx()
    C.dense_moe = dense_moe
    C.nc = nc
    C.din = {nm: nc.dram_tensor(nm, shp, dt, kind="ExternalInput").ap() for nm, shp, dt in IN_SPECS}

    def scratch(nm, shp, dt):
        kind = "ExternalOutput" if nm in dumps else "Internal"
        return nc.dram_tensor(nm, shp, dt, kind=kind).ap()
    C.out = nc.dram_tensor("out", [SEQ, D], F32, kind="ExternalOutput").ap()
    C.xs = scratch("xs", [NT + 128, D], F32)
    C.h2tok = scratch("h2tok", [NT + 128, D], BF16)
    C.mrow_d = scratch("mrow_d", [DEPTH, 2, 6 * D], F32)
    C.wb = {
        "w_in": scratch("wb_in", [DEPTH, D, WIN_COLS], BF16), "w_qu": scratch("wb_qu", [DEPTH, 384, 1024], BF16),
        "w_kvu": scratch("wb_kvu", [DEPTH, 256, 1024], BF16), "w_out": scratch("wb_out", [DEPTH, D, D], BF16),
        "w_gate": scratch("wb_gate", [DEPTH, NE, D, DFF], BF16), "w_up": scratch("wb_up", [DEPTH, NE, D, DFF], BF16),
        "w_down": scratch("wb_down", [DEPTH, NE, DFF, D], BF16),
    }
    C.wbuf = {}
    for l in range(DEPTH):
        bl = Buf("wcast%d" % l, persist=True)
        bl2 = Buf("wcastE%d" % l, persist=True)
        for nm in ("w_in", "w_qu", "w_kvu", "w_out"):
            C.wbuf[(nm, l)] = bl
        for nm in ("w_gate", "w_up", "w_down"):
            for e in range(NE):
                C.wbuf[(nm, l, e)] = bl2
    C.QaT = scratch("QaT", [2, 64, NBLK, 4, 128], BF16)
    C.KaT = scratch("KaT", [128, NT], BF16)
    C.Va = scratch("Va", [NT, 128], BF16)
    C.QbT = scratch("QbT", [8, 96, NT], BF16)
    C.KbT = scratch("KbT", [8, 96, NT], BF16)
    C.Vb = scratch("Vb", [NT, 512], BF16)
    C.OT = scratch("OT", [D, NT], BF16)
    C.h2T = scratch("h2T", [D, NT], BF16)
    C.aff = scratch("aff", [NT + 128, NE], F32)
    for nm in ("QaT", "KaT", "Va", "QbT", "KbT", "Vb", "OT", "h2T", "aff", "xs", "out", "h2tok", "mrow_d", "idxu"):
        setattr(C, "b_" + nm, Buf(nm))
    C.dbg = {}
    if "dbg_mod" in dumps:
        C.dbg["mod"] = nc.dram_tensor("dbg_mod", [128, 96], F32, kind="ExternalOutput").ap()

    with ExitStack() as st:
        P = Prog(nc, st)
        C.P = P
        C.ident = _sb(st, nc, "ident", [128, 128], F32)
        C.modT = _sb(st, nc, "modT", [128, 48, 2], F32)
        C.nrm = _sb(st, nc, "nrm", [128, 2, 8], F32)
        C.gcol = _sb(st, nc, "gcol", [128, 2, 2, 8], F32)
        C.gate_bc = [_sb(st, nc, "gate_bc%d" % i, [128, 2, 1024], F32) for i in range(2)]
        C.sel2 = _sb(st, nc, "sel2", [2, 2, 128], F32)
        C.eps_col = _sb(st, nc, "eps_col", [128, 1], F32)
        C.ones_q = _sb(st, nc, "ones_q", [128, 128], BF16)
        C.ones_kv = _sb(st, nc, "ones_kv", [128, 128], BF16)
        C.thr = _sb(st, nc, "thr", [128, 32], F32)
        C.idxu = _sb(st, nc, "idxu", [128, 2, NE, 8], U32)
        C.b_thr = Buf("thr")
        C.b_const, C.b_modT, C.b_nrm, C.b_gcol = Buf("const"), Buf("modT"), Buf("nrm"), Buf("gcol")
        C.b_gate = [Buf("gate0"), Buf("gate1")]
        block = st.enter_context(nc.Block())
        P.dma("sp", C.ident[:], C.din["cst"][:, 0:128], C.b_const, writes=[C.b_const])
        P.dma("sp", C.sel2[:].rearrange("k r m -> k (r m)"), C.din["cst"][0:2, 384:640], C.b_const, writes=[C.b_const])
        P.op("dve", I("memset", C.eps_col[:], EPS), writes=[C.b_const])
        P.op("dve", I("memset", C.ones_q[:], 1.0 / 384), writes=[C.b_const])
        P.op("dve", I("memset", C.ones_kv[:], 1.0 / 256), writes=[C.b_const])
        P.barrier()

        phase_init(C)
        phase_cast(C, 0, "small")
        for l in range(DEPTH):
            phase_mod(C, l)
            if upto == "mod":
                break
            phase_proj(C, l)
            if upto == "proj":
                break
            phase_attn(C, l)
            if upto == "attn":
                break
            phase_oproj(C, l)
            if upto == "oproj":
                break
            phase_thr(C, l)
            if upto == "thr":
                break
            if C.dense_moe:
                phase_moe(C, l)
            else:
                phase_idx(C, l)
                if upto == "idx":
                    break
                phase_moe2(C, l)
                if l == DEPTH - 1:
                    phase_final(C)
            if upto == "moe":
                break
        if "dbg_idx" in dumps:
            P.dma("sp", nc.dram_tensor("dbg_idx", [128, 256], U32, kind="ExternalOutput").ap(),
                  C.idxu[:].rearrange("p s e b -> p (s e b)"), C.b_idxu, reads=[C.b_idxu], writes=[Buf("x3")])
        if "dbg_thr" in dumps:
            P.dma("sp", nc.dram_tensor("dbg_thr", [128, 32], F32, kind="ExternalOutput").ap(), C.thr[:], C.b_thr, reads=[C.b_thr], writes=[Buf("x2")])
        if "mod" in C.dbg:
            P.dma("sp", C.dbg["mod"], C.modT[:].rearrange("p j r -> p (j r)"), C.b_modT, reads=[C.b_modT], writes=[Buf("x")])
        P.barrier()
        P.replay(block)
    print("instructions (incl waits):", P.ninstr, "dma sems:", len(P.semcnt))
    return nc


def _swap_idx(dh):
    nf = dh // 4
    idx = np.arange(dh)
    out = idx.copy()
    for base in (0, dh // 2):
        out[base:base + nf] = idx[base + nf:base + 2 * nf]
        out[base + nf:base + 2 * nf] = idx[base:base + nf]
    return out


def _rope_tables():
    t = np.arange(SEQ)
    rows, cols = t // 64, t % 64

    def tab(dh):
        da = dh // 2
        nf = da // 2
        inv = (10000.0 ** (-np.arange(nf, dtype=np.float32) / nf)).astype(np.float32)
        cos = np.ones((dh, NT), np.float32)
        sin = np.zeros((dh, NT), np.float32)
        for base, pos in ((0, rows), (da, cols)):
            ang = pos.astype(np.float32)[None, :] * inv[:, None]
            cos[base:base + nf, CT:] = np.cos(ang)
            cos[base + nf:base + 2 * nf, CT:] = np.cos(ang)
            sin[base:base + nf, CT:] = -np.sin(ang)
            sin[base + nf:base + 2 * nf, CT:] = np.sin(ang)
        return cos, sin
    ca, sa = tab(64)
    cb, sb = tab(32)
    out = np.zeros((128, 4, NT), np.float32)
    out[:, 0] = np.tile(ca, (2, 1))
    out[:, 1] = np.tile(sa, (2, 1))
    out[:, 2] = np.tile(cb, (4, 1))
    out[:, 3] = np.tile(sb, (4, 1))
    return out


def _consts():
    c = np.zeros((128, 1024), np.float32)
    c[:, 0:128] = np.eye(128, dtype=np.float32)
    s = np.arange(128)[:, None]
    q = np.arange(128)[None, :]
    c[:, 128:256] = (s >= q)
    c[:, 256:384] = (s <= q)
    c[0, 384:512] = 1.0
    c[1, 512:640] = 1.0
    c[:, 640] = np.arange(128)
    return c


def _consts2():
    c = np.zeros((128, 2048), np.float32)
    pp = np.arange(128)
    c[:, 0:128] = (pp[:, None] <= pp[None, :])
    bb = np.arange(NBLK)
    same = ((bb[:, None] < 2) == (bb[None, :] < 2))
    c[0:NBLK, 128:128 + NBLK] = ((bb[:, None] <= bb[None, :]) & same)
    c[0:NBLK, 256] = (bb >= 2)
    c[0:NBLK, 257] = (bb < 2)
    c[0:NBLK, 258] = bb * 128
    c[:, 259] = 1.0
    for sb in range(8):
        c[:, 260 + sb] = sb * 128 + pp
    c[:, 268] = NT + pp
    c[:, 1024:2048] = np.arange(1024)[None, :]
    return c


def prep_inputs(inp):
    f = lambda a: np.ascontiguousarray(np.asarray(a, dtype=np.float32))
    w_in = f(inp["w_in"])
    sa = _swap_idx(64)
    sb = _swap_idx(32)
    qa = w_in[:, :, 0:512]
    ka = w_in[:, :, 512:640]
    va = w_in[:, :, 640:768]
    cq = w_in[:, :, 768:1152]
    ckv = w_in[:, :, 1152:1408]
    kr = w_in[:, :, 1408:1440]
    qa_sw = qa.reshape(DEPTH, D, 8, 64)[..., sa].reshape(DEPTH, D, 512)
    ka_sw = ka.reshape(DEPTH, D, 2, 64)[..., sa].reshape(DEPTH, D, 128)
    kr_sw = kr[..., sb]
    w_in2 = np.concatenate([qa, qa_sw, ka, ka_sw, cq, ckv, kr, kr_sw, va], axis=-1)
    assert w_in2.shape[-1] == WIN_COLS
    wq = f(inp["w_q_up"]).reshape(DEPTH, 384, 8, 96)
    q_nope = wq[..., :64].reshape(DEPTH, 384, 512)
    q_rope = wq[..., 64:]
    w_qu = np.concatenate([q_nope, q_rope.reshape(DEPTH, 384, 256), q_rope[..., sb].reshape(DEPTH, 384, 256)], axis=-1)
    wkv = f(inp["w_kv_up"]).reshape(DEPTH, 256, 8, 128)
    w_kvu = np.concatenate([wkv[..., :64].reshape(DEPTH, 256, 512), wkv[..., 64:].reshape(DEPTH, 256, 512)], axis=-1)
    shared = {
        "c_ctx": f(inp["c_ctx"]), "w_mod": f(inp["w_mod"]), "b_mod": f(inp["b_mod"]),
        "norm_attn": f(inp["norm_attn"]), "norm_ffn": f(inp["norm_ffn"]),
        "w_in": np.ascontiguousarray(w_in2), "sink": f(inp["sink"]), "q_norm": f(inp["q_norm"]), "kv_norm": f(inp["kv_norm"]),
        "w_qu": np.ascontiguousarray(w_qu), "w_kvu": np.ascontiguousarray(w_kvu), "w_out": f(inp["w_out"]),
        "w_router": f(inp["w_router"]), "w_gate": f(inp["w_gate"]), "w_up": f(inp["w_up"]), "w_down": f(inp["w_down"]),
        "norm_final": f(inp["norm_final"]), "rope": _rope_tables(), "cst": _consts(), "cst2": _consts2(),
    }
    x = f(inp["x"])
    c = f(inp["c"])
    ctx = f(inp["ctx"])
    maps = []
    for core in range(8):
        b = core // 2
        m = dict(shared)
        m["x"] = x[b]
        m["ctx"] = ctx[b]
        m["c"] = c[b]
        maps.append(m)
    return maps


def kernel(**inputs):
    nc = build()
    maps = prep_inputs(inputs)
    res = run_bass_kernel_spmd(nc, maps, core_ids=list(range(8)))
    out = np.stack([res.results[2 * b]["out"] for b in range(4)], axis=0)
    return out.astype(np.float32)
```
